# Optimizing a Trainium2 kernel written in Bass

```python
import math
import jax, jax.numpy as jnp
from jax import lax
import numpy as np

D_MODEL = 1024
BATCH = 16
SEQ = 2048
DEPTH = 1

CHUNK = 64
Q_BLOCK = 128
N_HEADS_A = 4
HEAD_DIM_A = 128
D_QK_A = 2 * N_HEADS_A * HEAD_DIM_A
D_V_A = 2 * N_HEADS_A * HEAD_DIM_A
D_CONV = D_MODEL
CONV_WIDTH = 3
N_BUCKETS = 32
MAX_DISTANCE = 128
N_GROUPS = 4
EXPERTS_PER_GROUP = 8
N_EXPERTS = N_GROUPS * EXPERTS_PER_GROUP
TOP_K = 2
D_EXPERT = 512
MOE_BLOCK = 512
LN_EPS = 1e-5
RMS_EPS = 1e-6
DEEPNORM_ALPHA = (2.0 * DEPTH) ** 0.25
DEEPNORM_BETA = (8.0 * DEPTH) ** -0.25
N_IN = 2 * D_QK_A + D_V_A + 3 * D_CONV + 2 * D_MODEL
SPLITS = (D_QK_A, 2 * D_QK_A, 2 * D_QK_A + D_V_A,
          2 * D_QK_A + D_V_A + D_CONV, 2 * D_QK_A + D_V_A + 2 * D_CONV,
          2 * D_QK_A + D_V_A + 3 * D_CONV, 2 * D_QK_A + D_V_A + 3 * D_CONV + D_MODEL)

kernel_name = 'hybrid_diffattn_shortconv_hiermoe_deepnorm'


def layer_norm(x, g, b):
    xf = x.astype(jnp.float32)
    mu = jnp.mean(xf, axis=-1, keepdims=True)
    var = jnp.mean(jnp.square(xf - mu), axis=-1, keepdims=True)
    y = (xf - mu) * lax.rsqrt(var + LN_EPS) * g.astype(jnp.float32) + b.astype(jnp.float32)
    return y.astype(x.dtype)


def t5_bucket(rel):
    nb = N_BUCKETS // 2
    max_exact = nb // 2
    bucket = jnp.where(rel > 0, nb, 0)
    n = jnp.abs(rel)
    nf = jnp.maximum(n, 1).astype(jnp.float32)
    large = max_exact + (jnp.log(nf / max_exact) / math.log(MAX_DISTANCE / max_exact)
                         * (nb - max_exact)).astype(jnp.int32)
    large = jnp.minimum(large, nb - 1)
    return bucket + jnp.where(n < max_exact, n, large)


def diff_attention(q, k, v, rel_bias, lam, lam_init, subln_g):
    bsz, seq = q.shape[0], q.shape[1]
    scale = HEAD_DIM_A ** -0.5
    outs = []
    for qb in range(seq // Q_BLOCK):
        q0 = qb * Q_BLOCK
        kv_end = q0 + Q_BLOCK
        q_blk = q[:, q0:kv_end]
        k_blk = k[:, :kv_end]
        v_blk = v[:, :kv_end]
        qpos = jnp.arange(q0, kv_end, dtype=jnp.int32)
        kpos = jnp.arange(kv_end, dtype=jnp.int32)
        bias = rel_bias.astype(jnp.float32)[t5_bucket(kpos[None, :] - qpos[:, None])]
        bias = jnp.transpose(bias, (2, 0, 1))
        allowed = (kpos[None, :] // CHUNK) <= (qpos[:, None] // CHUNK)
        logits = jnp.einsum('bqhmd,bkhmd->bhmqk', q_blk, k_blk).astype(jnp.float32) * scale
        logits = logits + bias[None, :, None]
        logits = jnp.where(allowed, logits, -jnp.inf)
        p = jax.nn.softmax(logits, axis=-1)
        attn = p[:, :, 0] - lam * p[:, :, 1]
        outs.append(jnp.einsum('bhqk,bkhd->bqhd', attn.astype(v.dtype), v_blk))
    o = jnp.concatenate(outs, axis=1)
    of = o.astype(jnp.float32)
    of = of * lax.rsqrt(jnp.mean(jnp.square(of), axis=-1, keepdims=True) + RMS_EPS)
    of = of * subln_g.astype(jnp.float32) * (1.0 - lam_init)
    return of.astype(o.dtype).reshape(bsz, seq, N_HEADS_A * 2 * HEAD_DIM_A)


def mixer_block(u, w_in, b_gate, lambda_q, lambda_k, subln_g, rel_bias, conv_w,
                w_a_proj, w_b_proj, w_o, lam_init):
    bsz, seq, _ = u.shape
    proj = u @ w_in
    q, k, v, cb, cc, ch, ga, gb = jnp.split(proj, SPLITS, axis=-1)
    q = q.reshape(bsz, seq, N_HEADS_A, 2, HEAD_DIM_A)
    k = k.reshape(bsz, seq, N_HEADS_A, 2, HEAD_DIM_A)
    v = v.reshape(bsz, seq, N_HEADS_A, 2 * HEAD_DIM_A)
    lq = lambda_q.astype(jnp.float32)
    lk = lambda_k.astype(jnp.float32)
    lam = jnp.exp(jnp.sum(lq[0] * lk[0])) - jnp.exp(jnp.sum(lq[1] * lk[1])) + lam_init
    y_a = diff_attention(q, k, v, rel_bias, lam, lam_init, subln_g) @ w_a_proj
    z = cc * ch
    z = lax.conv_general_dilated(z, conv_w, window_strides=(1,),
                                 padding=[(CONV_WIDTH - 1, 0)],
                                 dimension_numbers=('NWC', 'WIO', 'NWC'),
                                 feature_group_count=D_CONV)
    y_b = (cb * z) @ w_b_proj
    g_a = jax.nn.sigmoid(ga + b_gate[0])
    g_b = jax.nn.sigmoid(gb + b_gate[1])
    return (g_a * y_a + g_b * y_b) @ w_o


def hier_moe(h, w_group, b_group, w_sub, b_sub, w_gate_e, w_up_e, w_down_e):
    bsz, seq, d = h.shape
    n_tok = bsz * seq
    xt = h.reshape(n_tok, d)
    xf = xt.astype(jnp.float32)
    g_prob = jax.nn.softmax(xf @ w_group.astype(jnp.float32) + b_group.astype(jnp.float32), axis=-1)
    g_p, g_idx = lax.top_k(g_prob, 1)
    sub_logits = jnp.einsum('td,gde->tge', xf, w_sub.astype(jnp.float32)) + b_sub.astype(jnp.float32)
    sel = jnp.take_along_axis(sub_logits, g_idx[:, :, None], axis=1)[:, 0]
    s_top, s_idx = lax.top_k(sel, TOP_K)
    weights = g_p * jax.nn.softmax(s_top, axis=-1)
    expert = g_idx * EXPERTS_PER_GROUP + s_idx
    n_assign = n_tok * TOP_K
    e_flat = expert.reshape(n_assign)
    tok_flat = jnp.repeat(jnp.arange(n_tok, dtype=jnp.int32), TOP_K)
    w_flat = weights.reshape(n_assign)
    order = jnp.argsort(e_flat)
    e_s, tok_s, w_s = e_flat[order], tok_flat[order], w_flat[order]
    counts = jnp.bincount(e_flat, length=N_EXPERTS)
    padded = ((counts + MOE_BLOCK - 1) // MOE_BLOCK) * MOE_BLOCK
    pad_end = jnp.cumsum(padded)
    pad_start = pad_end - padded
    cnt_start = jnp.cumsum(counts) - counts
    dest = pad_start[e_s] + (jnp.arange(n_assign, dtype=jnp.int32) - cnt_start[e_s])
    n_blocks = -(-n_assign // MOE_BLOCK) + N_EXPERTS
    n_rows = n_blocks * MOE_BLOCK
    tok_pad = jnp.full((n_rows,), n_tok, dtype=jnp.int32).at[dest].set(tok_s)
    w_pad = jnp.zeros((n_rows,), jnp.float32).at[dest].set(w_s)
    block_expert = jnp.minimum(
        jnp.searchsorted(pad_end, jnp.arange(n_blocks, dtype=jnp.int32) * MOE_BLOCK, side='right'),
        N_EXPERTS - 1).astype(jnp.int32)
    x_pad = jnp.concatenate([xt, jnp.zeros((1, d), xt.dtype)], axis=0)
    xb = x_pad[tok_pad].reshape(n_blocks, MOE_BLOCK, d)

    def expert_block(args):
        x_blk, e = args
        return (jax.nn.silu(x_blk @ w_gate_e[e]) * (x_blk @ w_up_e[e])) @ w_down_e[e]

    yb = lax.map(expert_block, (xb, block_expert))
    y = yb.reshape(n_rows, d).astype(jnp.float32) * w_pad[:, None]
    out = jax.ops.segment_sum(y, tok_pad, num_segments=n_tok + 1)[:n_tok]
    return out.astype(h.dtype).reshape(bsz, seq, d)


def setup_inputs(seed: int = 0) -> dict:
    key = jax.random.key(seed)
    ks = jax.random.split(key, 32)
    f32 = jnp.float32
    D = D_MODEL
    beta = DEEPNORM_BETA

    def nrm(k, shape, scale):
        return jax.random.normal(k, shape, f32) * scale

    w_q = nrm(ks[1], (DEPTH, D, D_QK_A), D ** -0.5)
    w_k = nrm(ks[2], (DEPTH, D, D_QK_A), D ** -0.5)
    w_v = nrm(ks[3], (DEPTH, D, D_V_A), beta * D ** -0.5)
    w_rest = nrm(ks[4], (DEPTH, D, 3 * D_CONV + 2 * D_MODEL), D ** -0.5)
    w_in = jnp.concatenate([w_q, w_k, w_v, w_rest], axis=-1)
    return {
        'x': jax.random.normal(ks[0], (BATCH, SEQ, D), f32),
        'ln_in_g': 1.0 + nrm(ks[5], (D,), 0.02),
        'ln_in_b': nrm(ks[6], (D,), 0.02),
        'w_in': w_in,
        'b_gate': nrm(ks[7], (DEPTH, 2, D), 0.02),
        'lambda_q': nrm(ks[8], (DEPTH, 2, HEAD_DIM_A), 0.1),
        'lambda_k': nrm(ks[9], (DEPTH, 2, HEAD_DIM_A), 0.1),
        'subln_g': 1.0 + nrm(ks[10], (DEPTH, 2 * HEAD_DIM_A), 0.02),
        'rel_bias': nrm(ks[11], (N_BUCKETS, N_HEADS_A), 0.5),
        'conv_w': nrm(ks[12], (DEPTH, CONV_WIDTH, 1, D_CONV), CONV_WIDTH ** -0.5),
        'w_a_proj': nrm(ks[13], (DEPTH, D_V_A, D), beta * D_V_A ** -0.5),
        'w_b_proj': nrm(ks[14], (DEPTH, D_CONV, D), beta * D_CONV ** -0.5),
        'w_o': nrm(ks[15], (DEPTH, D, D), beta * D ** -0.5),
        'ln1_g': 1.0 + nrm(ks[16], (DEPTH, D), 0.02),
        'ln1_b': nrm(ks[17], (DEPTH, D), 0.02),
        'w_group': nrm(ks[18], (DEPTH, D, N_GROUPS), D ** -0.5),
        'b_group': nrm(ks[19], (DEPTH, N_GROUPS), 0.01),
        'w_sub': nrm(ks[20], (DEPTH, N_GROUPS, D, EXPERTS_PER_GROUP), D ** -0.5),
        'b_sub': nrm(ks[21], (DEPTH, N_GROUPS, EXPERTS_PER_GROUP), 0.01),
        'w_gate_e': nrm(ks[22], (DEPTH, N_EXPERTS, D, D_EXPERT), beta * D ** -0.5),
        'w_up_e': nrm(ks[23], (DEPTH, N_EXPERTS, D, D_EXPERT), beta * D ** -0.5),
        'w_down_e': nrm(ks[24], (DEPTH, N_EXPERTS, D_EXPERT, D), beta * D_EXPERT ** -0.5),
        'ln2_g': 1.0 + nrm(ks[25], (DEPTH, D), 0.02),
        'ln2_b': nrm(ks[26], (DEPTH, D), 0.02),
    }


def reference(x, ln_in_g, ln_in_b, w_in, b_gate, lambda_q, lambda_k, subln_g, rel_bias,
              conv_w, w_a_proj, w_b_proj, w_o, ln1_g, ln1_b, w_group, b_group, w_sub, b_sub,
              w_gate_e, w_up_e, w_down_e, ln2_g, ln2_b):
    x = layer_norm(x, ln_in_g, ln_in_b)
    for l in range(DEPTH):
        lam_init = 0.8 - 0.6 * math.exp(-0.3 * l)
        mix = mixer_block(x, w_in[l], b_gate[l], lambda_q[l], lambda_k[l], subln_g[l], rel_bias,
                          conv_w[l], w_a_proj[l], w_b_proj[l], w_o[l], lam_init)
        x = layer_norm(DEEPNORM_ALPHA * x + mix, ln1_g[l], ln1_b[l])
        ffn = hier_moe(x, w_group[l], b_group[l], w_sub[l], b_sub[l],
                       w_gate_e[l], w_up_e[l], w_down_e[l])
        x = layer_norm(DEEPNORM_ALPHA * x + ffn, ln2_g[l], ln2_b[l])
    return x
```

```python
import contextlib
import math
import numpy as np
import concourse.bass as bass
import concourse.mybir as mybir
from concourse.bass_utils import run_bass_kernel_spmd

F32 = mybir.dt.float32
BF16 = mybir.dt.bfloat16
I32 = mybir.dt.int32
AF = mybir.ActivationFunctionType
ALU = mybir.AluOpType
AX = mybir.AxisListType

SAME_ENGINE_SYNC = True
NCORES = 8
D = 1024
SEQ = 2048
NSEQ = 2
NTOK = NSEQ * SEQ
NT = NTOK // 128
H = 4
LN_EPS = 1e-5
RMS_EPS = 1e-6
ALPHA = 2.0 ** 0.25
LAM_INIT = 0.2
SCALE = 128 ** -0.5
NE = 32
CAP = 384
NCT = CAP // 128
NSLOT = NE * CAP
BIGIDX = 1.0e6


class Res:
    __slots__ = ("w", "w0", "r", "excl")

    def __init__(self, excl=False):
        self.w = {}
        self.w0 = {}
        self.r = {}
        self.excl = excl


class Sched:
    ENG = ("pe", "dve", "act", "pool", "sp")

    def __init__(self, nc, stack, n_dma_sems=40):
        self.nc = nc
        self.q = {e: [] for e in self.ENG}
        self.sems = {}
        self.cnt = {}
        for e in self.ENG:
            self.sems[e] = stack.enter_context(nc.semaphore("s_" + e))
            self.cnt[e] = 0
        self.dma_keys = []
        for i in range(n_dma_sems):
            k = "d%d" % i
            self.sems[k] = stack.enter_context(nc.semaphore("s_" + k))
            self.cnt[k] = 0
            self.dma_keys.append(k)
        self.dma_rr = 0
        self.dma_rr_sw = 0
        self.waited = {}
        self.nops = 0
        self.deferred = []
        self.pumping = False

    def _wait(self, e, key, val):
        if key == e and not SAME_ENGINE_SYNC:
            return
        if self.waited.get((e, key), 0) >= val:
            return
        self.waited[(e, key)] = val
        sem = self.sems[key]
        self.q[e].append(lambda eng, sem=sem, val=val: eng.wait_ge(sem, val))

    def _deps(self, e, reads, writes, cowrite):
        for r in reads:
            for k, v in r.w.items():
                self._wait(e, k, v)
        for w in writes:
            src = w.w0 if cowrite else w.w
            for k, v in src.items():
                self._wait(e, k, v)
            for k, v in w.r.items():
                self._wait(e, k, v)

    def _record(self, key, val, reads, writes, cowrite):
        for r in reads:
            if r.r.get(key, 0) < val:
                r.r[key] = val
        for w in writes:
            if cowrite:
                w.w[key] = max(w.w.get(key, 0), val)
            else:
                w.w = {key: val}
                w.w0 = {key: val}
                w.r = {}

    @staticmethod
    def _split(reads, writes, cowrite):
        ex = [r for r in reads if r.excl]
        if ex:
            assert not cowrite
            reads = [r for r in reads if not r.excl]
            writes = list(writes) + ex
        return reads, writes

    def ops(self, e, fns, reads=(), writes=(), cowrite=False):
        reads, writes = self._split(reads, writes, cowrite)
        self._deps(e, reads, writes, cowrite)
        self.cnt[e] += 1
        val = self.cnt[e]
        sem = self.sems[e]
        for fn in fns[:-1]:
            self.q[e].append(lambda eng, fn=fn: fn(eng))
        fn = fns[-1]
        self.q[e].append(lambda eng, fn=fn, sem=sem: fn(eng).then_inc(sem, 1))
        self._record(e, val, reads, writes, cowrite)
        self.nops += len(fns)
        self._autopump()

    def op(self, e, fn, reads=(), writes=(), cowrite=False):
        self.ops(e, [fn], reads, writes, cowrite)

    def dma(self, e, fn, reads=(), writes=(), cowrite=False):
        half = len(self.dma_keys) // 2
        if e == "pool":
            k = self.dma_keys[half + self.dma_rr_sw % half]
            self.dma_rr_sw += 1
        else:
            k = self.dma_keys[self.dma_rr % half]
            self.dma_rr += 1
        if self.cnt[k] > 0:
            self._wait(e, k, self.cnt[k])
        reads, writes = self._split(reads, writes, cowrite)
        self._deps(e, reads, writes, cowrite)
        self.cnt[k] += 16
        val = self.cnt[k]
        sem = self.sems[k]
        self.q[e].append(lambda eng, fn=fn, sem=sem: fn(eng).then_inc(sem, 16))
        self._record(k, val, reads, writes, cowrite)
        self.nops += 1
        self._autopump()

    def _autopump(self):
        if self.deferred and not self.pumping:
            self.pumping = True
            self.pump(2)
            self.pumping = False

    def defer(self, thunks):
        self.deferred.extend(thunks)

    def pump(self, n=1):
        while n > 0 and self.deferred:
            self.deferred.pop(0)()
            n -= 1

    def flush(self):
        self.pump(1 << 30)

    def barrier(self):
        for e in self.ENG:
            for k, v in self.cnt.items():
                if v > 0 and k != e:
                    self._wait(e, k, v)

    def finish(self, e="sp"):
        self.flush()
        for k in self.dma_keys:
            if self.cnt[k] > 0:
                self._wait(e, k, self.cnt[k])

    def emit(self):
        with self.nc.Block() as block:
            @block.tensor
            def _(eng):
                for f in self.q["pe"]:
                    f(eng)

            @block.vector
            def _(eng):
                for f in self.q["dve"]:
                    f(eng)

            @block.scalar
            def _(eng):
                for f in self.q["act"]:
                    f(eng)

            @block.gpsimd
            def _(eng):
                for f in self.q["pool"]:
                    f(eng)

            @block.sync
            def _(eng):
                for f in self.q["sp"]:
                    f(eng)


def _t5_bucket(rel):
    nb = 16
    max_exact = 8
    bucket = np.where(rel > 0, nb, 0)
    n = np.abs(rel)
    nf = np.maximum(n, 1).astype(np.float32)
    large = max_exact + (np.log(nf / max_exact) / math.log(128 / max_exact) * (nb - max_exact)).astype(np.int32)
    large = np.minimum(large, nb - 1)
    return bucket + np.where(n < max_exact, n, large)


def _static_tables():
    k = np.arange(128)[:, None]
    q = np.arange(128)[None, :]
    allowed = (k // 64) <= (q // 64)
    bd = _t5_bucket(k - q)
    bo = _t5_bucket(k - q - 128)
    entries = []
    ohs = []
    for b in sorted(set(bd[allowed].tolist())):
        entries.append((0, b))
        ohs.append(((bd == b) & allowed).astype(np.float32))
    for b in sorted(set(bo.flatten().tolist())):
        entries.append((1, b))
        ohs.append((bo == b).astype(np.float32))
    oh = np.stack(ohs, axis=1)
    negm = np.where(allowed, 0.0, -8192.0).astype(np.float32)
    return entries, np.ascontiguousarray(oh.reshape(128, -1)), negm


_OH_ENTRIES, _OH_NP, _NEGM_NP = _static_tables()
NOH = len(_OH_ENTRIES)

PR_G1, PR_B1, PR_SUBG, PR_G2, PR_B2, PR_BR = 0, 1024, 2048, 2304, 3328, 4352
PR_N = 4352 + 36
PS_LQ, PS_LK, PS_RB, PS_N = 0, 256, 512, 640
PC_GIN, PC_BIN, PC_BGA, PC_BGB, PC_CW = 0, 8, 16, 24, 32
PC_N = 56


import os as _os
_DBG = {k: True for k in _os.environ.get("MK_DBG", "").split(",") if k}


def build_program(stage=2, stop=9):
    nc = bass.Bass("TRN2", target_bir_lowering=False)

    def din(name, shape, dt=F32):
        return nc.dram_tensor(name, shape, dt, kind="ExternalInput").ap()

    x_d = din("x", [NTOK, D])
    wqkv_d = din("wqkv", [H, 128, 3 * 8 * 256])
    wr_d = din("wr", [8, 128, 5 * 8 * 128])
    wa_d = din("wa", [8, 128, 8 * 128])
    wb_d = din("wb", [8, 128, 8 * 128])
    wo_d = din("wo", [128, 8 * 1024])
    pcols_d = din("pcols", [128, PC_N])
    prow_d = din("prow", [128, PR_N])
    prows_d = din("prows", [128, PS_N])
    oh_d = din("oh", [128, NOH * 128])
    negm_d = din("negm", [128, 128])
    ident_d = din("ident", [128, 128])
    if stage == 2:
        srow_d = din("srow", [128, 32])
        tball_d = din("tball0", [128, NT * 8])
        prow2_d = din("prow2", [128, 2048])
        ustr_d = din("ustr", [128, 128])
        wrt_d = din("wrt", [128, 8 * 36])
        wg_d = din("wg", [NE, 128, 8 * 512])
        wu_d = din("wu", [NE, 128, 8 * 512])
        wd_d = din("wd", [NE, 128, 4 * 1024])
    out_d = nc.dram_tensor("out", [NTOK, D], F32, kind="ExternalOutput").ap()
    x1f_d = nc.dram_tensor("x1f", [NTOK, D], F32, kind="Internal").ap()
    x1b_d = nc.dram_tensor("x1b", [NTOK + 128, D], BF16, kind="Internal").ap()
    tbl_d = nc.dram_tensor("tbl", [NSLOT, 4], F32, kind="Internal").ap()
    y2_d = nc.dram_tensor("y2", [2 * NTOK, D], BF16, kind="Internal").ap()
    if stage == 2:
        wgb_d = nc.dram_tensor("wgb", [NE, 128, 8 * 512], BF16, kind="Internal").ap()
        wub_d = nc.dram_tensor("wub", [NE, 128, 8 * 512], BF16, kind="Internal").ap()
        wdb_d = nc.dram_tensor("wdb", [NE, 128, 4 * 1024], BF16, kind="Internal").ap()

    with contextlib.ExitStack() as st:
        S = Sched(nc, st)

        def sb(name, shape, dt):
            return st.enter_context(nc.sbuf_tensor(name, shape, dt))

        identf = sb("identf", [128, 128], F32)
        identb = sb("identb", [128, 128], BF16)
        pcols = sb("pcols_s", [128, PC_N], F32)
        prow = sb("prow_s", [128, PR_N], F32)
        bcat = sb("bcat", [128, H, 2, 256], BF16)
        lamc = sb("lamc", [128, 4], F32)
        subg = sb("subg", [128, 256], F32)
        epsc = sb("epsc", [128, 2], F32)
        rstd_in = sb("rstd_in", [128, NT], F32)
        nmr_in = sb("nmr_in", [128, NT], F32)
        wo_s = sb("wo_s", [128, 8, 1024], BF16)
        uT = sb("uT", [128, 8, SEQ], BF16)
        oT = sb("oT", [128, 8, SEQ], BF16)
        halo = sb("halo", [128, 8, 2], F32)
        xs = [sb("xs%d" % i, [128, 1024], F32) for i in range(2)]
        xh = [sb("xh0", [128, 1024], BF16)] * 2
        stt = sb("stt", [128, 12], F32)
        sml = sb("sml", [128, 16], F32)
        wk = [sb("wk%d" % i, [128, 1024], F32) for i in range(3)]
        r_identf, r_identb, r_pcols, r_prow, r_bcat, r_lamc, r_subg, r_eps = [Res() for _ in range(8)]
        r_mv, r_g1, r_wo, r_halo, r_stt, r_sml = [Res() for _ in range(6)]
        r_mvs = [Res() for _ in range(NT)]
        r_xhB = Res()
        r_uT = [Res() for _ in range(32)]
        r_oT = [Res() for _ in range(16)]
        r_xs = [Res(), Res()]
        r_xh = [Res()] * 2
        r_wk = [Res() for _ in range(3)]
        ARENA = 75792
        arena = sb("arena", [128, ARENA], mybir.dt.uint8)

        NPG = 3
        pg = [st.enter_context(nc.psum_tensor("pg%d" % i, [128, 512], F32)) for i in range(NPG)]
        pv = [st.enter_context(nc.psum_tensor("pv%d" % i, [128, 512], F32)) for i in range(3)]
        pt = [st.enter_context(nc.psum_tensor("pt%d" % i, [128, 1024], BF16)) for i in range(2)]
        r_pg = [Res(True) for _ in range(NPG)]
        r_pv = [Res(True) for _ in range(3)]
        r_pt = [Res(True), Res(True)]
        cnt = {"pg": 0, "pv": 0, "pt": 0, "ev": 0}

        def next_pg():
            if cnt.get("wide", False) == "moe" or (cnt.get("wide", False) and _DBG.get("wide" + str(cnt["wide"]))):
                i = cnt["pg"] % (NPG + 3)
                cnt["pg"] += 1
                return (pg + pv)[i], (r_pg + r_pv)[i]
            i = cnt["pg"] % NPG
            cnt["pg"] += 1
            return pg[i], r_pg[i]

        def next_pv():
            i = cnt["pv"] % 3
            cnt["pv"] += 1
            return pv[i], r_pv[i]

        def next_pt():
            i = cnt["pt"] % 2
            cnt["pt"] += 1
            return pt[i][:, 0:512], r_pt[i]

        def mm(out_ap, pairs, reads, writes, first=True, last=True):
            n = len(pairs)
            fns = []
            for i, (a, b) in enumerate(pairs):
                fns.append(lambda e, a=a, b=b, i=i: e.matmul(out_ap, lhsT=a, rhs=b, start=(first and i == 0), stop=(last and i == n - 1)))
            S.ops("pe", fns, reads=reads, writes=writes)

        def evac(out_ap, in_ap, reads, writes, eng=None):
            if eng is None:
                eng = "act" if cnt["ev"] % 2 == 0 else "dve"
                cnt["ev"] += 1
            if eng == "act":
                S.op("act", lambda e: e.activation(out=out_ap, in_=in_ap, func=AF.Copy), reads=reads, writes=writes)
            else:
                S.op("dve", lambda e: e.tensor_copy(out=out_ap, in_=in_ap), reads=reads, writes=writes)

        S.dma("sp", lambda e: e.dma_start(out=identf[:], in_=ident_d), writes=[r_identf])
        S.dma("sp", lambda e: e.dma_start(out=pcols[:], in_=pcols_d), writes=[r_pcols])
        S.dma("sp", lambda e: e.dma_start(out=prow[:], in_=prow_d), writes=[r_prow])
        S.op("dve", lambda e: e.tensor_copy(out=identb[:], in_=identf[:]), reads=[r_identf], writes=[r_identb])
        S.op("dve", lambda e: e.memset(epsc[:, 0:1], LN_EPS), writes=[r_eps])
        S.op("dve", lambda e: e.memset(epsc[:, 1:2], RMS_EPS), writes=[r_eps])
        S.op("dve", lambda e: e.memset(halo[:], 0.0), writes=[r_halo])
        PS_OFF = NOH * 128 * 4 + 512
        prows = arena[:, PS_OFF:PS_OFF + PS_N * 4].bitcast(F32)
        r_prows = Res()
        S.dma("sp", lambda e: e.dma_start(out=prows, in_=prows_d), writes=[r_prows])
        S.op("dve", lambda e: e.tensor_tensor(out=wk[0][:, 0:256], in0=prows[:, PS_LQ:PS_LQ + 256], in1=prows[:, PS_LK:PS_LK + 256], op=ALU.mult),
             reads=[r_prows], writes=[r_wk[0]])
        S.op("dve", lambda e: e.tensor_reduce(out=sml[:, 0:2], in_=wk[0][:, 0:256].rearrange("p (a b) -> p a b", a=2), axis=AX.X, op=ALU.add),
             reads=[r_wk[0]], writes=[r_sml])
        S.op("act", lambda e: e.activation(out=sml[:, 2:4], in_=sml[:, 0:2], func=AF.Exp), reads=[r_sml], writes=[r_sml])
        S.op("dve", lambda e: e.tensor_tensor(out=sml[:, 4:5], in0=sml[:, 2:3], in1=sml[:, 3:4], op=ALU.subtract), reads=[r_sml], writes=[r_sml])
        S.op("dve", lambda e: e.tensor_scalar(out=lamc[:, 0:1], in0=sml[:, 4:5], scalar1=LAM_INIT, scalar2=None, op0=ALU.add), reads=[r_sml], writes=[r_lamc])
        S.op("dve", lambda e: e.tensor_scalar(out=lamc[:, 1:2], in0=lamc[:, 0:1], scalar1=-1.0, scalar2=None, op0=ALU.mult), reads=[r_lamc], writes=[r_lamc])
        S.op("dve", lambda e: e.tensor_scalar(out=subg[:], in0=prow[:, PR_SUBG:PR_SUBG + 256], scalar1=1.0 - LAM_INIT, scalar2=None, op0=ALU.mult),
             reads=[r_prow], writes=[r_subg])
        oh_s = arena[:, 0:NOH * 128 * 4].bitcast(F32).rearrange("p (n q) -> p n q", q=128)
        negm_s = arena[:, NOH * 128 * 4:NOH * 128 * 4 + 512].bitcast(F32)
        r_oh, r_negm = Res(), Res()
        S.dma("sp", lambda e: e.dma_start(out=oh_s, in_=oh_d.rearrange("p (n q) -> p n q", q=128)), writes=[r_oh])
        S.dma("sp", lambda e: e.dma_start(out=negm_s, in_=negm_d), writes=[r_negm])
        rbd = arena[:, 40960:40960 + 32 * H * 4].bitcast(F32).rearrange("p (b h) -> p b h", h=H)
        r_rbd = Res()
        rbv = prows[:, PS_RB:PS_RB + 128].rearrange("p (b h) -> p b h", h=H)
        for h in range(H):
            S.op("dve", lambda e, h=h: e.tensor_scalar(out=rbd[:, :, h], in0=rbv[:, :, h], scalar1=rbv[:, 15, h:h + 1], scalar2=1.0 / SCALE,
                                                        op0=ALU.subtract, op1=ALU.mult), reads=[r_prows], writes=[r_rbd])
        acc = wk[1]
        accp = arena[:, 32768:32768 + 640 * 4].bitcast(F32)
        r_accp = Res()
        for h in range(H):
            on_pool = True
            A, r_A, eng = (accp, r_accp, "pool") if on_pool else (acc, r_wk[1], "dve")
            for typ in range(2):
                ents = [(i, b) for i, (t, b) in enumerate(_OH_ENTRIES) if t == typ]
                av = A[:, typ * 128:(typ + 1) * 128]
                tv = A[:, 512:640]
                if typ == 0:
                    S.op(eng, lambda e, av=av: e.tensor_copy(out=av, in_=negm_s), reads=[r_negm], writes=[r_A])
                else:
                    S.op(eng, lambda e, av=av: e.memset(av, 0.0), writes=[r_A])
                for (i, b) in ents:
                    if on_pool:
                        S.op("pool", lambda e, tv=tv, i=i, b=b, h=h: e.tensor_scalar(out=tv, in0=oh_s[:, i, :], scalar1=rbd[:, b, h:h + 1], scalar2=0.0, op0=ALU.mult, op1=ALU.add),
                             reads=[r_oh, r_rbd, r_A], writes=[r_A])
                        S.op("pool", lambda e, av=av, tv=tv: e.tensor_tensor(out=av, in0=av, in1=tv, op=ALU.add), reads=[r_A], writes=[r_A])
                    else:
                        S.op("dve", lambda e, av=av, i=i, b=b, h=h: e.scalar_tensor_tensor(out=av, in0=oh_s[:, i, :], scalar=rbd[:, b, h:h + 1], in1=av,
                                                                                            op0=ALU.mult, op1=ALU.add),
                             reads=[r_oh, r_rbd, r_A], writes=[r_A])
            S.op(eng, lambda e, h=h, A=A: e.tensor_copy(out=bcat[:, h, 0, :], in_=A[:, 0:256]), reads=[r_A], writes=[r_bcat], cowrite=True)
            S.op(eng, lambda e, h=h, A=A: e.tensor_tensor(out=A[:, 256:512], in0=A[:, 0:256], in1=bcat[:, h, 0, :], op=ALU.subtract),
                 reads=[r_A, r_bcat], writes=[r_A])
            S.op(eng, lambda e, h=h, A=A: e.tensor_copy(out=bcat[:, h, 1, :], in_=A[:, 256:512]), reads=[r_A], writes=[r_bcat], cowrite=True)
        if stage == 2:
            rt = sb("rt", [128, 256], F32)
            Mall = sb("Mall", [128, NT, 32], BF16)
            ustr_b = sb("ustr_b", [128, 128], BF16)
            ones_b = sb("ones_b", [128, 128], BF16)
            Lbuf = sb("Lbuf", [128, 8, 36], F32)
            r_L = [Res() for _ in range(8)]
            tball = sb("tball", [128, NT, 2, 4], F32)
            destall = sb("destall", [128, NT, 2], I32)
            srow = sb("srow_s", [128, 32], F32)
            wrt_s = sb("wrt_s", [128, 8, 36], F32)
            r_rt, r_Mall, r_ustr, r_tbk, r_desti, r_srow, r_wrt, r_tbl, r_x1b, r_y2, r_x1f = [Res() for _ in range(11)]
            S.dma("sp", lambda e: e.dma_start(out=srow[:], in_=srow_d), writes=[r_srow])
            S.dma("sp", lambda e: e.dma_start(out=wrt_s[:], in_=wrt_d.rearrange("p (k n) -> p k n", k=8)), writes=[r_wrt])
            S.dma("pool", lambda e: e.dma_start(out=ustr_b[:], in_=ustr_d), writes=[r_ustr])
            S.op("pool", lambda e: e.memset(ones_b[:], 1.0), writes=[r_ustr])
            S.dma("sp", lambda e: e.dma_start(out=tball[:], in_=tball_d.rearrange("p (t k f) -> p t k f", k=2, f=4)), writes=[r_tbk])
            def init_scratch():
                S.op("dve", lambda e: e.memset(wk[2][:], 0.0), writes=[r_wk[2]])
                zv = wk[2][:, :].bitcast(BF16)
                y2v = y2_d.rearrange("(p c) d -> p (c d)", p=128)
                for i in range(32):
                    S.dma("sp", lambda e, i=i: e.dma_start(out=y2v[:, i * 2048:(i + 1) * 2048], in_=zv), reads=[r_wk[2]], writes=[r_y2], cowrite=True)
                S.dma("sp", lambda e: e.dma_start(out=x1b_d[NTOK:NTOK + 128, :], in_=zv[:, 0:1024]), reads=[r_wk[2]], writes=[r_x1b], cowrite=True)
                tinit = wk[0][:, 0:(NSLOT // 128) * 4].rearrange("p (c f) -> p c f", f=4)
                S.op("dve", lambda e: e.memset(tinit[:, :, 0:1], float(NTOK)), writes=[r_wk[0]])
                S.op("dve", lambda e: e.memset(tinit[:, :, 1:2], 0.0), writes=[r_wk[0]])
                S.op("dve", lambda e: e.memset(tinit[:, :, 2:3], BIGIDX), writes=[r_wk[0]])
                S.op("dve", lambda e: e.memset(tinit[:, :, 3:4], 0.0), writes=[r_wk[0]])
                S.dma("sp", lambda e: e.dma_start(out=tbl_d.rearrange("(p c) f -> p c f", p=128), in_=tinit), reads=[r_wk[0]], writes=[r_tbl])
        S.dma("pool", lambda e: e.dma_start(out=wo_s[:], in_=wo_d.rearrange("p (k c) -> p k c", k=8), max_dma_last_dim=4096), writes=[r_wo])

        def carve(off, shape, dt):
            esz = 2 if dt == BF16 else 4
            n = int(np.prod(shape))
            v = arena[:, off:off + n * esz].bitcast(dt)
            if len(shape) == 2:
                v = v.rearrange("p (a b) -> p a b", a=shape[0])
            elif len(shape) == 3:
                v = v.rearrange("p (a b c) -> p a b c", a=shape[0], b=shape[1])
            return v, off + n * esz

        gin_row = prow[:, PR_G2:PR_G2 + 1024]
        bin_row = prow[:, PR_B2:PR_B2 + 1024]

        smcs = sb("smcs", [128, 16], F32)
        sml2 = sb("sml2", [128, 16], F32)
        stt2 = sb("stt2", [128, 12], F32)
        r_sml2, r_stt2 = Res(), Res()
        LNS = [(sml, r_sml, stt, r_stt), (sml2, r_sml2, stt2, r_stt2)]

        def ln_stats(src, r_src, si=0):
            sm, r_sm, sx, r_sx = LNS[si]
            S.ops("dve", [lambda e: e.bn_stats(out=sx[:, 0:6], in_=src[:, 0:512]),
                          lambda e: e.bn_stats(out=sx[:, 6:12], in_=src[:, 512:1024])], reads=(r_src if isinstance(r_src, list) else [r_src]), writes=[r_sx])
            S.op("dve", lambda e: e.bn_aggr(out=sm[:, 6:8], in_=sx[:, 0:12]), reads=[r_sx], writes=[r_sm])
            S.op("act", lambda e: e.activation(out=sm[:, 8:9], in_=sm[:, 7:8], func=AF.Ln, bias=epsc[:, 0:1], scale=1.0),
                 reads=[r_sm, r_eps], writes=[r_sm])
            S.op("act", lambda e: e.activation(out=sm[:, 8:9], in_=sm[:, 8:9], func=AF.Exp, scale=-0.5), reads=[r_sm], writes=[r_sm])
            S.op("dve", lambda e: e.tensor_scalar(out=sm[:, 9:10], in0=sm[:, 6:7], scalar1=sm[:, 8:9], scalar2=-1.0, op0=ALU.mult, op1=ALU.mult),
                 reads=[r_sm], writes=[r_sm])
            return sm, r_sm

        r_cw = [Res() for _ in range(NE)]
        conv_next = [0]

        def emit_conv(n=1):
            if stage != 2:
                return
            for _ in range(n):
                ex = conv_next[0]
                if ex >= NE:
                    return
                conv_next[0] += 1
                for (dst, src) in ((wgb_d, wg_d), (wub_d, wu_d), (wdb_d, wd_d)):
                    S.dma("pool", lambda e, dst=dst, src=src, ex=ex: e.dma_start(out=dst[ex], in_=src[ex], max_dma_last_dim=8192), writes=[r_cw[ex]], cowrite=True)

        pregs = {}

        def preg(e, val):
            if val not in pregs:
                pregs[val] = e.to_reg(val)
            return pregs[val]

        def carve_from(buf, off, shape, dt):
            esz = 2 if dt == BF16 else 4
            n = int(np.prod(shape))
            v = buf[:, off:off + n * esz].bitcast(dt)
            if len(shape) == 2:
                v = v.rearrange("p (a b) -> p a b", a=shape[0])
            return v, off + n * esz

        def moe_phase():
            emit_conv(NE)
            S.flush()
            S.barrier()
            cnt["wide"] = "moe"
            S.dma("sp", lambda e: e.dma_start(out=prow[:, PR_G2:PR_G2 + 2048], in_=prow2_d), writes=[r_prow])
            ubytes = uT[:, :, :].rearrange("p a b -> p (a b)").bitcast(mybir.dt.uint8)
            obytes = oT[:, :, :].rearrange("p a b -> p (a b)").bitcast(mybir.dt.uint8)
            off = 0
            wgb, wub, wdb = [], [], []
            for i in range(2):
                a, off = carve(off, [8, 512], BF16); wgb.append(a)
                a, off = carve(off, [8, 512], BF16); wub.append(a)
                a, off = carve(off, [4, 1024], BF16); wdb.append(a)
            XgT, hT, sg, yo = [], [], [], []
            for i in range(2):
                a, off = carve(off, [8, CAP], BF16); XgT.append(a)
                a, off = carve(off, [4, CAP], BF16); hT.append(a)
                a, off = carve(off, [CAP], F32); sg.append(a)
            assert off <= ARENA, off
            uo = 0
            xg, tbe, idxt, idxa = [], [], [], []
            for i in range(2):
                a, uo = carve_from(ubytes, uo, [NCT, 1024], BF16); xg.append(a)
            NIB = 3
            for i in range(NIB):
                a, uo = carve_from(ubytes, uo, [NCT, 4], F32); tbe.append(a)
                a, uo = carve_from(ubytes, uo, [NCT], I32); idxt.append(a)
                a, uo = carve_from(ubytes, uo, [NCT], I32); idxa.append(a)
            NYO = 4
            for i in range(NYO):
                a, uo = carve_from(ubytes, uo, [1024], BF16); yo.append(a)
            assert uo <= 32768
            r_wg, r_wu, r_wd = [Res(), Res()], [Res(), Res()], [Res(), Res()]
            r_xg = [[Res() for _ in range(NCT)] for _ in range(2)]
            r_XgT, r_hT, r_sg = [[Res(), Res()] for _ in range(3)]
            r_yo = [Res() for _ in range(NYO)]
            r_tbe, r_idt, r_ida = [[Res() for _ in range(NIB)] for _ in range(3)]
            nyo = [0]

            def load_expert(ex):
                bi = ex % 2
                ib = ex % NIB
                S.dma("sp", lambda e: e.dma_start(out=tbe[ib], in_=tbl_d[ex * CAP:(ex + 1) * CAP, :].rearrange("(c p) f -> p c f", p=128)), reads=[r_tbl], writes=[r_tbe[ib]])
                S.op("dve", lambda e: e.tensor_copy(out=idxt[ib], in_=tbe[ib][:, :, 0]), reads=[r_tbe[ib]], writes=[r_idt[ib]])
                S.op("dve", lambda e: e.tensor_copy(out=idxa[ib], in_=tbe[ib][:, :, 2]), reads=[r_tbe[ib]], writes=[r_ida[ib]])
                for c in range(NCT):
                    S.dma("pool", lambda e, c=c: e.indirect_dma_start(out=xg[bi][:, c, :], out_offset=None, in_=x1b_d[:, :],
                                                                       in_offset=bass.IndirectOffsetOnAxis(ap=idxt[ib][:, c:c + 1], axis=0)),
                          reads=[r_idt[ib], r_x1b], writes=[r_xg[bi][c]])
                S.dma("sp", lambda e: e.dma_start(out=wgb[bi], in_=wgb_d[ex].rearrange("p (k f) -> p k f", k=8)), reads=[r_cw[ex]], writes=[r_wg[bi]])
                S.dma("sp", lambda e: e.dma_start(out=wub[bi], in_=wub_d[ex].rearrange("p (k f) -> p k f", k=8)), reads=[r_cw[ex]], writes=[r_wu[bi]])
                S.dma("sp", lambda e: e.dma_start(out=wdb[bi], in_=wdb_d[ex].rearrange("p (k f) -> p k f", k=4)), reads=[r_cw[ex]], writes=[r_wd[bi]])

            def compute_expert(ex):
                bi = ex % 2
                ib = ex % NIB
                for c in range(NCT):
                    for half in range(2):
                        pt_ap, r_p = next_pt()
                        S.ops("pe", [(lambda e, k=k, c=c, pt_ap=pt_ap: e.transpose(out=pt_ap[:, (k % 4) * 128:(k % 4 + 1) * 128], in_=xg[bi][:, c, k * 128:(k + 1) * 128], identity=identb[:]))
                                     for k in range(half * 4, half * 4 + 4)], reads=[r_xg[bi][c], r_identb], writes=[r_p])
                        evac(XgT[bi][:, half * 4:half * 4 + 4, c * 128:(c + 1) * 128], pt_ap[:, 0:512].rearrange("p (a b) -> p a b", a=4), reads=[r_p], writes=[r_XgT[bi]], eng="dve")
                for fc in range(4):
                    psG, r_psG = next_pg()
                    mm(psG[:, 0:CAP], [(wgb[bi][:, k, fc * 128:(fc + 1) * 128], XgT[bi][:, k, :]) for k in range(8)], reads=[r_wg[bi], r_XgT[bi]], writes=[r_psG])
                    si = fc % 2
                    S.op("act", lambda e, psG=psG, si=si: e.activation(out=sg[si], in_=psG[:, 0:CAP], func=AF.Silu), reads=[r_psG], writes=[r_sg[si]])
                    psU, r_psU = next_pg()
                    mm(psU[:, 0:CAP], [(wub[bi][:, k, fc * 128:(fc + 1) * 128], XgT[bi][:, k, :]) for k in range(8)], reads=[r_wu[bi], r_XgT[bi]], writes=[r_psU])
                    S.op("dve", lambda e, psU=psU, fc=fc, si=si: e.tensor_tensor(out=hT[bi][:, fc, :], in0=psU[:, 0:CAP], in1=sg[si], op=ALU.mult),
                         reads=[r_psU, r_sg[si]], writes=[r_hT[bi]])
                for c in range(NCT):
                    yi = nyo[0] % NYO
                    nyo[0] += 1
                    for hh in range(2):
                        ps, r_ps = next_pg()
                        mm(ps[:, :], [(hT[bi][:, fc, c * 128:(c + 1) * 128], wdb[bi][:, fc, hh * 512:(hh + 1) * 512]) for fc in range(4)], reads=[r_hT[bi], r_wd[bi]], writes=[r_ps])
                        S.op("act", lambda e, ps=ps, hh=hh, c=c, yi=yi: e.activation(out=yo[yi][:, hh * 512:(hh + 1) * 512], in_=ps[:, :], func=AF.Identity, scale=tbe[ib][:, c, 1:2]),
                             reads=[r_ps, r_tbe[ib]], writes=[r_yo[yi]])
                    S.dma("pool", lambda e, c=c, yi=yi: e.indirect_dma_start(out=y2_d[:, :], out_offset=bass.IndirectOffsetOnAxis(ap=idxa[ib][:, c:c + 1], axis=0),
                                                                             in_=yo[yi][:, :], in_offset=None, bounds_check=preg(e, 2 * NTOK - 1), oob_is_err=False),
                          reads=[r_yo[yi], r_ida[ib]], writes=[r_y2], cowrite=True)

            if _DBG.get("nopf"):
                for ex in range(NE):
                    load_expert(ex)
                    compute_expert(ex)
            else:
                load_expert(0)
                for ex in range(NE):
                    if ex + 1 < NE:
                        load_expert(ex + 1)
                    compute_expert(ex)

            S.barrier()
            NFS = 6
            FX, FY, FT, FN = [], [], [], []
            ao, oo = 0, 0
            for i in range(NFS):
                if i < 4:
                    a, ao = carve(ao, [1024], F32); FX.append(a)
                    a, ao = carve(ao, [2, 1024], BF16); FY.append(a)
                    a, ao = carve(ao, [1024], F32); FT.append(a)
                    a, ao = carve(ao, [1024], F32); FN.append(a)
                else:
                    a, oo = carve_from(obytes, oo, [1024], F32); FX.append(a)
                    a, oo = carve_from(obytes, oo, [2, 1024], BF16); FY.append(a)
                    a, oo = carve_from(obytes, oo, [1024], F32); FT.append(a)
                    a, oo = carve_from(obytes, oo, [1024], F32); FN.append(a)
            assert ao <= ARENA and oo <= 32768
            r_FX, r_FY, r_FT, r_FN = [[Res() for _ in range(NFS)] for _ in range(4)]
            def fin_load(gT):
                row0 = gT * 128
                b = gT % NFS
                S.dma("sp", lambda e: e.dma_start(out=FX[b], in_=x1f_d[row0:row0 + 128, :]), reads=[r_x1f], writes=[r_FX[b]])
                S.dma("sp", lambda e: e.dma_start(out=FY[b], in_=y2_d[2 * row0:2 * row0 + 256, :].rearrange("(p two) d -> p two d", two=2)), reads=[r_y2], writes=[r_FY[b]])

            for gT in range(min(NFS - 1, NT)):
                fin_load(gT)

            def fin_A(gT):
                b = gT % NFS
                S.op("dve", lambda e: e.scalar_tensor_tensor(out=FT[b], in0=FX[b], scalar=ALPHA, in1=FY[b][:, 0, :], op0=ALU.mult, op1=ALU.add),
                     reads=[r_FX[b], r_FY[b]], writes=[r_FT[b]])
                S.op("dve", lambda e: e.tensor_tensor(out=FT[b], in0=FT[b], in1=FY[b][:, 1, :], op=ALU.add), reads=[r_FT[b], r_FY[b]], writes=[r_FT[b]])
                return ln_stats(FT[b], r_FT[b], gT % 2)

            def fin_B(gT, sm, r_sm):
                row0 = gT * 128
                b = gT % NFS
                S.op("act", lambda e: e.activation(out=FN[b], in_=FT[b], func=AF.Identity, bias=sm[:, 9:10], scale=sm[:, 8:9]),
                     reads=[r_FT[b], r_sm], writes=[r_FN[b]])
                S.op("pool", lambda e: e.tensor_tensor(out=FN[b], in0=FN[b], in1=prow[:, PR_G2:PR_G2 + 1024], op=ALU.mult), reads=[r_FN[b], r_prow], writes=[r_FN[b]])
                S.op("pool", lambda e: e.tensor_tensor(out=FN[b], in0=FN[b], in1=prow[:, PR_B2:PR_B2 + 1024], op=ALU.add), reads=[r_FN[b], r_prow], writes=[r_FN[b]])
                S.dma("sp", lambda e: e.dma_start(out=out_d[row0:row0 + 128, :], in_=FN[b]), reads=[r_FN[b]])
                if gT + NFS - 1 < NT:
                    fin_load(gT + NFS - 1)

            pend = fin_A(0)
            for gT in range(NT):
                nxt = fin_A(gT + 1) if gT + 1 < NT else None
                fin_B(gT, *pend)
                pend = nxt

        def route_tile(gT, row0, W):
            (Wu, Ru), (Wt, Rt), (Wxb, Rxb) = W["u"], W["t"], W["xb"]
            slot = gT % 8
            S.op("act", lambda e: e.activation(out=Wxb, in_=Wu, func=AF.Copy), reads=Ru, writes=Rxb)
            S.dma("sp", lambda e: e.dma_start(out=x1f_d[row0:row0 + 128, :], in_=Wu), reads=Ru, writes=[r_x1f], cowrite=True)
            S.dma("sp", lambda e: e.dma_start(out=x1b_d[row0:row0 + 128, :], in_=Wxb), reads=Rxb, writes=[r_x1b], cowrite=True)
            x1T = Wt.rearrange("p (k t) -> p k t", k=8)
            for half in range(2):
                ps, r_ps = next_pg()
                S.ops("pe", [(lambda e, c=c, ps=ps: e.transpose(out=ps[:, (c % 4) * 128:(c % 4 + 1) * 128], in_=Wu[:, c * 128:(c + 1) * 128], identity=identf[:]))
                             for c in range(half * 4, half * 4 + 4)], reads=Ru + [r_identf], writes=[r_ps])
                evac(x1T[:, half * 4:half * 4 + 4, :], ps[:, :].rearrange("p (a b) -> p a b", a=4), reads=[r_ps], writes=Rt)
            ps, r_ps = next_pg()
            mm(ps[:, 0:36], [(x1T[:, k, :], wrt_s[:, k, :]) for k in range(8)], reads=Rt + [r_wrt], writes=[r_ps])
            R = lambda a, b: rt[:, a:b]
            S.op("dve", lambda e, ps=ps: e.tensor_tensor(out=Lbuf[:, slot, :], in0=ps[:, 0:36], in1=prow[:, PR_BR:PR_BR + 36], op=ALU.add), reads=[r_ps, r_prow], writes=[r_L[slot]])
            th = []
            ctx = {}
            th.append(lambda: S.op("dve", lambda e: e.tensor_copy(out=R(0, 36), in_=Lbuf[:, slot, :]), reads=[r_L[slot], r_rt], writes=[r_rt]))

            def dv(fn, rd=(), wr=None, cw=False):
                th.append(lambda: S.op("dve", fn, reads=[r_rt] + list(rd), writes=[r_rt] if wr is None else wr, cowrite=cw))

            def ac(fn):
                th.append(lambda: S.op("act", fn, reads=[r_rt], writes=[r_rt]))

            dv(lambda e: e.tensor_reduce(out=R(37, 38), in_=R(0, 4), axis=AX.X, op=ALU.max, negate=True))
            ac(lambda e: e.activation(out=R(40, 44), in_=R(0, 4), func=AF.Exp, bias=R(37, 38), scale=1.0, accum_out=R(38, 39)))
            dv(lambda e: e.reciprocal(out=R(39, 40), in_=R(38, 39)))
            dv(lambda e: e.tensor_scalar(out=R(44, 48), in0=R(0, 4), scalar1=R(37, 38), scalar2=1.0e30, op0=ALU.add, op1=ALU.mult))
            for g in range(4):
                dv(lambda e, g=g: e.tensor_scalar(out=R(48 + g * 8, 56 + g * 8), in0=R(4 + g * 8, 12 + g * 8), scalar1=R(44 + g, 45 + g), scalar2=None, op0=ALU.add))
            dv(lambda e: e.max(out=R(80, 88), in_=R(48, 80)))
            dv(lambda e: e.tensor_tensor(out=R(88, 89), in0=R(80, 81), in1=R(81, 82), op=ALU.subtract))
            ac(lambda e: e.activation(out=R(89, 90), in_=R(88, 89), func=AF.Exp, scale=-1.0))
            dv(lambda e: e.tensor_scalar(out=R(89, 90), in0=R(89, 90), scalar1=1.0, scalar2=None, op0=ALU.add))
            dv(lambda e: e.reciprocal(out=R(89, 90), in_=R(89, 90)))
            dv(lambda e: e.tensor_tensor(out=tball[:, gT, 0, 1:2], in0=R(89, 90), in1=R(39, 40), op=ALU.mult), wr=[r_tbk], cw=True)
            dv(lambda e: e.tensor_tensor(out=tball[:, gT, 1, 1:2], in0=R(39, 40), in1=tball[:, gT, 0, 1:2], op=ALU.subtract), rd=[r_tbk], wr=[r_tbk], cw=True)
            for k in range(2):
                dv(lambda e, k=k: e.tensor_scalar(out=R(96 + 32 * k, 128 + 32 * k), in0=R(48, 80), scalar1=R(80 + k, 81 + k), scalar2=None, op0=ALU.is_equal))
            dv(lambda e: e.tensor_tensor(out=Mall[:, gT, :], in0=R(96, 128), in1=R(128, 160), op=ALU.add), wr=[r_Mall])

            def pm():
                ps2, r_ps2 = next_pg()
                ctx["ps"], ctx["r"] = ps2, r_ps2
                mm(ps2[:, 0:32], [(ustr_b[:], Mall[:, gT, :])] + [(ones_b[:], Mall[:, g2, :]) for g2 in range(gT)], reads=[r_Mall, r_ustr], writes=[r_ps2])
            th.append(pm)
            th.append(lambda: S.op("dve", lambda e: e.tensor_scalar(out=R(160, 192), in0=ctx["ps"][:, 0:32], scalar1=float(CAP), scalar2=None, op0=ALU.is_lt),
                                   reads=[ctx["r"], r_rt], writes=[r_rt]))
            th.append(lambda: S.op("dve", lambda e: e.tensor_tensor(out=R(192, 224), in0=ctx["ps"][:, 0:32], in1=srow[:, 0:32], op=ALU.add),
                                   reads=[ctx["r"], r_rt, r_srow], writes=[r_rt]))
            dv(lambda e: e.tensor_tensor(out=R(192, 224), in0=R(192, 224), in1=R(160, 192), op=ALU.mult))
            for k in range(2):
                dv(lambda e, k=k: e.tensor_tensor(out=R(224, 256), in0=R(96 + 32 * k, 128 + 32 * k), in1=R(192, 224), op=ALU.mult))
                dv(lambda e, k=k: e.tensor_reduce(out=R(92 + k, 93 + k), in_=R(224, 256), axis=AX.X, op=ALU.add))
                dv(lambda e, k=k: e.tensor_scalar(out=destall[:, gT, k:k + 1], in0=R(92 + k, 93 + k), scalar1=BIGIDX, scalar2=None, op0=ALU.add), wr=[r_desti], cw=True)
            for k in range(2):
                th.append(lambda k=k: S.dma("pool", lambda e: e.indirect_dma_start(out=tbl_d[:, :], out_offset=bass.IndirectOffsetOnAxis(ap=destall[:, gT, k:k + 1], axis=0),
                                                                                  in_=tball[:, gT, k, :], in_offset=None, bounds_check=preg(e, NSLOT - 1), oob_is_err=False),
                                            reads=[r_tbk, r_desti], writes=[r_tbl], cowrite=True))
            S.defer(th)

        for s in range(NSEQ if stop > 0 else 0):
            base = s * SEQ
            xh2 = [xh[0][:, :], wk[2][:, 0:512].bitcast(BF16)]
            r_xh2 = [r_xh[0], r_wk[2]]

            def p1_A(T):
                gT = s * 16 + T
                b = gT % 2
                r0 = base + T * 128
                S.dma("sp", lambda e: e.dma_start(out=xs[b][:], in_=x_d[r0:r0 + 128, :]), writes=[r_xs[b]])
                sm, r_sm = ln_stats(xs[b], r_xs[b], T % 2)
                S.op("dve", lambda e: e.tensor_copy(out=rstd_in[:, gT:gT + 1], in_=sm[:, 8:9]), reads=[r_sm], writes=[r_mvs[gT]])
                S.op("dve", lambda e: e.tensor_copy(out=nmr_in[:, gT:gT + 1], in_=sm[:, 9:10]), reads=[r_sm], writes=[r_mvs[gT]])
                S.op("act", lambda e: e.activation(out=xh2[b], in_=xs[b][:], func=AF.Identity, bias=nmr_in[:, gT:gT + 1], scale=rstd_in[:, gT:gT + 1]),
                     reads=[r_xs[b], r_mvs[gT]], writes=[r_xh2[b]])

            def p1_B(T):
                gT = s * 16 + T
                b = gT % 2
                for half in range(2):
                    pt_ap, r_p = next_pt()
                    S.ops("pe", [(lambda e, c=c, pt_ap=pt_ap: e.transpose(out=pt_ap[:, (c % 4) * 128:(c % 4 + 1) * 128], in_=xh2[b][:, c * 128:(c + 1) * 128], identity=identb[:]))
                                 for c in range(half * 4, half * 4 + 4)], reads=[r_xh2[b], r_identb], writes=[r_p])
                    for c in range(half * 4, half * 4 + 4):
                        src = pt_ap[:, (c % 4) * 128:(c % 4 + 1) * 128]
                        dst = uT[:, c, T * 128:(T + 1) * 128]
                        if half == 0:
                            S.op("act", lambda e, c=c, src=src, dst=dst: e.activation(out=dst, in_=src, func=AF.Identity, bias=pcols[:, PC_BIN + c:PC_BIN + c + 1],
                                                                                       scale=pcols[:, PC_GIN + c:PC_GIN + c + 1]),
                                 reads=[r_p, r_pcols], writes=[r_uT[2 * T + half]])
                        else:
                            S.op("dve", lambda e, c=c, src=src, dst=dst: e.tensor_scalar(out=dst, in0=src, scalar1=pcols[:, PC_GIN + c:PC_GIN + c + 1],
                                                                                          scalar2=pcols[:, PC_BIN + c:PC_BIN + c + 1], op0=ALU.mult, op1=ALU.add),
                                 reads=[r_p, r_pcols], writes=[r_uT[2 * T + half]])

            p1_A(0)
            for T in range(16):
                if T + 1 < 16:
                    p1_A(T + 1)
                p1_B(T)

            if stop < 2:
                continue
            S.barrier()
            cnt["wide"] = False
            if stage == 2 and s == 0:
                init_scratch()
            off = 0
            wqkv, off = carve(off, [3, 8, 256], BF16)
            qT, off = carve(off, [2, SEQ], BF16)
            kT, off = carve(off, [2, SEQ], BF16)
            Vh, off = carve(off, [16, 260], BF16)
            PT0, off = carve(off, [16, 512], BF16)
            PT1, off = carve(off, [16, 512], BF16)
            PT = [PT0, PT1]
            o1s, o2s, onbs = [], [], []
            for _i in range(2):
                a, off = carve(off, [258], F32); o1s.append(a)
                a, off = carve(off, [258], F32); o2s.append(a)
            for _i in range(3):
                a, off = carve(off, [256], BF16); onbs.append(a)
            assert off <= ARENA, off
            r_o1s, r_o2s, r_smcs = [[Res(), Res()] for _ in range(3)]
            r_onbs = [Res(), Res(), Res()]
            cnt["cmb"] = 0
            pend_tr = []
            r_wseg, r_q, r_k = [Res(), Res(), Res()], Res(), Res()
            r_V = [Res() for _ in range(16)]
            r_PT = [[Res() for _ in range(16)] for _ in range(2)]
            S.op("pool", lambda e: e.memset(Vh[:, :, 256:257], 1.0), writes=r_V)

            def load_wseg(h, si):
                src = wqkv_d[h].rearrange("p (s k c) -> p s k c", s=3, k=8)
                S.dma("pool", lambda e: e.dma_start(out=wqkv[:, si, :, :], in_=src[:, si, :, :], max_dma_last_dim=8192), writes=[r_wseg[si]])

            for si in range(3):
                load_wseg(0, si)
            for h in range(H):
                for (dst, si, r_dst) in ((qT, 0, r_q), (kT, 1, r_k)):
                    for m in range(2):
                        for tg in range(4):
                            ps, r_ps = next_pg()
                            mm(ps[:, :], [(wqkv[:, si, k, m * 128:(m + 1) * 128], uT[:, k, tg * 512:(tg + 1) * 512]) for k in range(8)],
                               reads=[r_wseg[si]] + r_uT[tg * 8:tg * 8 + 8], writes=[r_ps])
                            evac(dst[:, m, tg * 512:(tg + 1) * 512], ps[:, :], reads=[r_ps], writes=[r_dst], )
                    if h + 1 < H:
                        load_wseg(h + 1, si)
                for T in range(16):
                    ps, r_ps = next_pg()
                    mm(ps[:, 0:256], [(uT[:, k, T * 128:(T + 1) * 128], wqkv[:, 2, k, :]) for k in range(8)], reads=[r_wseg[2], r_uT[2 * T], r_uT[2 * T + 1]], writes=[r_ps])
                    evac(Vh[:, T, 0:256], ps[:, 0:256], reads=[r_ps], writes=[r_V[T]])
                if h + 1 < H:
                    load_wseg(h + 1, 2)
                for g in range(4):
                    nk = 4 * g + 4
                    emit_conv(1)
                    for m in range(2):
                        for j in range(nk):
                            c0 = max(0, j - 4 * g) * 128
                            ps, r_ps = next_pg()
                            fns = [lambda e, ps=ps, j=j, c0=c0, m=m, g=g: e.matmul(ps[:, c0:512], lhsT=kT[:, m, j * 128:(j + 1) * 128],
                                                                                 rhs=qT[:, m, g * 512 + c0:(g + 1) * 512], start=True, stop=False)]
                            lo_q = j - 4 * g
                            if lo_q >= 0:
                                ncol = 256 if lo_q <= 2 else 128
                                cs, bs = lo_q * 128, 0
                            else:
                                ncol, cs, bs = (128, 0, 128) if lo_q == -1 else (0, 0, 0)
                            if ncol > 0:
                                for hl in range(2):
                                    fns.append(lambda e, ps=ps, cs=cs, ncol=ncol, bs=bs, hl=hl, h=h: e.matmul(ps[:, cs:cs + ncol], lhsT=identb[:], rhs=bcat[:, h, hl, bs:bs + ncol],
                                                                                                         start=False, stop=(hl == 1)))
                            else:
                                fns[0] = lambda e, ps=ps, j=j, c0=c0, m=m, g=g: e.matmul(ps[:, c0:512], lhsT=kT[:, m, j * 128:(j + 1) * 128],
                                                                                        rhs=qT[:, m, g * 512 + c0:(g + 1) * 512], start=True, stop=True)
                            S.ops("pe", fns, reads=[r_k, r_q, r_bcat, r_identb], writes=[r_ps])
                            S.op("act", lambda e, ps=ps, m=m, j=j, c0=c0: e.activation(out=PT[m][:, j, c0:512], in_=ps[:, c0:512], func=AF.Exp, scale=SCALE),
                                 reads=[r_ps], writes=[r_PT[m][j]])
                    for li in range(4):
                        i = 4 * g + li
                        ovs = []
                        for m in range(2):
                            pvb, r_pvb = next_pv()
                            mm(pvb[:, 0:257], [(PT[m][:, j, li * 128:(li + 1) * 128], Vh[:, j, 0:257]) for j in range(i + 1)],
                               reads=r_PT[m][0:i + 1] + r_V[0:i + 1], writes=[r_pvb])
                            ovs.append((pvb, r_pvb))
                        (o1, r_o1), (o2, r_o2) = ovs
                        while len(pend_tr) > 1:
                            pend_tr.pop(0)()
                        cs = cnt["cmb"] % 2
                        cnt["cmb"] += 1
                        c3 = (cnt["cmb"] - 1) % 3
                        a1, a2, onb, smc = o1s[cs], o2s[cs], onbs[c3], smcs[:, cs * 8:cs * 8 + 8]
                        r_a1, r_a2, r_onb, r_smc = r_o1s[cs], r_o2s[cs], r_onbs[c3], r_smcs[cs]
                        S.op("dve", lambda e, o1=o1, a1=a1: e.tensor_copy(out=a1[:, 0:257], in_=o1[:, 0:257]), reads=[r_o1], writes=[r_a1])
                        S.op("dve", lambda e, o2=o2, a2=a2: e.tensor_copy(out=a2[:, 0:257], in_=o2[:, 0:257]), reads=[r_o2], writes=[r_a2])
                        S.op("dve", lambda e, a1=a1, smc=smc: e.reciprocal(out=smc[:, 0:1], in_=a1[:, 256:257]), reads=[r_a1], writes=[r_smc])
                        S.op("dve", lambda e, a2=a2, smc=smc: e.reciprocal(out=smc[:, 1:2], in_=a2[:, 256:257]), reads=[r_a2, r_smc], writes=[r_smc])
                        S.op("dve", lambda e, smc=smc: e.tensor_tensor(out=smc[:, 1:2], in0=smc[:, 1:2], in1=lamc[:, 1:2], op=ALU.mult), reads=[r_smc, r_lamc], writes=[r_smc])
                        S.op("dve", lambda e, a1=a1, smc=smc: e.tensor_scalar(out=a1[:, 0:256], in0=a1[:, 0:256], scalar1=smc[:, 0:1], scalar2=None, op0=ALU.mult), reads=[r_a1, r_smc], writes=[r_a1])
                        S.op("dve", lambda e, a1=a1, a2=a2, smc=smc: e.scalar_tensor_tensor(out=a2[:, 0:256], in0=a2[:, 0:256], scalar=smc[:, 1:2], in1=a1[:, 0:256], op0=ALU.mult, op1=ALU.add),
                             reads=[r_a2, r_smc, r_a1], writes=[r_a2])
                        S.op("dve", lambda e, a1=a1, a2=a2, smc=smc: e.scalar_tensor_tensor(out=a1[:, 0:256], in0=a2[:, 0:256], scalar=1.0, in1=a2[:, 0:256], op0=ALU.mult, op1=ALU.mult,
                                                                                            accum_out=smc[:, 2:3]), reads=[r_a2], writes=[r_a1, r_smc])
                        S.op("act", lambda e, smc=smc: e.activation(out=smc[:, 3:4], in_=smc[:, 2:3], func=AF.Ln, bias=epsc[:, 1:2], scale=1.0 / 256.0),
                             reads=[r_smc, r_eps], writes=[r_smc])
                        S.op("act", lambda e, smc=smc: e.activation(out=smc[:, 3:4], in_=smc[:, 3:4], func=AF.Exp, scale=-0.5), reads=[r_smc], writes=[r_smc])
                        S.op("dve", lambda e, a2=a2, onb=onb, smc=smc: e.scalar_tensor_tensor(out=onb, in0=a2[:, 0:256], scalar=smc[:, 3:4], in1=subg[:], op0=ALU.mult, op1=ALU.mult),
                             reads=[r_a2, r_smc, r_subg], writes=[r_onb])

                        def tr_out(i=i, h=h, onb=onb, r_onb=r_onb):
                            pt_ap, r_p = next_pt()
                            S.ops("pe", [(lambda e, cc=cc, pt_ap=pt_ap, onb=onb: e.transpose(out=pt_ap[:, cc * 128:(cc + 1) * 128], in_=onb[:, cc * 128:(cc + 1) * 128], identity=identb[:]))
                                         for cc in range(2)], reads=[r_onb, r_identb], writes=[r_p])
                            evac(oT[:, 2 * h:2 * h + 2, i * 128:(i + 1) * 128], pt_ap[:, 0:256].rearrange("p (a b) -> p a b", a=2), reads=[r_p], writes=[r_oT[i]], )
                        pend_tr.append(tr_out)

            while pend_tr:
                pend_tr.pop(0)()
            if stage == 3 and s == 0:
                S.dma("pool", lambda e: e.dma_start(out=out_d[0:2048, :].rearrange("(c p two) d -> p c (two d)", c=8, two=2), in_=oT[:, :, :]), reads=r_oT)
                S.dma("pool", lambda e: e.dma_start(out=out_d[2048:4096, :].rearrange("(c p two) d -> p c (two d)", c=8, two=2), in_=uT[:, :, :]), reads=r_uT)
                break
            if stop < 3:
                continue
            S.barrier()
            cnt["wide"] = "p3"
            off = 0
            wr2, wa2, w22 = [], [], []
            for _i in range(2):
                a, off = carve(off, [4, 8, 128], BF16); wr2.append(a)
                a, off = carve(off, [8, 128], BF16); wa2.append(a)
                a, off = carve(off, [2, 8, 128], BF16); w22.append(a)
            ybuf, off = carve(off, [1026], F32)
            cbs, off = carve(off, [1024], BF16)
            sa, off = carve(off, [512], F32)
            tmp, off = carve(off, [512], F32)
            zb, off = carve(off, [1024], F32)
            off_cT = off
            cT, off = carve(off, [8, 1024], BF16)
            mT, off = carve(off, [8, 1024], BF16)
            assert off <= ARENA, off
            r_wr2, r_wa2, r_w22 = [Res(), Res()], [Res(), Res()], [[Res(), Res()], [Res(), Res()]]
            r_y, r_cbs, r_sa, r_tmp, r_z = [Res() for _ in range(5)]
            r_cT = [Res() for _ in range(8)]
            r_mT = [Res() for _ in range(8)]

            def load_w1(n):
                j, bi = n % 8, n % 2
                src = wr_d[j].rearrange("p (s k c) -> p s k c", s=5, k=8)
                S.dma("pool", lambda e: e.dma_start(out=wr2[bi], in_=src[:, 0:4, :, :], max_dma_last_dim=8192), writes=[r_wr2[bi]])
                S.dma("pool", lambda e: e.dma_start(out=wa2[bi], in_=wa_d[j].rearrange("p (k c) -> p k c", k=8)), writes=[r_wa2[bi]])

            def load_w2(n):
                j, bi = n % 8, n % 2
                src = wr_d[j].rearrange("p (s k c) -> p s k c", s=5, k=8)
                S.dma("pool", lambda e: e.dma_start(out=w22[bi][:, 0, :, :], in_=src[:, 4, :, :]), writes=[r_w22[bi][0]])
                S.dma("pool", lambda e: e.dma_start(out=w22[bi][:, 1, :, :], in_=wb_d[j].rearrange("p (k c) -> p k c", k=8)), writes=[r_w22[bi][1]])

            load_w1(0)
            for hs in range(2):
                h0 = hs * 1024
                for j in range(8):
                    n1 = hs * 8 + j
                    bi = n1 % 2
                    wr, wa, r_wr, r_wa = wr2[bi], wa2[bi], r_wr2[bi], r_wa2[bi]
                    if j + 1 < 8:
                        load_w1(n1 + 1)
                    else:
                        load_w2(hs * 8)
                    if hs == 0:
                        S.op("dve", lambda e: e.memset(ybuf[:, 0:2], 0.0), writes=[r_y])
                    else:
                        S.op("dve", lambda e, j=j: e.tensor_copy(out=ybuf[:, 0:2], in_=halo[:, j, :]), reads=[r_halo], writes=[r_y])
                    for tg2 in range(2):
                        t0 = h0 + tg2 * 512
                        rU = r_uT[t0 // 64:t0 // 64 + 8]
                        l0 = tg2 * 512

                        def proj(si):
                            ps, r_ps = next_pg()
                            mm(ps[:, :], [(wr[:, si, k, :], uT[:, k, t0:t0 + 512]) for k in range(8)], reads=[r_wr] + rU, writes=[r_ps])
                            return ps, r_ps
                        pc, r_pc = proj(1)
                        S.op("act", lambda e, pc=pc: e.activation(out=tmp, in_=pc[:, :], func=AF.Copy), reads=[r_pc], writes=[r_tmp])
                        ph, r_ph = proj(2)
                        S.op("dve", lambda e, ph=ph, l0=l0: e.tensor_tensor(out=ybuf[:, 2 + l0:2 + l0 + 512], in0=ph[:, :], in1=tmp, op=ALU.mult),
                             reads=[r_ph, r_tmp], writes=[r_y])
                        pb, r_pb = proj(0)
                        S.op("act", lambda e, pb=pb, l0=l0: e.activation(out=cbs[:, l0:l0 + 512], in_=pb[:, :], func=AF.Copy), reads=[r_pb], writes=[r_cbs])
                        pa, r_pa = proj(3)
                        S.op("act", lambda e, pa=pa, j=j: e.activation(out=sa, in_=pa[:, :], func=AF.Sigmoid, bias=pcols[:, PC_BGA + j:PC_BGA + j + 1], scale=1.0),
                             reads=[r_pa, r_pcols], writes=[r_sa])
                        pya, r_pya = next_pg()
                        mm(pya[:, :], [(wa[:, k, :], oT[:, k, t0:t0 + 512]) for k in range(8)], reads=[r_wa] + r_oT[t0 // 128:t0 // 128 + 4], writes=[r_pya])
                        S.op("dve", lambda e, pya=pya, j=j, l0=l0: e.tensor_tensor(out=mT[:, j, l0:l0 + 512], in0=pya[:, :], in1=sa, op=ALU.mult),
                             reads=[r_pya, r_sa], writes=[r_mT[j]])
                    cw = lambda w, j=j: pcols[:, PC_CW + w * 8 + j:PC_CW + w * 8 + j + 1]
                    S.op("dve", lambda e, cw=cw: e.tensor_scalar(out=zb, in0=ybuf[:, 2:1026], scalar1=cw(2), scalar2=None, op0=ALU.mult), reads=[r_y, r_pcols], writes=[r_z])
                    S.op("dve", lambda e, cw=cw: e.scalar_tensor_tensor(out=zb, in0=ybuf[:, 1:1025], scalar=cw(1), in1=zb, op0=ALU.mult, op1=ALU.add),
                         reads=[r_y, r_z, r_pcols], writes=[r_z])
                    S.op("dve", lambda e, cw=cw: e.scalar_tensor_tensor(out=zb, in0=ybuf[:, 0:1024], scalar=cw(0), in1=zb, op0=ALU.mult, op1=ALU.add),
                         reads=[r_y, r_z, r_pcols], writes=[r_z])
                    S.op("pool", lambda e, j=j: e.tensor_tensor(out=cT[:, j, :], in0=zb, in1=cbs, op=ALU.mult), reads=[r_z, r_cbs], writes=[r_cT[j]])
                    if hs == 0:
                        S.op("dve", lambda e, j=j: e.tensor_copy(out=halo[:, j, :], in_=ybuf[:, 1024:1026]), reads=[r_y], writes=[r_halo])
                for j in range(8):
                    n2 = hs * 8 + j
                    bi = n2 % 2
                    w2, r_w2 = w22[bi], r_w22[bi]
                    if j + 1 < 8:
                        load_w2(n2 + 1)
                    elif hs == 0:
                        load_w1(8)
                    for tg2 in range(2):
                        l0 = tg2 * 512
                        t0 = h0 + l0
                        pgb, r_pgb = next_pg()
                        mm(pgb[:, :], [(w2[:, 0, k, :], uT[:, k, t0:t0 + 512]) for k in range(8)], reads=[r_w2[0]] + r_uT[t0 // 64:t0 // 64 + 8], writes=[r_pgb])
                        S.op("act", lambda e, pgb=pgb, j=j: e.activation(out=sa, in_=pgb[:, :], func=AF.Sigmoid, bias=pcols[:, PC_BGB + j:PC_BGB + j + 1], scale=1.0),
                             reads=[r_pgb, r_pcols], writes=[r_sa])
                        pyb, r_pyb = next_pg()
                        mm(pyb[:, :], [(w2[:, 1, k, :], cT[:, k, l0:l0 + 512]) for k in range(8)], reads=[r_w2[1]] + r_cT, writes=[r_pyb])
                        S.op("dve", lambda e, pyb=pyb: e.tensor_tensor(out=tmp, in0=pyb[:, :], in1=sa, op=ALU.mult), reads=[r_pyb, r_sa], writes=[r_tmp])
                        S.op("pool", lambda e, j=j, l0=l0: e.tensor_tensor(out=mT[:, j, l0:l0 + 512], in0=mT[:, j, l0:l0 + 512], in1=tmp, op=ALU.add),
                             reads=[r_tmp, r_mT[j]], writes=[r_mT[j]])
                if stage == 4 and s == 0 and hs == 0:
                    for ii, (buf, rr) in enumerate(((mT, r_mT), (cT, r_cT))):
                        S.dma("pool", lambda e, ii=ii, buf=buf: e.dma_start(out=out_d[ii * 1024:(ii + 1) * 1024, :].rearrange("(c p) d -> p c d", c=8), in_=buf), reads=rr)
                    break
                if stage == 5 and hs == 1:
                    break
                WS = [{"u": (wk[0][:, :], [r_wk[0]]), "t": (wk[1][:, :], [r_wk[1]]), "n": (wk[2][:, :], [r_wk[2]]), "xb": (xh[0][:, :], [r_xh[0]])},
                      {"u": (arena[:, off_cT:off_cT + 4096].bitcast(F32), r_cT[0:2]), "t": (arena[:, off_cT + 4096:off_cT + 8192].bitcast(F32), r_cT[2:4]),
                       "n": (arena[:, off_cT + 8192:off_cT + 12288].bitcast(F32), r_cT[4:6]), "xb": (arena[:, off_cT + 12288:off_cT + 14336].bitcast(BF16), r_cT[6:7])}]
                def ln1_A(tl):
                    T = hs * 8 + tl
                    gT = s * 16 + T
                    row0 = base + T * 128
                    b = gT % 2
                    W = WS[tl % 2] if stage == 2 else WS[0]
                    (Wu, Ru), (Wt, Rt), (Wn, Rn) = W["u"], W["t"], W["n"]
                    if tl == 0:
                        S.dma("sp", lambda e: e.dma_start(out=xs[b][:], in_=x_d[row0:row0 + 128, :]), writes=[r_xs[b]])
                    if tl + 1 < 8:
                        S.dma("sp", lambda e: e.dma_start(out=xs[1 - b][:], in_=x_d[row0 + 128:row0 + 256, :]), writes=[r_xs[1 - b]])
                    S.op("act", lambda e: e.activation(out=Wu, in_=xs[b][:], func=AF.Identity, bias=nmr_in[:, gT:gT + 1], scale=rstd_in[:, gT:gT + 1]),
                         reads=[r_xs[b], r_mvs[gT]], writes=Ru)
                    S.op("pool", lambda e: e.tensor_tensor(out=Wu, in0=Wu, in1=gin_row, op=ALU.mult), reads=Ru + [r_prow], writes=Ru)
                    S.op("pool", lambda e: e.tensor_tensor(out=Wu, in0=Wu, in1=bin_row, op=ALU.add), reads=Ru + [r_prow], writes=Ru)

                def ln1_A2(tl):
                    W = WS[tl % 2] if stage == 2 else WS[0]
                    (Wu, Ru), (Wt, Rt), (Wn, Rn) = W["u"], W["t"], W["n"]
                    for hh in range(2):
                        ps, r_ps = next_pg()
                        mm(ps[:, :], [(mT[:, k, tl * 128:(tl + 1) * 128], wo_s[:, k, hh * 512:(hh + 1) * 512]) for k in range(8)], reads=r_mT + [r_wo], writes=[r_ps])
                        S.op("dve", lambda e, ps=ps, hh=hh: e.scalar_tensor_tensor(out=Wt[:, hh * 512:(hh + 1) * 512], in0=Wu[:, hh * 512:(hh + 1) * 512], scalar=ALPHA,
                                                                                   in1=ps[:, :], op0=ALU.mult, op1=ALU.add),
                             reads=[r_ps] + Ru, writes=Rt)
                    return ln_stats(Wt, Rt, tl % 2)

                def ln1_B(tl, sm, r_sm):
                    T = hs * 8 + tl
                    gT = s * 16 + T
                    row0 = base + T * 128
                    W = WS[tl % 2] if stage == 2 else WS[0]
                    (Wu, Ru), (Wt, Rt), (Wn, Rn) = W["u"], W["t"], W["n"]
                    S.op("act", lambda e: e.activation(out=Wn, in_=Wt, func=AF.Identity, bias=sm[:, 9:10], scale=sm[:, 8:9]),
                         reads=Rt + [r_sm], writes=Rn)
                    S.op("pool", lambda e: e.tensor_tensor(out=Wn, in0=Wn, in1=prow[:, PR_G1:PR_G1 + 1024], op=ALU.mult), reads=Rn + [r_prow], writes=Rn)
                    S.op("dve", lambda e: e.tensor_tensor(out=Wu, in0=Wn, in1=prow[:, PR_B1:PR_B1 + 1024], op=ALU.add), reads=Rn + [r_prow], writes=Ru)
                    if stage == 6:
                        S.dma("sp", lambda e: e.dma_start(out=out_d[row0:row0 + 128, :], in_=wk[1][:]), reads=[r_wk[1]])
                    elif stage == 1:
                        S.dma("sp", lambda e: e.dma_start(out=out_d[row0:row0 + 128, :], in_=wk[0][:]), reads=[r_wk[0]])
                    else:
                        route_tile(gT, row0, W)

                if stage == 2:
                    ln1_A(0)
                    for tl in range(8):
                        if tl + 1 < 8:
                            ln1_A(tl + 1)
                        pend = ln1_A2(tl)
                        ln1_B(tl, *pend)
                else:
                    for tl in range(8):
                        ln1_A(tl)
                        pend = ln1_A2(tl)
                        if stage == 5:
                            S.dma("sp", lambda e: e.dma_start(out=out_d[0:128, :], in_=wk[0][:]), reads=[r_wk[0]])
                            S.dma("sp", lambda e: e.dma_start(out=out_d[128:256, :], in_=wk[1][:]), reads=[r_wk[1]])
                            break
                        ln1_B(tl, *pend)
            if stage in (4, 5):
                break
            S.barrier()

        if stage == 2:
            moe_phase()
        S.finish()
        S.emit()
    return nc


_PROG = {}


def _tile_w(w, ncol_blocks, cb):
    return np.ascontiguousarray(w.reshape(8, 128, ncol_blocks, cb).transpose(2, 1, 0, 3).reshape(ncol_blocks, 128, 8 * cb))


def _cols(v):
    return np.ascontiguousarray(np.asarray(v, np.float32).reshape(8, 128).T)


def kernel(stage=2, **inp):
    f = lambda a: np.asarray(a, dtype=np.float32)
    x = f(inp["x"])
    w_in = f(inp["w_in"])[0]
    wq = _tile_w(w_in[:, 0:1024], 4, 256).reshape(4, 128, 8, 256)
    wk_ = _tile_w(w_in[:, 1024:2048], 4, 256).reshape(4, 128, 8, 256)
    wv = _tile_w(w_in[:, 2048:3072], 4, 256).reshape(4, 128, 8, 256)
    wqkv = np.ascontiguousarray(np.stack([wq, wk_, wv], axis=2).reshape(4, 128, 3 * 8 * 256))
    wr5 = [_tile_w(w_in[:, 3072 + si * 1024:3072 + (si + 1) * 1024], 8, 128).reshape(8, 128, 8, 128) for si in range(5)]
    wr = np.ascontiguousarray(np.stack(wr5, axis=2).reshape(8, 128, 5 * 8 * 128))
    wa = _tile_w(f(inp["w_a_proj"])[0], 8, 128)
    wb = _tile_w(f(inp["w_b_proj"])[0], 8, 128)
    wo = _tile_w(f(inp["w_o"])[0], 1, 1024)[0]
    pcols = np.zeros((128, PC_N), np.float32)
    pcols[:, PC_GIN:PC_GIN + 8] = _cols(inp["ln_in_g"])
    pcols[:, PC_BIN:PC_BIN + 8] = _cols(inp["ln_in_b"])
    pcols[:, PC_BGA:PC_BGA + 8] = _cols(f(inp["b_gate"])[0, 0])
    pcols[:, PC_BGB:PC_BGB + 8] = _cols(f(inp["b_gate"])[0, 1])
    cw = f(inp["conv_w"])[0, :, 0, :]
    for w in range(3):
        pcols[:, PC_CW + w * 8:PC_CW + (w + 1) * 8] = _cols(cw[w])
    prow1 = np.zeros((PR_N,), np.float32)
    prow1[PR_G1:PR_G1 + 1024] = f(inp["ln1_g"])[0]
    prow1[PR_B1:PR_B1 + 1024] = f(inp["ln1_b"])[0]
    prow1[PR_SUBG:PR_SUBG + 256] = f(inp["subln_g"])[0]
    prow1[PR_G2:PR_G2 + 1024] = f(inp["ln_in_g"])
    prow1[PR_B2:PR_B2 + 1024] = f(inp["ln_in_b"])
    prow1[PR_BR:PR_BR + 4] = f(inp["b_group"])[0]
    prow1[PR_BR + 4:PR_BR + 36] = f(inp["b_sub"])[0].reshape(-1)
    prow = np.ascontiguousarray(np.broadcast_to(prow1[None, :], (128, PR_N)))
    prs1 = np.zeros((PS_N,), np.float32)
    prs1[PS_LQ:PS_LQ + 256] = f(inp["lambda_q"])[0].reshape(-1)
    prs1[PS_LK:PS_LK + 256] = f(inp["lambda_k"])[0].reshape(-1)
    prs1[PS_RB:PS_RB + 128] = f(inp["rel_bias"]).reshape(-1)
    prows = np.ascontiguousarray(np.broadcast_to(prs1[None, :], (128, PS_N)))
    ident = np.eye(128, dtype=np.float32)
    shared = {"wqkv": wqkv, "wr": wr, "wa": wa, "wb": wb, "wo": wo, "pcols": pcols, "prow": prow, "prows": prows, "oh": _OH_NP, "negm": _NEGM_NP,
              "ident": ident}
    if stage == 2:
        prow2 = np.ascontiguousarray(np.broadcast_to(np.concatenate([f(inp["ln2_g"])[0], f(inp["ln2_b"])[0]])[None, :], (128, 2048)))
        srow = np.ascontiguousarray(np.broadcast_to((np.arange(32, dtype=np.float32) * CAP - BIGIDX)[None, :], (128, 32)))
        tb0 = np.zeros((128, NT, 2, 4), np.float32)
        tokid = np.arange(NT, dtype=np.float32)[None, :] * 128 + np.arange(128, dtype=np.float32)[:, None]
        for k in range(2):
            tb0[:, :, k, 0] = tokid
            tb0[:, :, k, 2] = 2 * tokid + k
        ustr = np.triu(np.ones((128, 128), np.float32), 1)
        wrf = np.concatenate([f(inp["w_group"])[0]] + [f(inp["w_sub"])[0, g] for g in range(4)], axis=1)
        wrt = np.ascontiguousarray(wrf.reshape(8, 128, 36).transpose(1, 0, 2).reshape(128, 8 * 36))
        wg = np.ascontiguousarray(f(inp["w_gate_e"])[0].reshape(NE, 8, 128, 512).transpose(0, 2, 1, 3).reshape(NE, 128, 8 * 512))
        wu = np.ascontiguousarray(f(inp["w_up_e"])[0].reshape(NE, 8, 128, 512).transpose(0, 2, 1, 3).reshape(NE, 128, 8 * 512))
        wd = np.ascontiguousarray(f(inp["w_down_e"])[0].reshape(NE, 4, 128, 1024).transpose(0, 2, 1, 3).reshape(NE, 128, 4 * 1024))
        shared.update({"prow2": prow2, "srow": srow, "tball0": np.ascontiguousarray(tb0.reshape(128, NT * 8)), "ustr": ustr, "wrt": wrt, "wg": wg, "wu": wu, "wd": wd})
    if stage not in _PROG:
        _PROG[stage] = build_program(stage)
    nc = _PROG[stage]
    xr = x.reshape(NCORES, NTOK, D)
    in_maps = [dict(shared, x=np.ascontiguousarray(xr[c])) for c in range(NCORES)]
    res = run_bass_kernel_spmd(nc, in_maps, core_ids=list(range(NCORES)))
    out = np.stack([np.asarray(r["out"], np.float32) for r in res.results], axis=0)
    return out.reshape(16, SEQ, D)
```

```python
import contextlib
import math
import numpy as np
import concourse.bass as bass
import concourse.mybir as mybir
from concourse.bass_utils import run_bass_kernel_spmd

F32 = mybir.dt.float32
BF16 = mybir.dt.bfloat16
I32 = mybir.dt.int32
AF = mybir.ActivationFunctionType
ALU = mybir.AluOpType
AX = mybir.AxisListType

SAME_ENGINE_SYNC = True
NCORES = 8
D = 1024
SEQ = 2048
NSEQ = 2
NTOK = NSEQ * SEQ
NT = NTOK // 128
H = 4
LN_EPS = 1e-5
RMS_EPS = 1e-6
ALPHA = 2.0 ** 0.25
LAM_INIT = 0.2
SCALE = 128 ** -0.5
NE = 32
CAP = 384
NCT = CAP // 128
NSLOT = NE * CAP
BIGIDX = 1.0e6


class Res:
    __slots__ = ("w", "w0", "r", "excl")

    def __init__(self, excl=False):
        self.w = {}
        self.w0 = {}
        self.r = {}
        self.excl = excl


class Sched:
    ENG = ("pe", "dve", "act", "pool", "sp")

    def __init__(self, nc, stack, n_dma_sems=40):
        self.nc = nc
        self.q = {e: [] for e in self.ENG}
        self.sems = {}
        self.cnt = {}
        for e in self.ENG:
            self.sems[e] = stack.enter_context(nc.semaphore("s_" + e))
            self.cnt[e] = 0
        self.dma_keys = []
        for i in range(n_dma_sems):
            k = "d%d" % i
            self.sems[k] = stack.enter_context(nc.semaphore("s_" + k))
            self.cnt[k] = 0
            self.dma_keys.append(k)
        self.dma_rr = 0
        self.dma_rr_sw = 0
        self.waited = {}
        self.nops = 0
        self.deferred = []
        self.pumping = False

    def _wait(self, e, key, val):
        if key == e and not SAME_ENGINE_SYNC:
            return
        if self.waited.get((e, key), 0) >= val:
            return
        self.waited[(e, key)] = val
        sem = self.sems[key]
        self.q[e].append(lambda eng, sem=sem, val=val: eng.wait_ge(sem, val))

    def _deps(self, e, reads, writes, cowrite):
        for r in reads:
            for k, v in r.w.items():
                self._wait(e, k, v)
        for w in writes:
            src = w.w0 if cowrite else w.w
            for k, v in src.items():
                self._wait(e, k, v)
            for k, v in w.r.items():
                self._wait(e, k, v)

    def _record(self, key, val, reads, writes, cowrite):
        for r in reads:
            if r.r.get(key, 0) < val:
                r.r[key] = val
        for w in writes:
            if cowrite:
                w.w[key] = max(w.w.get(key, 0), val)
            else:
                w.w = {key: val}
                w.w0 = {key: val}
                w.r = {}

    @staticmethod
    def _split(reads, writes, cowrite):
        ex = [r for r in reads if r.excl]
        if ex:
            assert not cowrite
            reads = [r for r in reads if not r.excl]
            writes = list(writes) + ex
        return reads, writes

    def ops(self, e, fns, reads=(), writes=(), cowrite=False):
        reads, writes = self._split(reads, writes, cowrite)
        self._deps(e, reads, writes, cowrite)
        self.cnt[e] += 1
        val = self.cnt[e]
        sem = self.sems[e]
        for fn in fns[:-1]:
            self.q[e].append(lambda eng, fn=fn: fn(eng))
        fn = fns[-1]
        self.q[e].append(lambda eng, fn=fn, sem=sem: fn(eng).then_inc(sem, 1))
        self._record(e, val, reads, writes, cowrite)
        self.nops += len(fns)
        self._autopump()

    def op(self, e, fn, reads=(), writes=(), cowrite=False):
        self.ops(e, [fn], reads, writes, cowrite)

    def dma(self, e, fn, reads=(), writes=(), cowrite=False):
        half = len(self.dma_keys) // 2
        if e == "pool":
            k = self.dma_keys[half + self.dma_rr_sw % half]
            self.dma_rr_sw += 1
        else:
            k = self.dma_keys[self.dma_rr % half]
            self.dma_rr += 1
        if self.cnt[k] > 0:
            self._wait(e, k, self.cnt[k])
        reads, writes = self._split(reads, writes, cowrite)
        self._deps(e, reads, writes, cowrite)
        self.cnt[k] += 16
        val = self.cnt[k]
        sem = self.sems[k]
        self.q[e].append(lambda eng, fn=fn, sem=sem: fn(eng).then_inc(sem, 16))
        self._record(k, val, reads, writes, cowrite)
        self.nops += 1
        self._autopump()

    def _autopump(self):
        if self.deferred and not self.pumping:
            self.pumping = True
            self.pump(2)
            self.pumping = False

    def defer(self, thunks):
        self.deferred.extend(thunks)

    def pump(self, n=1):
        while n > 0 and self.deferred:
            self.deferred.pop(0)()
            n -= 1

    def flush(self):
        self.pump(1 << 30)

    def barrier(self):
        for e in self.ENG:
            for k, v in self.cnt.items():
                if v > 0 and k != e:
                    self._wait(e, k, v)

    def finish(self, e="sp"):
        self.flush()
        for k in self.dma_keys:
            if self.cnt[k] > 0:
                self._wait(e, k, self.cnt[k])

    def emit(self):
        with self.nc.Block() as block:
            @block.tensor
            def _(eng):
                for f in self.q["pe"]:
                    f(eng)

            @block.vector
            def _(eng):
                for f in self.q["dve"]:
                    f(eng)

            @block.scalar
            def _(eng):
                for f in self.q["act"]:
                    f(eng)

            @block.gpsimd
            def _(eng):
                for f in self.q["pool"]:
                    f(eng)

            @block.sync
            def _(eng):
                for f in self.q["sp"]:
                    f(eng)


def _t5_bucket(rel):
    nb = 16
    max_exact = 8
    bucket = np.where(rel > 0, nb, 0)
    n = np.abs(rel)
    nf = np.maximum(n, 1).astype(np.float32)
    large = max_exact + (np.log(nf / max_exact) / math.log(128 / max_exact) * (nb - max_exact)).astype(np.int32)
    large = np.minimum(large, nb - 1)
    return bucket + np.where(n < max_exact, n, large)


def _static_tables():
    k = np.arange(128)[:, None]
    q = np.arange(128)[None, :]
    allowed = (k // 64) <= (q // 64)
    bd = _t5_bucket(k - q)
    bo = _t5_bucket(k - q - 128)
    entries = []
    ohs = []
    for b in sorted(set(bd[allowed].tolist())):
        entries.append((0, b))
        ohs.append(((bd == b) & allowed).astype(np.float32))
    for b in sorted(set(bo.flatten().tolist())):
        entries.append((1, b))
        ohs.append((bo == b).astype(np.float32))
    oh = np.stack(ohs, axis=1)
    negm = np.where(allowed, 0.0, -8192.0).astype(np.float32)
    return entries, np.ascontiguousarray(oh.reshape(128, -1)), negm


_OH_ENTRIES, _OH_NP, _NEGM_NP = _static_tables()
NOH = len(_OH_ENTRIES)

PR_G1, PR_B1, PR_SUBG, PR_G2, PR_B2, PR_BR = 0, 1024, 2048, 2304, 3328, 4352
PR_N = 4352 + 36
PS_LQ, PS_LK, PS_RB, PS_N = 0, 256, 512, 640
PC_GIN, PC_BIN, PC_BGA, PC_BGB, PC_CW = 0, 8, 16, 24, 32
PC_N = 56


import os as _os
_DBG = {k: True for k in _os.environ.get("MK_DBG", "").split(",") if k}


def build_program(stage=2, stop=9):
    nc = bass.Bass("TRN2", target_bir_lowering=False)

    def din(name, shape, dt=F32):
        return nc.dram_tensor(name, shape, dt, kind="ExternalInput").ap()

    x_d = din("x", [NTOK, D])
    wqkv_d = din("wqkv", [H, 128, 3 * 8 * 256])
    wr_d = din("wr", [8, 128, 5 * 8 * 128])
    wa_d = din("wa", [8, 128, 8 * 128])
    wb_d = din("wb", [8, 128, 8 * 128])
    wo_d = din("wo", [128, 8 * 1024])
    pcols_d = din("pcols", [128, PC_N])
    prow_d = din("prow", [128, PR_N])
    prows_d = din("prows", [128, PS_N])
    oh_d = din("oh", [128, NOH * 128])
    negm_d = din("negm", [128, 128])
    ident_d = din("ident", [128, 128])
    if stage == 2:
        srow_d = din("srow", [128, 32])
        tball_d = din("tball0", [128, NT * 8])
        prow2_d = din("prow2", [128, 2048])
        ustr_d = din("ustr", [128, 128])
        wrt_d = din("wrt", [128, 8 * 36])
        wg_d = din("wg", [NE, 128, 8 * 512])
        wu_d = din("wu", [NE, 128, 8 * 512])
        wd_d = din("wd", [NE, 128, 4 * 1024])
    out_d = nc.dram_tensor("out", [NTOK, D], F32, kind="ExternalOutput").ap()
    x1f_d = nc.dram_tensor("x1f", [NTOK, D], F32, kind="Internal").ap()
    x1b_d = nc.dram_tensor("x1b", [NTOK + 128, D], BF16, kind="Internal").ap()
    tbl_d = nc.dram_tensor("tbl", [NSLOT, 4], F32, kind="Internal").ap()
    y2_d = nc.dram_tensor("y2", [2 * NTOK, D], BF16, kind="Internal").ap()
    if stage == 2:
        wgb_d = nc.dram_tensor("wgb", [NE, 128, 8 * 512], BF16, kind="Internal").ap()
        wub_d = nc.dram_tensor("wub", [NE, 128, 8 * 512], BF16, kind="Internal").ap()
        wdb_d = nc.dram_tensor("wdb", [NE, 128, 4 * 1024], BF16, kind="Internal").ap()

    with contextlib.ExitStack() as st:
        S = Sched(nc, st)

        def sb(name, shape, dt):
            return st.enter_context(nc.sbuf_tensor(name, shape, dt))

        identf = sb("identf", [128, 128], F32)
        identb = sb("identb", [128, 128], BF16)
        pcols = sb("pcols_s", [128, PC_N], F32)
        prow = sb("prow_s", [128, PR_N], F32)
        bcat = sb("bcat", [128, H, 2, 256], BF16)
        lamc = sb("lamc", [128, 4], F32)
        subg = sb("subg", [128, 256], F32)
        epsc = sb("epsc", [128, 2], F32)
        rstd_in = sb("rstd_in", [128, NT], F32)
        nmr_in = sb("nmr_in", [128, NT], F32)
        wo_s = sb("wo_s", [128, 8, 1024], BF16)
        uT = sb("uT", [128, 8, SEQ], BF16)
        oT = sb("oT", [128, 8, SEQ], BF16)
        halo = sb("halo", [128, 8, 2], F32)
        xs = [sb("xs%d" % i, [128, 1024], F32) for i in range(2)]
        xh = [sb("xh0", [128, 1024], BF16)] * 2
        stt = sb("stt", [128, 12], F32)
        sml = sb("sml", [128, 16], F32)
        wk = [sb("wk%d" % i, [128, 1024], F32) for i in range(3)]
        r_identf, r_identb, r_pcols, r_prow, r_bcat, r_lamc, r_subg, r_eps = [Res() for _ in range(8)]
        r_mv, r_g1, r_wo, r_halo, r_stt, r_sml = [Res() for _ in range(6)]
        r_mvs = [Res() for _ in range(NT)]
        r_xhB = Res()
        r_uT = [Res() for _ in range(32)]
        r_oT = [Res() for _ in range(16)]
        r_xs = [Res(), Res()]
        r_xh = [Res()] * 2
        r_wk = [Res() for _ in range(3)]
        ARENA = 75792
        arena = sb("arena", [128, ARENA], mybir.dt.uint8)

        NPG = 3
        pg = [st.enter_context(nc.psum_tensor("pg%d" % i, [128, 512], F32)) for i in range(NPG)]
        pv = [st.enter_context(nc.psum_tensor("pv%d" % i, [128, 512], F32)) for i in range(3)]
        pt = [st.enter_context(nc.psum_tensor("pt%d" % i, [128, 1024], BF16)) for i in range(2)]
        r_pg = [Res(True) for _ in range(NPG)]
        r_pv = [Res(True) for _ in range(3)]
        r_pt = [Res(True), Res(True)]
        cnt = {"pg": 0, "pv": 0, "pt": 0, "ev": 0}

        def next_pg():
            if cnt.get("wide", False) == "moe" or (cnt.get("wide", False) and _DBG.get("wide" + str(cnt["wide"]))):
                i = cnt["pg"] % (NPG + 3)
                cnt["pg"] += 1
                return (pg + pv)[i], (r_pg + r_pv)[i]
            i = cnt["pg"] % NPG
            cnt["pg"] += 1
            return pg[i], r_pg[i]

        def next_pv():
            i = cnt["pv"] % 3
            cnt["pv"] += 1
            return pv[i], r_pv[i]

        def next_pt():
            i = cnt["pt"] % 2
            cnt["pt"] += 1
            return pt[i][:, 0:512], r_pt[i]

        def mm(out_ap, pairs, reads, writes, first=True, last=True):
            n = len(pairs)
            fns = []
            for i, (a, b) in enumerate(pairs):
                fns.append(lambda e, a=a, b=b, i=i: e.matmul(out_ap, lhsT=a, rhs=b, start=(first and i == 0), stop=(last and i == n - 1)))
            S.ops("pe", fns, reads=reads, writes=writes)

        def evac(out_ap, in_ap, reads, writes, eng=None):
            if eng is None:
                eng = "act" if cnt["ev"] % 2 == 0 else "dve"
                cnt["ev"] += 1
            if eng == "act":
                S.op("act", lambda e: e.activation(out=out_ap, in_=in_ap, func=AF.Copy), reads=reads, writes=writes)
            else:
                S.op("dve", lambda e: e.tensor_copy(out=out_ap, in_=in_ap), reads=reads, writes=writes)

        S.dma("sp", lambda e: e.dma_start(out=identf[:], in_=ident_d), writes=[r_identf])
        S.dma("sp", lambda e: e.dma_start(out=pcols[:], in_=pcols_d), writes=[r_pcols])
        S.dma("sp", lambda e: e.dma_start(out=prow[:], in_=prow_d), writes=[r_prow])
        S.op("dve", lambda e: e.tensor_copy(out=identb[:], in_=identf[:]), reads=[r_identf], writes=[r_identb])
        S.op("dve", lambda e: e.memset(epsc[:, 0:1], LN_EPS), writes=[r_eps])
        S.op("dve", lambda e: e.memset(epsc[:, 1:2], RMS_EPS), writes=[r_eps])
        S.op("dve", lambda e: e.memset(halo[:], 0.0), writes=[r_halo])
        PS_OFF = NOH * 128 * 4 + 512
        prows = arena[:, PS_OFF:PS_OFF + PS_N * 4].bitcast(F32)
        r_prows = Res()
        S.dma("sp", lambda e: e.dma_start(out=prows, in_=prows_d), writes=[r_prows])
        S.op("dve", lambda e: e.tensor_tensor(out=wk[0][:, 0:256], in0=prows[:, PS_LQ:PS_LQ + 256], in1=prows[:, PS_LK:PS_LK + 256], op=ALU.mult),
             reads=[r_prows], writes=[r_wk[0]])
        S.op("dve", lambda e: e.tensor_reduce(out=sml[:, 0:2], in_=wk[0][:, 0:256].rearrange("p (a b) -> p a b", a=2), axis=AX.X, op=ALU.add),
             reads=[r_wk[0]], writes=[r_sml])
        S.op("act", lambda e: e.activation(out=sml[:, 2:4], in_=sml[:, 0:2], func=AF.Exp), reads=[r_sml], writes=[r_sml])
        S.op("dve", lambda e: e.tensor_tensor(out=sml[:, 4:5], in0=sml[:, 2:3], in1=sml[:, 3:4], op=ALU.subtract), reads=[r_sml], writes=[r_sml])
        S.op("dve", lambda e: e.tensor_scalar(out=lamc[:, 0:1], in0=sml[:, 4:5], scalar1=LAM_INIT, scalar2=None, op0=ALU.add), reads=[r_sml], writes=[r_lamc])
        S.op("dve", lambda e: e.tensor_scalar(out=lamc[:, 1:2], in0=lamc[:, 0:1], scalar1=-1.0, scalar2=None, op0=ALU.mult), reads=[r_lamc], writes=[r_lamc])
        S.op("dve", lambda e: e.tensor_scalar(out=subg[:], in0=prow[:, PR_SUBG:PR_SUBG + 256], scalar1=1.0 - LAM_INIT, scalar2=None, op0=ALU.mult),
             reads=[r_prow], writes=[r_subg])
        oh_s = arena[:, 0:NOH * 128 * 4].bitcast(F32).rearrange("p (n q) -> p n q", q=128)
        negm_s = arena[:, NOH * 128 * 4:NOH * 128 * 4 + 512].bitcast(F32)
        r_oh, r_negm = Res(), Res()
        S.dma("sp", lambda e: e.dma_start(out=oh_s, in_=oh_d.rearrange("p (n q) -> p n q", q=128)), writes=[r_oh])
        S.dma("sp", lambda e: e.dma_start(out=negm_s, in_=negm_d), writes=[r_negm])
        rbd = arena[:, 40960:40960 + 32 * H * 4].bitcast(F32).rearrange("p (b h) -> p b h", h=H)
        r_rbd = Res()
        rbv = prows[:, PS_RB:PS_RB + 128].rearrange("p (b h) -> p b h", h=H)
        for h in range(H):
            S.op("dve", lambda e, h=h: e.tensor_scalar(out=rbd[:, :, h], in0=rbv[:, :, h], scalar1=rbv[:, 15, h:h + 1], scalar2=1.0 / SCALE,
                                                        op0=ALU.subtract, op1=ALU.mult), reads=[r_prows], writes=[r_rbd])
        acc = wk[1]
        accp = arena[:, 32768:32768 + 640 * 4].bitcast(F32)
        r_accp = Res()
        for h in range(H):
            on_pool = h >= 2
            A, r_A, eng = (accp, r_accp, "pool") if on_pool else (acc, r_wk[1], "dve")
            for typ in range(2):
                ents = [(i, b) for i, (t, b) in enumerate(_OH_ENTRIES) if t == typ]
                av = A[:, typ * 128:(typ + 1) * 128]
                tv = A[:, 512:640]
                if typ == 0:
                    S.op(eng, lambda e, av=av: e.tensor_copy(out=av, in_=negm_s), reads=[r_negm], writes=[r_A])
                else:
                    S.op(eng, lambda e, av=av: e.memset(av, 0.0), writes=[r_A])
                for (i, b) in ents:
                    if on_pool:
                        S.op("pool", lambda e, tv=tv, i=i, b=b, h=h: e.tensor_scalar(out=tv, in0=oh_s[:, i, :], scalar1=rbd[:, b, h:h + 1], scalar2=0.0, op0=ALU.mult, op1=ALU.add),
                             reads=[r_oh, r_rbd, r_A], writes=[r_A])
                        S.op("pool", lambda e, av=av, tv=tv: e.tensor_tensor(out=av, in0=av, in1=tv, op=ALU.add), reads=[r_A], writes=[r_A])
                    else:
                        S.op("dve", lambda e, av=av, i=i, b=b, h=h: e.scalar_tensor_tensor(out=av, in0=oh_s[:, i, :], scalar=rbd[:, b, h:h + 1], in1=av,
                                                                                            op0=ALU.mult, op1=ALU.add),
                             reads=[r_oh, r_rbd, r_A], writes=[r_A])
            S.op(eng, lambda e, h=h, A=A: e.tensor_copy(out=bcat[:, h, 0, :], in_=A[:, 0:256]), reads=[r_A], writes=[r_bcat], cowrite=True)
            S.op(eng, lambda e, h=h, A=A: e.tensor_tensor(out=A[:, 256:512], in0=A[:, 0:256], in1=bcat[:, h, 0, :], op=ALU.subtract),
                 reads=[r_A, r_bcat], writes=[r_A])
            S.op(eng, lambda e, h=h, A=A: e.tensor_copy(out=bcat[:, h, 1, :], in_=A[:, 256:512]), reads=[r_A], writes=[r_bcat], cowrite=True)
        if stage == 2:
            rt = sb("rt", [128, 256], F32)
            Mall = sb("Mall", [128, NT, 32], BF16)
            ustr_b = sb("ustr_b", [128, 128], BF16)
            ones_b = sb("ones_b", [128, 128], BF16)
            Lbuf = sb("Lbuf", [128, 8, 36], F32)
            r_L = [Res() for _ in range(8)]
            tball = sb("tball", [128, NT, 2, 4], F32)
            destall = sb("destall", [128, NT, 2], I32)
            srow = sb("srow_s", [128, 32], F32)
            wrt_s = sb("wrt_s", [128, 8, 36], F32)
            r_rt, r_Mall, r_ustr, r_tbk, r_desti, r_srow, r_wrt, r_tbl, r_x1b, r_y2, r_x1f = [Res() for _ in range(11)]
            S.dma("sp", lambda e: e.dma_start(out=srow[:], in_=srow_d), writes=[r_srow])
            S.dma("sp", lambda e: e.dma_start(out=wrt_s[:], in_=wrt_d.rearrange("p (k n) -> p k n", k=8)), writes=[r_wrt])
            S.dma("pool", lambda e: e.dma_start(out=ustr_b[:], in_=ustr_d), writes=[r_ustr])
            S.op("pool", lambda e: e.memset(ones_b[:], 1.0), writes=[r_ustr])
            S.dma("sp", lambda e: e.dma_start(out=tball[:], in_=tball_d.rearrange("p (t k f) -> p t k f", k=2, f=4)), writes=[r_tbk])
            def init_scratch():
                S.op("dve", lambda e: e.memset(wk[2][:], 0.0), writes=[r_wk[2]])
                zv = wk[2][:, :].bitcast(BF16)
                y2v = y2_d.rearrange("(p c) d -> p (c d)", p=128)
                for i in range(32):
                    S.dma("sp", lambda e, i=i: e.dma_start(out=y2v[:, i * 2048:(i + 1) * 2048], in_=zv), reads=[r_wk[2]], writes=[r_y2], cowrite=True)
                S.dma("sp", lambda e: e.dma_start(out=x1b_d[NTOK:NTOK + 128, :], in_=zv[:, 0:1024]), reads=[r_wk[2]], writes=[r_x1b], cowrite=True)
                tinit = wk[0][:, 0:(NSLOT // 128) * 4].rearrange("p (c f) -> p c f", f=4)
                S.op("dve", lambda e: e.memset(tinit[:, :, 0:1], float(NTOK)), writes=[r_wk[0]])
                S.op("dve", lambda e: e.memset(tinit[:, :, 1:2], 0.0), writes=[r_wk[0]])
                S.op("dve", lambda e: e.memset(tinit[:, :, 2:3], BIGIDX), writes=[r_wk[0]])
                S.op("dve", lambda e: e.memset(tinit[:, :, 3:4], 0.0), writes=[r_wk[0]])
                S.dma("sp", lambda e: e.dma_start(out=tbl_d.rearrange("(p c) f -> p c f", p=128), in_=tinit), reads=[r_wk[0]], writes=[r_tbl])
        S.dma("pool", lambda e: e.dma_start(out=wo_s[:], in_=wo_d.rearrange("p (k c) -> p k c", k=8), max_dma_last_dim=4096), writes=[r_wo])

        def carve(off, shape, dt):
            esz = 2 if dt == BF16 else 4
            n = int(np.prod(shape))
            v = arena[:, off:off + n * esz].bitcast(dt)
            if len(shape) == 2:
                v = v.rearrange("p (a b) -> p a b", a=shape[0])
            elif len(shape) == 3:
                v = v.rearrange("p (a b c) -> p a b c", a=shape[0], b=shape[1])
            return v, off + n * esz

        gin_row = prow[:, PR_G2:PR_G2 + 1024]
        bin_row = prow[:, PR_B2:PR_B2 + 1024]

        smcs = sb("smcs", [128, 16], F32)
        sml2 = sb("sml2", [128, 16], F32)
        stt2 = sb("stt2", [128, 12], F32)
        r_sml2, r_stt2 = Res(), Res()
        LNS = [(sml, r_sml, stt, r_stt), (sml2, r_sml2, stt2, r_stt2)]

        def ln_stats(src, r_src, si=0):
            sm, r_sm, sx, r_sx = LNS[si]
            S.ops("dve", [lambda e: e.bn_stats(out=sx[:, 0:6], in_=src[:, 0:512]),
                          lambda e: e.bn_stats(out=sx[:, 6:12], in_=src[:, 512:1024])], reads=(r_src if isinstance(r_src, list) else [r_src]), writes=[r_sx])
            S.op("dve", lambda e: e.bn_aggr(out=sm[:, 6:8], in_=sx[:, 0:12]), reads=[r_sx], writes=[r_sm])
            S.op("act", lambda e: e.activation(out=sm[:, 8:9], in_=sm[:, 7:8], func=AF.Ln, bias=epsc[:, 0:1], scale=1.0),
                 reads=[r_sm, r_eps], writes=[r_sm])
            S.op("act", lambda e: e.activation(out=sm[:, 8:9], in_=sm[:, 8:9], func=AF.Exp, scale=-0.5), reads=[r_sm], writes=[r_sm])
            S.op("dve", lambda e: e.tensor_scalar(out=sm[:, 9:10], in0=sm[:, 6:7], scalar1=sm[:, 8:9], scalar2=-1.0, op0=ALU.mult, op1=ALU.mult),
                 reads=[r_sm], writes=[r_sm])
            return sm, r_sm

        r_cw = [Res() for _ in range(NE)]
        conv_next = [0]

        def emit_conv(n=1):
            if stage != 2:
                return
            for _ in range(n):
                ex = conv_next[0]
                if ex >= NE:
                    return
                conv_next[0] += 1
                for (dst, src) in ((wgb_d, wg_d), (wub_d, wu_d), (wdb_d, wd_d)):
                    S.dma("pool", lambda e, dst=dst, src=src, ex=ex: e.dma_start(out=dst[ex], in_=src[ex], max_dma_last_dim=8192), writes=[r_cw[ex]], cowrite=True)

        pregs = {}

        def preg(e, val):
            if val not in pregs:
                pregs[val] = e.to_reg(val)
            return pregs[val]

        def carve_from(buf, off, shape, dt):
            esz = 2 if dt == BF16 else 4
            n = int(np.prod(shape))
            v = buf[:, off:off + n * esz].bitcast(dt)
            if len(shape) == 2:
                v = v.rearrange("p (a b) -> p a b", a=shape[0])
            return v, off + n * esz

        def moe_phase():
            emit_conv(NE)
            S.flush()
            S.barrier()
            cnt["wide"] = "moe"
            S.dma("sp", lambda e: e.dma_start(out=prow[:, PR_G2:PR_G2 + 2048], in_=prow2_d), writes=[r_prow])
            ubytes = uT[:, :, :].rearrange("p a b -> p (a b)").bitcast(mybir.dt.uint8)
            obytes = oT[:, :, :].rearrange("p a b -> p (a b)").bitcast(mybir.dt.uint8)
            off = 0
            wgb, wub, wdb = [], [], []
            for i in range(2):
                a, off = carve(off, [8, 512], BF16); wgb.append(a)
                a, off = carve(off, [8, 512], BF16); wub.append(a)
                a, off = carve(off, [4, 1024], BF16); wdb.append(a)
            XgT, hT, sg, yo = [], [], [], []
            for i in range(2):
                a, off = carve(off, [8, CAP], BF16); XgT.append(a)
                a, off = carve(off, [4, CAP], BF16); hT.append(a)
                a, off = carve(off, [CAP], F32); sg.append(a)
            assert off <= ARENA, off
            uo = 0
            xg, tbe, idxt, idxa = [], [], [], []
            for i in range(2):
                a, uo = carve_from(ubytes, uo, [NCT, 1024], BF16); xg.append(a)
            NIB = 3
            for i in range(NIB):
                a, uo = carve_from(ubytes, uo, [NCT, 4], F32); tbe.append(a)
                a, uo = carve_from(ubytes, uo, [NCT], I32); idxt.append(a)
                a, uo = carve_from(ubytes, uo, [NCT], I32); idxa.append(a)
            NYO = 4
            for i in range(NYO):
                a, uo = carve_from(ubytes, uo, [1024], BF16); yo.append(a)
            assert uo <= 32768
            r_wg, r_wu, r_wd = [Res(), Res()], [Res(), Res()], [Res(), Res()]
            r_xg = [[Res() for _ in range(NCT)] for _ in range(2)]
            r_XgT, r_hT, r_sg = [[Res(), Res()] for _ in range(3)]
            r_yo = [Res() for _ in range(NYO)]
            r_tbe, r_idt, r_ida = [[Res() for _ in range(NIB)] for _ in range(3)]
            nyo = [0]

            def load_expert(ex):
                bi = ex % 2
                ib = ex % NIB
                S.dma("sp", lambda e: e.dma_start(out=tbe[ib], in_=tbl_d[ex * CAP:(ex + 1) * CAP, :].rearrange("(c p) f -> p c f", p=128)), reads=[r_tbl], writes=[r_tbe[ib]])
                S.op("dve", lambda e: e.tensor_copy(out=idxt[ib], in_=tbe[ib][:, :, 0]), reads=[r_tbe[ib]], writes=[r_idt[ib]])
                S.op("dve", lambda e: e.tensor_copy(out=idxa[ib], in_=tbe[ib][:, :, 2]), reads=[r_tbe[ib]], writes=[r_ida[ib]])
                for c in range(NCT):
                    S.dma("pool", lambda e, c=c: e.indirect_dma_start(out=xg[bi][:, c, :], out_offset=None, in_=x1b_d[:, :],
                                                                       in_offset=bass.IndirectOffsetOnAxis(ap=idxt[ib][:, c:c + 1], axis=0)),
                          reads=[r_idt[ib], r_x1b], writes=[r_xg[bi][c]])
                S.dma("sp", lambda e: e.dma_start(out=wgb[bi], in_=wgb_d[ex].rearrange("p (k f) -> p k f", k=8)), reads=[r_cw[ex]], writes=[r_wg[bi]])
                S.dma("sp", lambda e: e.dma_start(out=wub[bi], in_=wub_d[ex].rearrange("p (k f) -> p k f", k=8)), reads=[r_cw[ex]], writes=[r_wu[bi]])
                S.dma("sp", lambda e: e.dma_start(out=wdb[bi], in_=wdb_d[ex].rearrange("p (k f) -> p k f", k=4)), reads=[r_cw[ex]], writes=[r_wd[bi]])

            def compute_expert(ex):
                bi = ex % 2
                ib = ex % NIB
                for c in range(NCT):
                    for half in range(2):
                        pt_ap, r_p = next_pt()
                        S.ops("pe", [(lambda e, k=k, c=c, pt_ap=pt_ap: e.transpose(out=pt_ap[:, (k % 4) * 128:(k % 4 + 1) * 128], in_=xg[bi][:, c, k * 128:(k + 1) * 128], identity=identb[:]))
                                     for k in range(half * 4, half * 4 + 4)], reads=[r_xg[bi][c], r_identb], writes=[r_p])
                        evac(XgT[bi][:, half * 4:half * 4 + 4, c * 128:(c + 1) * 128], pt_ap[:, 0:512].rearrange("p (a b) -> p a b", a=4), reads=[r_p], writes=[r_XgT[bi]], eng="dve")
                for fc in range(4):
                    psG, r_psG = next_pg()
                    mm(psG[:, 0:CAP], [(wgb[bi][:, k, fc * 128:(fc + 1) * 128], XgT[bi][:, k, :]) for k in range(8)], reads=[r_wg[bi], r_XgT[bi]], writes=[r_psG])
                    si = fc % 2
                    S.op("act", lambda e, psG=psG, si=si: e.activation(out=sg[si], in_=psG[:, 0:CAP], func=AF.Silu), reads=[r_psG], writes=[r_sg[si]])
                    psU, r_psU = next_pg()
                    mm(psU[:, 0:CAP], [(wub[bi][:, k, fc * 128:(fc + 1) * 128], XgT[bi][:, k, :]) for k in range(8)], reads=[r_wu[bi], r_XgT[bi]], writes=[r_psU])
                    S.op("dve", lambda e, psU=psU, fc=fc, si=si: e.tensor_tensor(out=hT[bi][:, fc, :], in0=psU[:, 0:CAP], in1=sg[si], op=ALU.mult),
                         reads=[r_psU, r_sg[si]], writes=[r_hT[bi]])
                for c in range(NCT):
                    yi = nyo[0] % NYO
                    nyo[0] += 1
                    for hh in range(2):
                        ps, r_ps = next_pg()
                        mm(ps[:, :], [(hT[bi][:, fc, c * 128:(c + 1) * 128], wdb[bi][:, fc, hh * 512:(hh + 1) * 512]) for fc in range(4)], reads=[r_hT[bi], r_wd[bi]], writes=[r_ps])
                        S.op("act", lambda e, ps=ps, hh=hh, c=c, yi=yi: e.activation(out=yo[yi][:, hh * 512:(hh + 1) * 512], in_=ps[:, :], func=AF.Identity, scale=tbe[ib][:, c, 1:2]),
                             reads=[r_ps, r_tbe[ib]], writes=[r_yo[yi]])
                    S.dma("pool", lambda e, c=c, yi=yi: e.indirect_dma_start(out=y2_d[:, :], out_offset=bass.IndirectOffsetOnAxis(ap=idxa[ib][:, c:c + 1], axis=0),
                                                                             in_=yo[yi][:, :], in_offset=None, bounds_check=preg(e, 2 * NTOK - 1), oob_is_err=False),
                          reads=[r_yo[yi], r_ida[ib]], writes=[r_y2], cowrite=True)

            if _DBG.get("nopf"):
                for ex in range(NE):
                    load_expert(ex)
                    compute_expert(ex)
            else:
                load_expert(0)
                for ex in range(NE):
                    if ex + 1 < NE:
                        load_expert(ex + 1)
                    compute_expert(ex)

            S.barrier()
            NFS = 6
            FX, FY, FT, FN = [], [], [], []
            ao, oo = 0, 0
            for i in range(NFS):
                if i < 4:
                    a, ao = carve(ao, [1024], F32); FX.append(a)
                    a, ao = carve(ao, [2, 1024], BF16); FY.append(a)
                    a, ao = carve(ao, [1024], F32); FT.append(a)
                    a, ao = carve(ao, [1024], F32); FN.append(a)
                else:
                    a, oo = carve_from(obytes, oo, [1024], F32); FX.append(a)
                    a, oo = carve_from(obytes, oo, [2, 1024], BF16); FY.append(a)
                    a, oo = carve_from(obytes, oo, [1024], F32); FT.append(a)
                    a, oo = carve_from(obytes, oo, [1024], F32); FN.append(a)
            assert ao <= ARENA and oo <= 32768
            r_FX, r_FY, r_FT, r_FN = [[Res() for _ in range(NFS)] for _ in range(4)]
            def fin_load(gT):
                row0 = gT * 128
                b = gT % NFS
                S.dma("sp", lambda e: e.dma_start(out=FX[b], in_=x1f_d[row0:row0 + 128, :]), reads=[r_x1f], writes=[r_FX[b]])
                S.dma("sp", lambda e: e.dma_start(out=FY[b], in_=y2_d[2 * row0:2 * row0 + 256, :].rearrange("(p two) d -> p two d", two=2)), reads=[r_y2], writes=[r_FY[b]])

            for gT in range(min(NFS - 1, NT)):
                fin_load(gT)

            def fin_A(gT):
                b = gT % NFS
                S.op("dve", lambda e: e.scalar_tensor_tensor(out=FT[b], in0=FX[b], scalar=ALPHA, in1=FY[b][:, 0, :], op0=ALU.mult, op1=ALU.add),
                     reads=[r_FX[b], r_FY[b]], writes=[r_FT[b]])
                S.op("dve", lambda e: e.tensor_tensor(out=FT[b], in0=FT[b], in1=FY[b][:, 1, :], op=ALU.add), reads=[r_FT[b], r_FY[b]], writes=[r_FT[b]])
                sm, r_sm, _sx, _r_sx = LNS[gT % 2]
                S.op("act", lambda e: e.activation(out=FN[b], in_=FT[b], func=AF.Copy, accum_out=sm[:, 0:1]), reads=[r_FT[b]], writes=[r_FN[b], r_sm])
                S.op("act", lambda e: e.activation(out=FN[b], in_=FT[b], func=AF.Square, accum_out=sm[:, 1:2]), reads=[r_FT[b], r_sm], writes=[r_FN[b], r_sm])
                S.op("dve", lambda e: e.tensor_scalar(out=sm[:, 6:7], in0=sm[:, 0:1], scalar1=1.0 / 1024.0, scalar2=None, op0=ALU.mult), reads=[r_sm], writes=[r_sm])
                S.op("dve", lambda e: e.tensor_tensor(out=sm[:, 2:3], in0=sm[:, 6:7], in1=sm[:, 6:7], op=ALU.mult), reads=[r_sm], writes=[r_sm])
                S.op("dve", lambda e: e.tensor_scalar(out=sm[:, 7:8], in0=sm[:, 1:2], scalar1=1.0 / 1024.0, scalar2=sm[:, 2:3], op0=ALU.mult, op1=ALU.subtract), reads=[r_sm], writes=[r_sm])
                S.op("act", lambda e: e.activation(out=sm[:, 8:9], in_=sm[:, 7:8], func=AF.Ln, bias=epsc[:, 0:1], scale=1.0), reads=[r_sm, r_eps], writes=[r_sm])
                S.op("act", lambda e: e.activation(out=sm[:, 8:9], in_=sm[:, 8:9], func=AF.Exp, scale=-0.5), reads=[r_sm], writes=[r_sm])
                S.op("dve", lambda e: e.tensor_scalar(out=sm[:, 9:10], in0=sm[:, 6:7], scalar1=sm[:, 8:9], scalar2=-1.0, op0=ALU.mult, op1=ALU.mult), reads=[r_sm], writes=[r_sm])
                return sm, r_sm

            def fin_B(gT, sm, r_sm):
                row0 = gT * 128
                b = gT % NFS
                S.op("act", lambda e: e.activation(out=FN[b], in_=FT[b], func=AF.Identity, bias=sm[:, 9:10], scale=sm[:, 8:9]),
                     reads=[r_FT[b], r_sm], writes=[r_FN[b]])
                S.op("pool", lambda e: e.tensor_tensor(out=FN[b], in0=FN[b], in1=prow[:, PR_G2:PR_G2 + 1024], op=ALU.mult), reads=[r_FN[b], r_prow], writes=[r_FN[b]])
                S.op("pool", lambda e: e.tensor_tensor(out=FN[b], in0=FN[b], in1=prow[:, PR_B2:PR_B2 + 1024], op=ALU.add), reads=[r_FN[b], r_prow], writes=[r_FN[b]])
                S.dma("sp", lambda e: e.dma_start(out=out_d[row0:row0 + 128, :], in_=FN[b]), reads=[r_FN[b]])
                if gT + NFS - 1 < NT:
                    fin_load(gT + NFS - 1)

            pend = fin_A(0)
            for gT in range(NT):
                nxt = fin_A(gT + 1) if gT + 1 < NT else None
                fin_B(gT, *pend)
                pend = nxt

        def route_tile(gT, row0, W):
            (Wu, Ru), (Wt, Rt), (Wxb, Rxb) = W["u"], W["t"], W["xb"]
            slot = gT % 8
            S.op("act", lambda e: e.activation(out=Wxb, in_=Wu, func=AF.Copy), reads=Ru, writes=Rxb)
            S.dma("sp", lambda e: e.dma_start(out=x1f_d[row0:row0 + 128, :], in_=Wu), reads=Ru, writes=[r_x1f], cowrite=True)
            S.dma("sp", lambda e: e.dma_start(out=x1b_d[row0:row0 + 128, :], in_=Wxb), reads=Rxb, writes=[r_x1b], cowrite=True)
            x1T = Wt.rearrange("p (k t) -> p k t", k=8)
            for half in range(2):
                ps, r_ps = next_pg()
                S.ops("pe", [(lambda e, c=c, ps=ps: e.transpose(out=ps[:, (c % 4) * 128:(c % 4 + 1) * 128], in_=Wu[:, c * 128:(c + 1) * 128], identity=identf[:]))
                             for c in range(half * 4, half * 4 + 4)], reads=Ru + [r_identf], writes=[r_ps])
                evac(x1T[:, half * 4:half * 4 + 4, :], ps[:, :].rearrange("p (a b) -> p a b", a=4), reads=[r_ps], writes=Rt)
            ps, r_ps = next_pg()
            mm(ps[:, 0:36], [(x1T[:, k, :], wrt_s[:, k, :]) for k in range(8)], reads=Rt + [r_wrt], writes=[r_ps])
            R = lambda a, b: rt[:, a:b]
            S.op("dve", lambda e, ps=ps: e.tensor_tensor(out=Lbuf[:, slot, :], in0=ps[:, 0:36], in1=prow[:, PR_BR:PR_BR + 36], op=ALU.add), reads=[r_ps, r_prow], writes=[r_L[slot]])
            th = []
            ctx = {}
            th.append(lambda: S.op("dve", lambda e: e.tensor_copy(out=R(0, 36), in_=Lbuf[:, slot, :]), reads=[r_L[slot], r_rt], writes=[r_rt]))

            def dv(fn, rd=(), wr=None, cw=False):
                th.append(lambda: S.op("dve", fn, reads=[r_rt] + list(rd), writes=[r_rt] if wr is None else wr, cowrite=cw))

            def ac(fn):
                th.append(lambda: S.op("act", fn, reads=[r_rt], writes=[r_rt]))

            dv(lambda e: e.tensor_reduce(out=R(37, 38), in_=R(0, 4), axis=AX.X, op=ALU.max, negate=True))
            ac(lambda e: e.activation(out=R(40, 44), in_=R(0, 4), func=AF.Exp, bias=R(37, 38), scale=1.0, accum_out=R(38, 39)))
            dv(lambda e: e.reciprocal(out=R(39, 40), in_=R(38, 39)))
            dv(lambda e: e.tensor_scalar(out=R(44, 48), in0=R(0, 4), scalar1=R(37, 38), scalar2=1.0e30, op0=ALU.add, op1=ALU.mult))
            for g in range(4):
                dv(lambda e, g=g: e.tensor_scalar(out=R(48 + g * 8, 56 + g * 8), in0=R(4 + g * 8, 12 + g * 8), scalar1=R(44 + g, 45 + g), scalar2=None, op0=ALU.add))
            dv(lambda e: e.max(out=R(80, 88), in_=R(48, 80)))
            dv(lambda e: e.tensor_tensor(out=R(88, 89), in0=R(80, 81), in1=R(81, 82), op=ALU.subtract))
            ac(lambda e: e.activation(out=R(89, 90), in_=R(88, 89), func=AF.Exp, scale=-1.0))
            dv(lambda e: e.tensor_scalar(out=R(89, 90), in0=R(89, 90), scalar1=1.0, scalar2=None, op0=ALU.add))
            dv(lambda e: e.reciprocal(out=R(89, 90), in_=R(89, 90)))
            dv(lambda e: e.tensor_tensor(out=tball[:, gT, 0, 1:2], in0=R(89, 90), in1=R(39, 40), op=ALU.mult), wr=[r_tbk], cw=True)
            dv(lambda e: e.tensor_tensor(out=tball[:, gT, 1, 1:2], in0=R(39, 40), in1=tball[:, gT, 0, 1:2], op=ALU.subtract), rd=[r_tbk], wr=[r_tbk], cw=True)
            for k in range(2):
                dv(lambda e, k=k: e.tensor_scalar(out=R(96 + 32 * k, 128 + 32 * k), in0=R(48, 80), scalar1=R(80 + k, 81 + k), scalar2=None, op0=ALU.is_equal))
            dv(lambda e: e.tensor_tensor(out=Mall[:, gT, :], in0=R(96, 128), in1=R(128, 160), op=ALU.add), wr=[r_Mall])

            def pm():
                ps2, r_ps2 = next_pg()
                ctx["ps"], ctx["r"] = ps2, r_ps2
                mm(ps2[:, 0:32], [(ustr_b[:], Mall[:, gT, :])] + [(ones_b[:], Mall[:, g2, :]) for g2 in range(gT)], reads=[r_Mall, r_ustr], writes=[r_ps2])
            th.append(pm)
            th.append(lambda: S.op("dve", lambda e: e.tensor_scalar(out=R(160, 192), in0=ctx["ps"][:, 0:32], scalar1=float(CAP), scalar2=None, op0=ALU.is_lt),
                                   reads=[ctx["r"], r_rt], writes=[r_rt]))
            th.append(lambda: S.op("dve", lambda e: e.tensor_tensor(out=R(192, 224), in0=ctx["ps"][:, 0:32], in1=srow[:, 0:32], op=ALU.add),
                                   reads=[ctx["r"], r_rt, r_srow], writes=[r_rt]))
            dv(lambda e: e.tensor_tensor(out=R(192, 224), in0=R(192, 224), in1=R(160, 192), op=ALU.mult))
            for k in range(2):
                dv(lambda e, k=k: e.tensor_tensor(out=R(224, 256), in0=R(96 + 32 * k, 128 + 32 * k), in1=R(192, 224), op=ALU.mult))
                dv(lambda e, k=k: e.tensor_reduce(out=R(92 + k, 93 + k), in_=R(224, 256), axis=AX.X, op=ALU.add))
                dv(lambda e, k=k: e.tensor_scalar(out=destall[:, gT, k:k + 1], in0=R(92 + k, 93 + k), scalar1=BIGIDX, scalar2=None, op0=ALU.add), wr=[r_desti], cw=True)
            for k in range(2):
                th.append(lambda k=k: S.dma("pool", lambda e: e.indirect_dma_start(out=tbl_d[:, :], out_offset=bass.IndirectOffsetOnAxis(ap=destall[:, gT, k:k + 1], axis=0),
                                                                                  in_=tball[:, gT, k, :], in_offset=None, bounds_check=preg(e, NSLOT - 1), oob_is_err=False),
                                            reads=[r_tbk, r_desti], writes=[r_tbl], cowrite=True))
            S.defer(th)

        for s in range(NSEQ if stop > 0 else 0):
            base = s * SEQ
            xh2 = [xh[0][:, :], wk[2][:, 0:512].bitcast(BF16)]
            r_xh2 = [r_xh[0], r_wk[2]]

            def p1_A(T):
                gT = s * 16 + T
                b = gT % 2
                r0 = base + T * 128
                S.dma("sp", lambda e: e.dma_start(out=xs[b][:], in_=x_d[r0:r0 + 128, :]), writes=[r_xs[b]])
                sm, r_sm = ln_stats(xs[b], r_xs[b], T % 2)
                S.op("dve", lambda e: e.tensor_copy(out=rstd_in[:, gT:gT + 1], in_=sm[:, 8:9]), reads=[r_sm], writes=[r_mvs[gT]])
                S.op("dve", lambda e: e.tensor_copy(out=nmr_in[:, gT:gT + 1], in_=sm[:, 9:10]), reads=[r_sm], writes=[r_mvs[gT]])
                S.op("act", lambda e: e.activation(out=xh2[b], in_=xs[b][:], func=AF.Identity, bias=nmr_in[:, gT:gT + 1], scale=rstd_in[:, gT:gT + 1]),
                     reads=[r_xs[b], r_mvs[gT]], writes=[r_xh2[b]])

            def p1_B(T):
                gT = s * 16 + T
                b = gT % 2
                for half in range(2):
                    pt_ap, r_p = next_pt()
                    S.ops("pe", [(lambda e, c=c, pt_ap=pt_ap: e.transpose(out=pt_ap[:, (c % 4) * 128:(c % 4 + 1) * 128], in_=xh2[b][:, c * 128:(c + 1) * 128], identity=identb[:]))
                                 for c in range(half * 4, half * 4 + 4)], reads=[r_xh2[b], r_identb], writes=[r_p])
                    for c in range(half * 4, half * 4 + 4):
                        src = pt_ap[:, (c % 4) * 128:(c % 4 + 1) * 128]
                        dst = uT[:, c, T * 128:(T + 1) * 128]
                        if half == 0:
                            S.op("act", lambda e, c=c, src=src, dst=dst: e.activation(out=dst, in_=src, func=AF.Identity, bias=pcols[:, PC_BIN + c:PC_BIN + c + 1],
                                                                                       scale=pcols[:, PC_GIN + c:PC_GIN + c + 1]),
                                 reads=[r_p, r_pcols], writes=[r_uT[2 * T + half]])
                        else:
                            S.op("dve", lambda e, c=c, src=src, dst=dst: e.tensor_scalar(out=dst, in0=src, scalar1=pcols[:, PC_GIN + c:PC_GIN + c + 1],
                                                                                          scalar2=pcols[:, PC_BIN + c:PC_BIN + c + 1], op0=ALU.mult, op1=ALU.add),
                                 reads=[r_p, r_pcols], writes=[r_uT[2 * T + half]])

            p1_A(0)
            for T in range(16):
                if T + 1 < 16:
                    p1_A(T + 1)
                p1_B(T)

            if stop < 2:
                continue
            S.barrier()
            cnt["wide"] = False
            if stage == 2 and s == 0:
                init_scratch()
            off = 0
            wqkv, off = carve(off, [3, 8, 256], BF16)
            qT, off = carve(off, [2, SEQ], BF16)
            kT, off = carve(off, [2, SEQ], BF16)
            Vh, off = carve(off, [16, 260], BF16)
            PT0, off = carve(off, [16, 512], BF16)
            PT1, off = carve(off, [16, 512], BF16)
            PT = [PT0, PT1]
            o1s, o2s, onbs = [], [], []
            for _i in range(2):
                a, off = carve(off, [258], F32); o1s.append(a)
                a, off = carve(off, [258], F32); o2s.append(a)
            for _i in range(3):
                a, off = carve(off, [256], BF16); onbs.append(a)
            assert off <= ARENA, off
            r_o1s, r_o2s, r_smcs = [[Res(), Res()] for _ in range(3)]
            r_onbs = [Res(), Res(), Res()]
            cnt["cmb"] = 0
            pend_tr = []
            r_wseg, r_q, r_k = [Res(), Res(), Res()], Res(), Res()
            r_V = [Res() for _ in range(16)]
            r_PT = [[Res() for _ in range(16)] for _ in range(2)]
            S.op("pool", lambda e: e.memset(Vh[:, :, 256:257], 1.0), writes=r_V)

            def load_wseg(h, si):
                src = wqkv_d[h].rearrange("p (s k c) -> p s k c", s=3, k=8)
                S.dma("pool", lambda e: e.dma_start(out=wqkv[:, si, :, :], in_=src[:, si, :, :], max_dma_last_dim=8192), writes=[r_wseg[si]])

            for si in range(3):
                load_wseg(0, si)
            for h in range(H):
                for (dst, si, r_dst) in ((qT, 0, r_q), (kT, 1, r_k)):
                    for m in range(2):
                        for tg in range(4):
                            ps, r_ps = next_pg()
                            mm(ps[:, :], [(wqkv[:, si, k, m * 128:(m + 1) * 128], uT[:, k, tg * 512:(tg + 1) * 512]) for k in range(8)],
                               reads=[r_wseg[si]] + r_uT[tg * 8:tg * 8 + 8], writes=[r_ps])
                            evac(dst[:, m, tg * 512:(tg + 1) * 512], ps[:, :], reads=[r_ps], writes=[r_dst], )
                    if h + 1 < H:
                        load_wseg(h + 1, si)
                for T in range(16):
                    ps, r_ps = next_pg()
                    mm(ps[:, 0:256], [(uT[:, k, T * 128:(T + 1) * 128], wqkv[:, 2, k, :]) for k in range(8)], reads=[r_wseg[2], r_uT[2 * T], r_uT[2 * T + 1]], writes=[r_ps])
                    evac(Vh[:, T, 0:256], ps[:, 0:256], reads=[r_ps], writes=[r_V[T]])
                if h + 1 < H:
                    load_wseg(h + 1, 2)
                for g in range(4):
                    nk = 4 * g + 4
                    emit_conv(1)
                    for m in range(2):
                        for j in range(nk):
                            c0 = max(0, j - 4 * g) * 128
                            ps, r_ps = next_pg()
                            fns = [lambda e, ps=ps, j=j, c0=c0, m=m, g=g: e.matmul(ps[:, c0:512], lhsT=kT[:, m, j * 128:(j + 1) * 128],
                                                                                 rhs=qT[:, m, g * 512 + c0:(g + 1) * 512], start=True, stop=False)]
                            lo_q = j - 4 * g
                            if lo_q >= 0:
                                ncol = 256 if lo_q <= 2 else 128
                                cs, bs = lo_q * 128, 0
                            else:
                                ncol, cs, bs = (128, 0, 128) if lo_q == -1 else (0, 0, 0)
                            if ncol > 0:
                                for hl in range(2):
                                    fns.append(lambda e, ps=ps, cs=cs, ncol=ncol, bs=bs, hl=hl, h=h: e.matmul(ps[:, cs:cs + ncol], lhsT=identb[:], rhs=bcat[:, h, hl, bs:bs + ncol],
                                                                                                         start=False, stop=(hl == 1)))
                            else:
                                fns[0] = lambda e, ps=ps, j=j, c0=c0, m=m, g=g: e.matmul(ps[:, c0:512], lhsT=kT[:, m, j * 128:(j + 1) * 128],
                                                                                        rhs=qT[:, m, g * 512 + c0:(g + 1) * 512], start=True, stop=True)
                            S.ops("pe", fns, reads=[r_k, r_q, r_bcat, r_identb], writes=[r_ps])
                            S.op("act", lambda e, ps=ps, m=m, j=j, c0=c0: e.activation(out=PT[m][:, j, c0:512], in_=ps[:, c0:512], func=AF.Exp, scale=SCALE),
                                 reads=[r_ps], writes=[r_PT[m][j]])
                    for li in range(4):
                        i = 4 * g + li
                        ovs = []
                        for m in range(2):
                            pvb, r_pvb = next_pv()
                            mm(pvb[:, 0:257], [(PT[m][:, j, li * 128:(li + 1) * 128], Vh[:, j, 0:257]) for j in range(i + 1)],
                               reads=r_PT[m][0:i + 1] + r_V[0:i + 1], writes=[r_pvb])
                            ovs.append((pvb, r_pvb))
                        (o1, r_o1), (o2, r_o2) = ovs
                        while len(pend_tr) > 1:
                            pend_tr.pop(0)()
                        cs = cnt["cmb"] % 2
                        cnt["cmb"] += 1
                        c3 = (cnt["cmb"] - 1) % 3
                        a1, a2, onb, smc = o1s[cs], o2s[cs], onbs[c3], smcs[:, cs * 8:cs * 8 + 8]
                        r_a1, r_a2, r_onb, r_smc = r_o1s[cs], r_o2s[cs], r_onbs[c3], r_smcs[cs]
                        S.op("dve", lambda e, o1=o1, a1=a1: e.tensor_copy(out=a1[:, 0:257], in_=o1[:, 0:257]), reads=[r_o1], writes=[r_a1])
                        S.op("dve", lambda e, o2=o2, a2=a2: e.tensor_copy(out=a2[:, 0:257], in_=o2[:, 0:257]), reads=[r_o2], writes=[r_a2])
                        S.op("dve", lambda e, a1=a1, smc=smc: e.reciprocal(out=smc[:, 0:1], in_=a1[:, 256:257]), reads=[r_a1], writes=[r_smc])
                        S.op("dve", lambda e, a2=a2, smc=smc: e.reciprocal(out=smc[:, 1:2], in_=a2[:, 256:257]), reads=[r_a2, r_smc], writes=[r_smc])
                        S.op("dve", lambda e, smc=smc: e.tensor_tensor(out=smc[:, 1:2], in0=smc[:, 1:2], in1=lamc[:, 1:2], op=ALU.mult), reads=[r_smc, r_lamc], writes=[r_smc])
                        S.op("dve", lambda e, a1=a1, smc=smc: e.tensor_scalar(out=a1[:, 0:256], in0=a1[:, 0:256], scalar1=smc[:, 0:1], scalar2=None, op0=ALU.mult), reads=[r_a1, r_smc], writes=[r_a1])
                        S.op("dve", lambda e, a1=a1, a2=a2, smc=smc: e.scalar_tensor_tensor(out=a2[:, 0:256], in0=a2[:, 0:256], scalar=smc[:, 1:2], in1=a1[:, 0:256], op0=ALU.mult, op1=ALU.add),
                             reads=[r_a2, r_smc, r_a1], writes=[r_a2])
                        S.op("dve", lambda e, a1=a1, a2=a2, smc=smc: e.scalar_tensor_tensor(out=a1[:, 0:256], in0=a2[:, 0:256], scalar=1.0, in1=a2[:, 0:256], op0=ALU.mult, op1=ALU.mult,
                                                                                            accum_out=smc[:, 2:3]), reads=[r_a2], writes=[r_a1, r_smc])
                        S.op("act", lambda e, smc=smc: e.activation(out=smc[:, 3:4], in_=smc[:, 2:3], func=AF.Ln, bias=epsc[:, 1:2], scale=1.0 / 256.0),
                             reads=[r_smc, r_eps], writes=[r_smc])
                        S.op("act", lambda e, smc=smc: e.activation(out=smc[:, 3:4], in_=smc[:, 3:4], func=AF.Exp, scale=-0.5), reads=[r_smc], writes=[r_smc])
                        S.op("dve", lambda e, a2=a2, onb=onb, smc=smc: e.scalar_tensor_tensor(out=onb, in0=a2[:, 0:256], scalar=smc[:, 3:4], in1=subg[:], op0=ALU.mult, op1=ALU.mult),
                             reads=[r_a2, r_smc, r_subg], writes=[r_onb])

                        def tr_out(i=i, h=h, onb=onb, r_onb=r_onb):
                            pt_ap, r_p = next_pt()
                            S.ops("pe", [(lambda e, cc=cc, pt_ap=pt_ap, onb=onb: e.transpose(out=pt_ap[:, cc * 128:(cc + 1) * 128], in_=onb[:, cc * 128:(cc + 1) * 128], identity=identb[:]))
                                         for cc in range(2)], reads=[r_onb, r_identb], writes=[r_p])
                            evac(oT[:, 2 * h:2 * h + 2, i * 128:(i + 1) * 128], pt_ap[:, 0:256].rearrange("p (a b) -> p a b", a=2), reads=[r_p], writes=[r_oT[i]], )
                        pend_tr.append(tr_out)

            while pend_tr:
                pend_tr.pop(0)()
            if stage == 3 and s == 0:
                S.dma("pool", lambda e: e.dma_start(out=out_d[0:2048, :].rearrange("(c p two) d -> p c (two d)", c=8, two=2), in_=oT[:, :, :]), reads=r_oT)
                S.dma("pool", lambda e: e.dma_start(out=out_d[2048:4096, :].rearrange("(c p two) d -> p c (two d)", c=8, two=2), in_=uT[:, :, :]), reads=r_uT)
                break
            if stop < 3:
                continue
            S.barrier()
            cnt["wide"] = "p3"
            off = 0
            wr2, wa2, w22 = [], [], []
            for _i in range(2):
                a, off = carve(off, [4, 8, 128], BF16); wr2.append(a)
                a, off = carve(off, [8, 128], BF16); wa2.append(a)
                a, off = carve(off, [2, 8, 128], BF16); w22.append(a)
            ybuf, off = carve(off, [1026], F32)
            cbs, off = carve(off, [1024], BF16)
            sa, off = carve(off, [512], F32)
            tmp, off = carve(off, [512], F32)
            zb, off = carve(off, [1024], F32)
            off_cT = off
            cT, off = carve(off, [8, 1024], BF16)
            mT, off = carve(off, [8, 1024], BF16)
            assert off <= ARENA, off
            r_wr2, r_wa2, r_w22 = [Res(), Res()], [Res(), Res()], [[Res(), Res()], [Res(), Res()]]
            r_y, r_cbs, r_sa, r_tmp, r_z = [Res() for _ in range(5)]
            r_cT = [Res() for _ in range(8)]
            r_mT = [Res() for _ in range(8)]

            def load_w1(n):
                j, bi = n % 8, n % 2
                src = wr_d[j].rearrange("p (s k c) -> p s k c", s=5, k=8)
                S.dma("pool", lambda e: e.dma_start(out=wr2[bi], in_=src[:, 0:4, :, :], max_dma_last_dim=8192), writes=[r_wr2[bi]])
                S.dma("pool", lambda e: e.dma_start(out=wa2[bi], in_=wa_d[j].rearrange("p (k c) -> p k c", k=8)), writes=[r_wa2[bi]])

            def load_w2(n):
                j, bi = n % 8, n % 2
                src = wr_d[j].rearrange("p (s k c) -> p s k c", s=5, k=8)
                S.dma("pool", lambda e: e.dma_start(out=w22[bi][:, 0, :, :], in_=src[:, 4, :, :]), writes=[r_w22[bi][0]])
                S.dma("pool", lambda e: e.dma_start(out=w22[bi][:, 1, :, :], in_=wb_d[j].rearrange("p (k c) -> p k c", k=8)), writes=[r_w22[bi][1]])

            load_w1(0)
            for hs in range(2):
                h0 = hs * 1024
                for j in range(8):
                    n1 = hs * 8 + j
                    bi = n1 % 2
                    wr, wa, r_wr, r_wa = wr2[bi], wa2[bi], r_wr2[bi], r_wa2[bi]
                    if j + 1 < 8:
                        load_w1(n1 + 1)
                    else:
                        load_w2(hs * 8)
                    if hs == 0:
                        S.op("dve", lambda e: e.memset(ybuf[:, 0:2], 0.0), writes=[r_y])
                    else:
                        S.op("dve", lambda e, j=j: e.tensor_copy(out=ybuf[:, 0:2], in_=halo[:, j, :]), reads=[r_halo], writes=[r_y])
                    for tg2 in range(2):
                        t0 = h0 + tg2 * 512
                        rU = r_uT[t0 // 64:t0 // 64 + 8]
                        l0 = tg2 * 512

                        def proj(si):
                            ps, r_ps = next_pg()
                            mm(ps[:, :], [(wr[:, si, k, :], uT[:, k, t0:t0 + 512]) for k in range(8)], reads=[r_wr] + rU, writes=[r_ps])
                            return ps, r_ps
                        pc, r_pc = proj(1)
                        S.op("act", lambda e, pc=pc: e.activation(out=tmp, in_=pc[:, :], func=AF.Copy), reads=[r_pc], writes=[r_tmp])
                        ph, r_ph = proj(2)
                        S.op("dve", lambda e, ph=ph, l0=l0: e.tensor_tensor(out=ybuf[:, 2 + l0:2 + l0 + 512], in0=ph[:, :], in1=tmp, op=ALU.mult),
                             reads=[r_ph, r_tmp], writes=[r_y])
                        pb, r_pb = proj(0)
                        S.op("act", lambda e, pb=pb, l0=l0: e.activation(out=cbs[:, l0:l0 + 512], in_=pb[:, :], func=AF.Copy), reads=[r_pb], writes=[r_cbs])
                        pa, r_pa = proj(3)
                        S.op("act", lambda e, pa=pa, j=j: e.activation(out=sa, in_=pa[:, :], func=AF.Sigmoid, bias=pcols[:, PC_BGA + j:PC_BGA + j + 1], scale=1.0),
                             reads=[r_pa, r_pcols], writes=[r_sa])
                        pya, r_pya = next_pg()
                        mm(pya[:, :], [(wa[:, k, :], oT[:, k, t0:t0 + 512]) for k in range(8)], reads=[r_wa] + r_oT[t0 // 128:t0 // 128 + 4], writes=[r_pya])
                        S.op("dve", lambda e, pya=pya, j=j, l0=l0: e.tensor_tensor(out=mT[:, j, l0:l0 + 512], in0=pya[:, :], in1=sa, op=ALU.mult),
                             reads=[r_pya, r_sa], writes=[r_mT[j]])
                    cw = lambda w, j=j: pcols[:, PC_CW + w * 8 + j:PC_CW + w * 8 + j + 1]
                    S.op("dve", lambda e, cw=cw: e.tensor_scalar(out=zb, in0=ybuf[:, 2:1026], scalar1=cw(2), scalar2=None, op0=ALU.mult), reads=[r_y, r_pcols], writes=[r_z])
                    S.op("dve", lambda e, cw=cw: e.scalar_tensor_tensor(out=zb, in0=ybuf[:, 1:1025], scalar=cw(1), in1=zb, op0=ALU.mult, op1=ALU.add),
                         reads=[r_y, r_z, r_pcols], writes=[r_z])
                    S.op("dve", lambda e, cw=cw: e.scalar_tensor_tensor(out=zb, in0=ybuf[:, 0:1024], scalar=cw(0), in1=zb, op0=ALU.mult, op1=ALU.add),
                         reads=[r_y, r_z, r_pcols], writes=[r_z])
                    S.op("pool", lambda e, j=j: e.tensor_tensor(out=cT[:, j, :], in0=zb, in1=cbs, op=ALU.mult), reads=[r_z, r_cbs], writes=[r_cT[j]])
                    if hs == 0:
                        S.op("dve", lambda e, j=j: e.tensor_copy(out=halo[:, j, :], in_=ybuf[:, 1024:1026]), reads=[r_y], writes=[r_halo])
                for j in range(8):
                    n2 = hs * 8 + j
                    bi = n2 % 2
                    w2, r_w2 = w22[bi], r_w22[bi]
                    if j + 1 < 8:
                        load_w2(n2 + 1)
                    elif hs == 0:
                        load_w1(8)
                    for tg2 in range(2):
                        l0 = tg2 * 512
                        t0 = h0 + l0
                        pgb, r_pgb = next_pg()
                        mm(pgb[:, :], [(w2[:, 0, k, :], uT[:, k, t0:t0 + 512]) for k in range(8)], reads=[r_w2[0]] + r_uT[t0 // 64:t0 // 64 + 8], writes=[r_pgb])
                        S.op("act", lambda e, pgb=pgb, j=j: e.activation(out=sa, in_=pgb[:, :], func=AF.Sigmoid, bias=pcols[:, PC_BGB + j:PC_BGB + j + 1], scale=1.0),
                             reads=[r_pgb, r_pcols], writes=[r_sa])
                        pyb, r_pyb = next_pg()
                        mm(pyb[:, :], [(w2[:, 1, k, :], cT[:, k, l0:l0 + 512]) for k in range(8)], reads=[r_w2[1]] + r_cT, writes=[r_pyb])
                        S.op("dve", lambda e, pyb=pyb: e.tensor_tensor(out=tmp, in0=pyb[:, :], in1=sa, op=ALU.mult), reads=[r_pyb, r_sa], writes=[r_tmp])
                        S.op("pool", lambda e, j=j, l0=l0: e.tensor_tensor(out=mT[:, j, l0:l0 + 512], in0=mT[:, j, l0:l0 + 512], in1=tmp, op=ALU.add),
                             reads=[r_tmp, r_mT[j]], writes=[r_mT[j]])
                if stage == 4 and s == 0 and hs == 0:
                    for ii, (buf, rr) in enumerate(((mT, r_mT), (cT, r_cT))):
                        S.dma("pool", lambda e, ii=ii, buf=buf: e.dma_start(out=out_d[ii * 1024:(ii + 1) * 1024, :].rearrange("(c p) d -> p c d", c=8), in_=buf), reads=rr)
                    break
                if stage == 5 and hs == 1:
                    break
                WS = [{"u": (wk[0][:, :], [r_wk[0]]), "t": (wk[1][:, :], [r_wk[1]]), "n": (wk[2][:, :], [r_wk[2]]), "xb": (xh[0][:, :], [r_xh[0]])},
                      {"u": (arena[:, off_cT:off_cT + 4096].bitcast(F32), r_cT[0:2]), "t": (arena[:, off_cT + 4096:off_cT + 8192].bitcast(F32), r_cT[2:4]),
                       "n": (arena[:, off_cT + 8192:off_cT + 12288].bitcast(F32), r_cT[4:6]), "xb": (arena[:, off_cT + 12288:off_cT + 14336].bitcast(BF16), r_cT[6:7])}]
                def ln1_A(tl):
                    T = hs * 8 + tl
                    gT = s * 16 + T
                    row0 = base + T * 128
                    b = gT % 2
                    W = WS[tl % 2] if stage == 2 else WS[0]
                    (Wu, Ru), (Wt, Rt), (Wn, Rn) = W["u"], W["t"], W["n"]
                    if tl == 0:
                        S.dma("sp", lambda e: e.dma_start(out=xs[b][:], in_=x_d[row0:row0 + 128, :]), writes=[r_xs[b]])
                    if tl + 1 < 8:
                        S.dma("sp", lambda e: e.dma_start(out=xs[1 - b][:], in_=x_d[row0 + 128:row0 + 256, :]), writes=[r_xs[1 - b]])
                    S.op("act", lambda e: e.activation(out=Wu, in_=xs[b][:], func=AF.Identity, bias=nmr_in[:, gT:gT + 1], scale=rstd_in[:, gT:gT + 1]),
                         reads=[r_xs[b], r_mvs[gT]], writes=Ru)
                    S.op("pool", lambda e: e.tensor_tensor(out=Wu, in0=Wu, in1=gin_row, op=ALU.mult), reads=Ru + [r_prow], writes=Ru)
                    S.op("pool", lambda e: e.tensor_tensor(out=Wu, in0=Wu, in1=bin_row, op=ALU.add), reads=Ru + [r_prow], writes=Ru)

                def ln1_A2(tl):
                    W = WS[tl % 2] if stage == 2 else WS[0]
                    (Wu, Ru), (Wt, Rt), (Wn, Rn) = W["u"], W["t"], W["n"]
                    for hh in range(2):
                        ps, r_ps = next_pg()
                        mm(ps[:, :], [(mT[:, k, tl * 128:(tl + 1) * 128], wo_s[:, k, hh * 512:(hh + 1) * 512]) for k in range(8)], reads=r_mT + [r_wo], writes=[r_ps])
                        S.op("dve", lambda e, ps=ps, hh=hh: e.scalar_tensor_tensor(out=Wt[:, hh * 512:(hh + 1) * 512], in0=Wu[:, hh * 512:(hh + 1) * 512], scalar=ALPHA,
                                                                                   in1=ps[:, :], op0=ALU.mult, op1=ALU.add),
                             reads=[r_ps] + Ru, writes=Rt)
                    return ln_stats(Wt, Rt, tl % 2)

                def ln1_B(tl, sm, r_sm):
                    T = hs * 8 + tl
                    gT = s * 16 + T
                    row0 = base + T * 128
                    W = WS[tl % 2] if stage == 2 else WS[0]
                    (Wu, Ru), (Wt, Rt), (Wn, Rn) = W["u"], W["t"], W["n"]
                    S.op("act", lambda e: e.activation(out=Wn, in_=Wt, func=AF.Identity, bias=sm[:, 9:10], scale=sm[:, 8:9]),
                         reads=Rt + [r_sm], writes=Rn)
                    S.op("pool", lambda e: e.tensor_tensor(out=Wn, in0=Wn, in1=prow[:, PR_G1:PR_G1 + 1024], op=ALU.mult), reads=Rn + [r_prow], writes=Rn)
                    S.op("dve", lambda e: e.tensor_tensor(out=Wu, in0=Wn, in1=prow[:, PR_B1:PR_B1 + 1024], op=ALU.add), reads=Rn + [r_prow], writes=Ru)
                    if stage == 6:
                        S.dma("sp", lambda e: e.dma_start(out=out_d[row0:row0 + 128, :], in_=wk[1][:]), reads=[r_wk[1]])
                    elif stage == 1:
                        S.dma("sp", lambda e: e.dma_start(out=out_d[row0:row0 + 128, :], in_=wk[0][:]), reads=[r_wk[0]])
                    else:
                        route_tile(gT, row0, W)

                if stage == 2:
                    ln1_A(0)
                    for tl in range(8):
                        if tl + 1 < 8:
                            ln1_A(tl + 1)
                        pend = ln1_A2(tl)
                        ln1_B(tl, *pend)
                else:
                    for tl in range(8):
                        ln1_A(tl)
                        pend = ln1_A2(tl)
                        if stage == 5:
                            S.dma("sp", lambda e: e.dma_start(out=out_d[0:128, :], in_=wk[0][:]), reads=[r_wk[0]])
                            S.dma("sp", lambda e: e.dma_start(out=out_d[128:256, :], in_=wk[1][:]), reads=[r_wk[1]])
                            break
                        ln1_B(tl, *pend)
            if stage in (4, 5):
                break
            S.barrier()

        if stage == 2:
            moe_phase()
        S.finish()
        S.emit()
    return nc


_PROG = {}


def _tile_w(w, ncol_blocks, cb):
    return np.ascontiguousarray(w.reshape(8, 128, ncol_blocks, cb).transpose(2, 1, 0, 3).reshape(ncol_blocks, 128, 8 * cb))


def _cols(v):
    return np.ascontiguousarray(np.asarray(v, np.float32).reshape(8, 128).T)


def kernel(stage=2, **inp):
    f = lambda a: np.asarray(a, dtype=np.float32)
    x = f(inp["x"])
    w_in = f(inp["w_in"])[0]
    wq = _tile_w(w_in[:, 0:1024], 4, 256).reshape(4, 128, 8, 256)
    wk_ = _tile_w(w_in[:, 1024:2048], 4, 256).reshape(4, 128, 8, 256)
    wv = _tile_w(w_in[:, 2048:3072], 4, 256).reshape(4, 128, 8, 256)
    wqkv = np.ascontiguousarray(np.stack([wq, wk_, wv], axis=2).reshape(4, 128, 3 * 8 * 256))
    wr5 = [_tile_w(w_in[:, 3072 + si * 1024:3072 + (si + 1) * 1024], 8, 128).reshape(8, 128, 8, 128) for si in range(5)]
    wr = np.ascontiguousarray(np.stack(wr5, axis=2).reshape(8, 128, 5 * 8 * 128))
    wa = _tile_w(f(inp["w_a_proj"])[0], 8, 128)
    wb = _tile_w(f(inp["w_b_proj"])[0], 8, 128)
    wo = _tile_w(f(inp["w_o"])[0], 1, 1024)[0]
    pcols = np.zeros((128, PC_N), np.float32)
    pcols[:, PC_GIN:PC_GIN + 8] = _cols(inp["ln_in_g"])
    pcols[:, PC_BIN:PC_BIN + 8] = _cols(inp["ln_in_b"])
    pcols[:, PC_BGA:PC_BGA + 8] = _cols(f(inp["b_gate"])[0, 0])
    pcols[:, PC_BGB:PC_BGB + 8] = _cols(f(inp["b_gate"])[0, 1])
    cw = f(inp["conv_w"])[0, :, 0, :]
    for w in range(3):
        pcols[:, PC_CW + w * 8:PC_CW + (w + 1) * 8] = _cols(cw[w])
    prow1 = np.zeros((PR_N,), np.float32)
    prow1[PR_G1:PR_G1 + 1024] = f(inp["ln1_g"])[0]
    prow1[PR_B1:PR_B1 + 1024] = f(inp["ln1_b"])[0]
    prow1[PR_SUBG:PR_SUBG + 256] = f(inp["subln_g"])[0]
    prow1[PR_G2:PR_G2 + 1024] = f(inp["ln_in_g"])
    prow1[PR_B2:PR_B2 + 1024] = f(inp["ln_in_b"])
    prow1[PR_BR:PR_BR + 4] = f(inp["b_group"])[0]
    prow1[PR_BR + 4:PR_BR + 36] = f(inp["b_sub"])[0].reshape(-1)
    prow = np.ascontiguousarray(np.broadcast_to(prow1[None, :], (128, PR_N)))
    prs1 = np.zeros((PS_N,), np.float32)
    prs1[PS_LQ:PS_LQ + 256] = f(inp["lambda_q"])[0].reshape(-1)
    prs1[PS_LK:PS_LK + 256] = f(inp["lambda_k"])[0].reshape(-1)
    prs1[PS_RB:PS_RB + 128] = f(inp["rel_bias"]).reshape(-1)
    prows = np.ascontiguousarray(np.broadcast_to(prs1[None, :], (128, PS_N)))
    ident = np.eye(128, dtype=np.float32)
    shared = {"wqkv": wqkv, "wr": wr, "wa": wa, "wb": wb, "wo": wo, "pcols": pcols, "prow": prow, "prows": prows, "oh": _OH_NP, "negm": _NEGM_NP,
              "ident": ident}
    if stage == 2:
        prow2 = np.ascontiguousarray(np.broadcast_to(np.concatenate([f(inp["ln2_g"])[0], f(inp["ln2_b"])[0]])[None, :], (128, 2048)))
        srow = np.ascontiguousarray(np.broadcast_to((np.arange(32, dtype=np.float32) * CAP - BIGIDX)[None, :], (128, 32)))
        tb0 = np.zeros((128, NT, 2, 4), np.float32)
        tokid = np.arange(NT, dtype=np.float32)[None, :] * 128 + np.arange(128, dtype=np.float32)[:, None]
        for k in range(2):
            tb0[:, :, k, 0] = tokid
            tb0[:, :, k, 2] = 2 * tokid + k
        ustr = np.triu(np.ones((128, 128), np.float32), 1)
        wrf = np.concatenate([f(inp["w_group"])[0]] + [f(inp["w_sub"])[0, g] for g in range(4)], axis=1)
        wrt = np.ascontiguousarray(wrf.reshape(8, 128, 36).transpose(1, 0, 2).reshape(128, 8 * 36))
        wg = np.ascontiguousarray(f(inp["w_gate_e"])[0].reshape(NE, 8, 128, 512).transpose(0, 2, 1, 3).reshape(NE, 128, 8 * 512))
        wu = np.ascontiguousarray(f(inp["w_up_e"])[0].reshape(NE, 8, 128, 512).transpose(0, 2, 1, 3).reshape(NE, 128, 8 * 512))
        wd = np.ascontiguousarray(f(inp["w_down_e"])[0].reshape(NE, 4, 128, 1024).transpose(0, 2, 1, 3).reshape(NE, 128, 4 * 1024))
        shared.update({"prow2": prow2, "srow": srow, "tball0": np.ascontiguousarray(tb0.reshape(128, NT * 8)), "ustr": ustr, "wrt": wrt, "wg": wg, "wu": wu, "wd": wd})
    if stage not in _PROG:
        _PROG[stage] = build_program(stage)
    nc = _PROG[stage]
    xr = x.reshape(NCORES, NTOK, D)
    in_maps = [dict(shared, x=np.ascontiguousarray(xr[c])) for c in range(NCORES)]
    res = run_bass_kernel_spmd(nc, in_maps, core_ids=list(range(NCORES)))
    out = np.stack([np.asarray(r["out"], np.float32) for r in res.results], axis=0)
    return out.reshape(16, SEQ, D)
```

```python
import contextlib
import math
import numpy as np
import concourse.bass as bass
import concourse.mybir as mybir
from concourse.bass_utils import run_bass_kernel_spmd

F32 = mybir.dt.float32
BF16 = mybir.dt.bfloat16
I32 = mybir.dt.int32
AF = mybir.ActivationFunctionType
ALU = mybir.AluOpType
AX = mybir.AxisListType

SAME_ENGINE_SYNC = True
NCORES = 8
D = 1024
SEQ = 2048
NSEQ = 2
NTOK = NSEQ * SEQ
NT = NTOK // 128
H = 4
LN_EPS = 1e-5
RMS_EPS = 1e-6
ALPHA = 2.0 ** 0.25
LAM_INIT = 0.2
SCALE = 128 ** -0.5
NE = 32
CAP = 384
NCT = CAP // 128
NSLOT = NE * CAP
BIGIDX = 1.0e6


class Res:
    __slots__ = ("w", "w0", "r", "excl")

    def __init__(self, excl=False):
        self.w = {}
        self.w0 = {}
        self.r = {}
        self.excl = excl


class Sched:
    ENG = ("pe", "dve", "act", "pool", "sp")

    def __init__(self, nc, stack, n_dma_sems=40):
        self.nc = nc
        self.q = {e: [] for e in self.ENG}
        self.sems = {}
        self.cnt = {}
        for e in self.ENG:
            self.sems[e] = stack.enter_context(nc.semaphore("s_" + e))
            self.cnt[e] = 0
        self.dma_keys = []
        for i in range(n_dma_sems):
            k = "d%d" % i
            self.sems[k] = stack.enter_context(nc.semaphore("s_" + k))
            self.cnt[k] = 0
            self.dma_keys.append(k)
        self.dma_rr = 0
        self.dma_rr_sw = 0
        self.waited = {}
        self.nops = 0
        self.deferred = []
        self.pumping = False

    def _wait(self, e, key, val):
        if key == e and not SAME_ENGINE_SYNC:
            return
        if self.waited.get((e, key), 0) >= val:
            return
        self.waited[(e, key)] = val
        sem = self.sems[key]
        self.q[e].append(lambda eng, sem=sem, val=val: eng.wait_ge(sem, val))

    def _deps(self, e, reads, writes, cowrite):
        for r in reads:
            for k, v in r.w.items():
                self._wait(e, k, v)
        for w in writes:
            src = w.w0 if cowrite else w.w
            for k, v in src.items():
                self._wait(e, k, v)
            for k, v in w.r.items():
                self._wait(e, k, v)

    def _record(self, key, val, reads, writes, cowrite):
        for r in reads:
            if r.r.get(key, 0) < val:
                r.r[key] = val
        for w in writes:
            if cowrite:
                w.w[key] = max(w.w.get(key, 0), val)
            else:
                w.w = {key: val}
                w.w0 = {key: val}
                w.r = {}

    @staticmethod
    def _split(reads, writes, cowrite):
        ex = [r for r in reads if r.excl]
        if ex:
            assert not cowrite
            reads = [r for r in reads if not r.excl]
            writes = list(writes) + ex
        return reads, writes

    def ops(self, e, fns, reads=(), writes=(), cowrite=False):
        reads, writes = self._split(reads, writes, cowrite)
        self._deps(e, reads, writes, cowrite)
        self.cnt[e] += 1
        val = self.cnt[e]
        sem = self.sems[e]
        for fn in fns[:-1]:
            self.q[e].append(lambda eng, fn=fn: fn(eng))
        fn = fns[-1]
        self.q[e].append(lambda eng, fn=fn, sem=sem: fn(eng).then_inc(sem, 1))
        self._record(e, val, reads, writes, cowrite)
        self.nops += len(fns)
        self._autopump()

    def op(self, e, fn, reads=(), writes=(), cowrite=False):
        self.ops(e, [fn], reads, writes, cowrite)

    def dma(self, e, fn, reads=(), writes=(), cowrite=False):
        half = len(self.dma_keys) // 2
        if e == "pool":
            k = self.dma_keys[half + self.dma_rr_sw % half]
            self.dma_rr_sw += 1
        else:
            k = self.dma_keys[self.dma_rr % half]
            self.dma_rr += 1
        if self.cnt[k] > 0:
            self._wait(e, k, self.cnt[k])
        reads, writes = self._split(reads, writes, cowrite)
        self._deps(e, reads, writes, cowrite)
        self.cnt[k] += 16
        val = self.cnt[k]
        sem = self.sems[k]
        self.q[e].append(lambda eng, fn=fn, sem=sem: fn(eng).then_inc(sem, 16))
        self._record(k, val, reads, writes, cowrite)
        self.nops += 1
        self._autopump()

    def _autopump(self):
        if self.deferred and not self.pumping:
            self.pumping = True
            self.pump(2)
            self.pumping = False

    def defer(self, thunks):
        self.deferred.extend(thunks)

    def pump(self, n=1):
        while n > 0 and self.deferred:
            self.deferred.pop(0)()
            n -= 1

    def flush(self):
        self.pump(1 << 30)

    def barrier(self):
        for e in self.ENG:
            for k, v in self.cnt.items():
                if v > 0 and k != e:
                    self._wait(e, k, v)

    def finish(self, e="sp"):
        self.flush()
        for k in self.dma_keys:
            if self.cnt[k] > 0:
                self._wait(e, k, self.cnt[k])

    def emit(self):
        with self.nc.Block() as block:
            @block.tensor
            def _(eng):
                for f in self.q["pe"]:
                    f(eng)

            @block.vector
            def _(eng):
                for f in self.q["dve"]:
                    f(eng)

            @block.scalar
            def _(eng):
                for f in self.q["act"]:
                    f(eng)

            @block.gpsimd
            def _(eng):
                for f in self.q["pool"]:
                    f(eng)

            @block.sync
            def _(eng):
                for f in self.q["sp"]:
                    f(eng)


def _t5_bucket(rel):
    nb = 16
    max_exact = 8
    bucket = np.where(rel > 0, nb, 0)
    n = np.abs(rel)
    nf = np.maximum(n, 1).astype(np.float32)
    large = max_exact + (np.log(nf / max_exact) / math.log(128 / max_exact) * (nb - max_exact)).astype(np.int32)
    large = np.minimum(large, nb - 1)
    return bucket + np.where(n < max_exact, n, large)


def _static_tables():
    k = np.arange(128)[:, None]
    q = np.arange(128)[None, :]
    allowed = (k // 64) <= (q // 64)
    bd = _t5_bucket(k - q)
    bo = _t5_bucket(k - q - 128)
    entries = []
    ohs = []
    for b in sorted(set(bd[allowed].tolist())):
        entries.append((0, b))
        ohs.append(((bd == b) & allowed).astype(np.float32))
    for b in sorted(set(bo.flatten().tolist())):
        entries.append((1, b))
        ohs.append((bo == b).astype(np.float32))
    oh = np.stack(ohs, axis=1)
    negm = np.where(allowed, 0.0, -8192.0).astype(np.float32)
    return entries, np.ascontiguousarray(oh.reshape(128, -1)), negm


_OH_ENTRIES, _OH_NP, _NEGM_NP = _static_tables()
NOH = len(_OH_ENTRIES)

PR_G1, PR_B1, PR_SUBG, PR_G2, PR_B2, PR_BR = 0, 1024, 2048, 2304, 3328, 4352
PR_N = 4352 + 36
PS_LQ, PS_LK, PS_RB, PS_N = 0, 256, 512, 640
PC_GIN, PC_BIN, PC_BGA, PC_BGB, PC_CW = 0, 8, 16, 24, 32
PC_N = 56


import os as _os
_DBG = {k: True for k in _os.environ.get("MK_DBG", "").split(",") if k}


def build_program(stage=2, stop=9):
    nc = bass.Bass("TRN2", target_bir_lowering=False)

    def din(name, shape, dt=F32):
        return nc.dram_tensor(name, shape, dt, kind="ExternalInput").ap()

    x_d = din("x", [NTOK, D])
    wqkv_d = din("wqkv", [H, 128, 3 * 8 * 256])
    wr_d = din("wr", [8, 128, 5 * 8 * 128])
    wa_d = din("wa", [8, 128, 8 * 128])
    wb_d = din("wb", [8, 128, 8 * 128])
    wo_d = din("wo", [128, 8 * 1024])
    pcols_d = din("pcols", [128, PC_N])
    prow_d = din("prow", [128, PR_N])
    prows_d = din("prows", [128, PS_N])
    oh_d = din("oh", [128, NOH * 128])
    negm_d = din("negm", [128, 128])
    ident_d = din("ident", [128, 128])
    if stage == 2:
        srow_d = din("srow", [128, 32])
        tball_d = din("tball0", [128, NT * 8])
        prow2_d = din("prow2", [128, 2048])
        ustr_d = din("ustr", [128, 128])
        wrt_d = din("wrt", [128, 8 * 36])
        wg_d = din("wg", [NE, 128, 8 * 512])
        wu_d = din("wu", [NE, 128, 8 * 512])
        wd_d = din("wd", [NE, 128, 4 * 1024])
    out_d = nc.dram_tensor("out", [NTOK, D], F32, kind="ExternalOutput").ap()
    x1f_d = nc.dram_tensor("x1f", [NTOK, D], F32, kind="Internal").ap()
    x1b_d = nc.dram_tensor("x1b", [NTOK + 128, D], BF16, kind="Internal").ap()
    tbl_d = nc.dram_tensor("tbl", [NSLOT, 4], F32, kind="Internal").ap()
    y2_d = nc.dram_tensor("y2", [2 * NTOK, D], BF16, kind="Internal").ap()
    if stage == 2:
        wgb_d = nc.dram_tensor("wgb", [NE, 128, 8 * 512], BF16, kind="Internal").ap()
        wub_d = nc.dram_tensor("wub", [NE, 128, 8 * 512], BF16, kind="Internal").ap()
        wdb_d = nc.dram_tensor("wdb", [NE, 128, 4 * 1024], BF16, kind="Internal").ap()

    with contextlib.ExitStack() as st:
        S = Sched(nc, st)

        def sb(name, shape, dt):
            return st.enter_context(nc.sbuf_tensor(name, shape, dt))

        identf = sb("identf", [128, 128], F32)
        identb = sb("identb", [128, 128], BF16)
        pcols = sb("pcols_s", [128, PC_N], F32)
        prow = sb("prow_s", [128, PR_N], F32)
        bcat = sb("bcat", [128, H, 2, 256], BF16)
        lamc = sb("lamc", [128, 4], F32)
        subg = sb("subg", [128, 256], F32)
        epsc = sb("epsc", [128, 2], F32)
        rstd_in = sb("rstd_in", [128, NT], F32)
        nmr_in = sb("nmr_in", [128, NT], F32)
        wo_s = sb("wo_s", [128, 8, 1024], BF16)
        uT = sb("uT", [128, 8, SEQ], BF16)
        oT = sb("oT", [128, 8, SEQ], BF16)
        halo = sb("halo", [128, 8, 2], F32)
        xs = [sb("xs%d" % i, [128, 1024], F32) for i in range(2)]
        xh = [sb("xh0", [128, 1024], BF16)] * 2
        stt = sb("stt", [128, 12], F32)
        sml = sb("sml", [128, 16], F32)
        wk = [sb("wk%d" % i, [128, 1024], F32) for i in range(3)]
        r_identf, r_identb, r_pcols, r_prow, r_bcat, r_lamc, r_subg, r_eps = [Res() for _ in range(8)]
        r_mv, r_g1, r_wo, r_halo, r_stt, r_sml = [Res() for _ in range(6)]
        r_mvs = [Res() for _ in range(NT)]
        r_xhB = Res()
        r_uT = [Res() for _ in range(32)]
        r_oT = [Res() for _ in range(16)]
        r_xs = [Res(), Res()]
        r_xh = [Res()] * 2
        r_wk = [Res() for _ in range(3)]
        ARENA = 75792
        arena = sb("arena", [128, ARENA], mybir.dt.uint8)

        NPG = 3
        pg = [st.enter_context(nc.psum_tensor("pg%d" % i, [128, 512], F32)) for i in range(NPG)]
        pv = [st.enter_context(nc.psum_tensor("pv%d" % i, [128, 512], F32)) for i in range(3)]
        pt = [st.enter_context(nc.psum_tensor("pt%d" % i, [128, 1024], BF16)) for i in range(2)]
        r_pg = [Res(True) for _ in range(NPG)]
        r_pv = [Res(True) for _ in range(3)]
        r_pt = [Res(True), Res(True)]
        cnt = {"pg": 0, "pv": 0, "pt": 0, "ev": 0}

        def next_pg():
            if cnt.get("wide", False) == "moe" or (cnt.get("wide", False) and _DBG.get("wide" + str(cnt["wide"]))):
                i = cnt["pg"] % (NPG + 3)
                cnt["pg"] += 1
                return (pg + pv)[i], (r_pg + r_pv)[i]
            if cnt.get("wide", None) is False:
                i = cnt["pg"] % (NPG + 1)
                cnt["pg"] += 1
                return (pg + pv[2:3])[i], (r_pg + r_pv[2:3])[i]
            i = cnt["pg"] % NPG
            cnt["pg"] += 1
            return pg[i], r_pg[i]

        def next_pv():
            i = cnt["pv"] % 2
            cnt["pv"] += 1
            return pv[i], r_pv[i]

        def next_pt():
            i = cnt["pt"] % 2
            cnt["pt"] += 1
            return pt[i][:, 0:512], r_pt[i]

        def mm(out_ap, pairs, reads, writes, first=True, last=True):
            n = len(pairs)
            fns = []
            for i, (a, b) in enumerate(pairs):
                fns.append(lambda e, a=a, b=b, i=i: e.matmul(out_ap, lhsT=a, rhs=b, start=(first and i == 0), stop=(last and i == n - 1)))
            S.ops("pe", fns, reads=reads, writes=writes)

        def evac(out_ap, in_ap, reads, writes, eng=None):
            if eng is None:
                eng = "act" if cnt["ev"] % 2 == 0 else "dve"
                cnt["ev"] += 1
            if eng == "act":
                S.op("act", lambda e: e.activation(out=out_ap, in_=in_ap, func=AF.Copy), reads=reads, writes=writes)
            else:
                S.op("dve", lambda e: e.tensor_copy(out=out_ap, in_=in_ap), reads=reads, writes=writes)

        S.dma("sp", lambda e: e.dma_start(out=identf[:], in_=ident_d), writes=[r_identf])
        S.dma("sp", lambda e: e.dma_start(out=pcols[:], in_=pcols_d), writes=[r_pcols])
        S.dma("sp", lambda e: e.dma_start(out=prow[:], in_=prow_d), writes=[r_prow])
        S.op("dve", lambda e: e.tensor_copy(out=identb[:], in_=identf[:]), reads=[r_identf], writes=[r_identb])
        S.op("dve", lambda e: e.memset(epsc[:, 0:1], LN_EPS), writes=[r_eps])
        S.op("dve", lambda e: e.memset(epsc[:, 1:2], RMS_EPS), writes=[r_eps])
        S.op("dve", lambda e: e.memset(halo[:], 0.0), writes=[r_halo])
        PS_OFF = NOH * 128 * 4 + 512
        prows = arena[:, PS_OFF:PS_OFF + PS_N * 4].bitcast(F32)
        r_prows = Res()
        S.dma("sp", lambda e: e.dma_start(out=prows, in_=prows_d), writes=[r_prows])
        S.op("dve", lambda e: e.tensor_tensor(out=wk[0][:, 0:256], in0=prows[:, PS_LQ:PS_LQ + 256], in1=prows[:, PS_LK:PS_LK + 256], op=ALU.mult),
             reads=[r_prows], writes=[r_wk[0]])
        S.op("dve", lambda e: e.tensor_reduce(out=sml[:, 0:2], in_=wk[0][:, 0:256].rearrange("p (a b) -> p a b", a=2), axis=AX.X, op=ALU.add),
             reads=[r_wk[0]], writes=[r_sml])
        S.op("act", lambda e: e.activation(out=sml[:, 2:4], in_=sml[:, 0:2], func=AF.Exp), reads=[r_sml], writes=[r_sml])
        S.op("dve", lambda e: e.tensor_tensor(out=sml[:, 4:5], in0=sml[:, 2:3], in1=sml[:, 3:4], op=ALU.subtract), reads=[r_sml], writes=[r_sml])
        S.op("dve", lambda e: e.tensor_scalar(out=lamc[:, 0:1], in0=sml[:, 4:5], scalar1=LAM_INIT, scalar2=None, op0=ALU.add), reads=[r_sml], writes=[r_lamc])
        S.op("dve", lambda e: e.tensor_scalar(out=lamc[:, 1:2], in0=lamc[:, 0:1], scalar1=-1.0, scalar2=None, op0=ALU.mult), reads=[r_lamc], writes=[r_lamc])
        S.op("dve", lambda e: e.tensor_scalar(out=subg[:], in0=prow[:, PR_SUBG:PR_SUBG + 256], scalar1=1.0 - LAM_INIT, scalar2=None, op0=ALU.mult),
             reads=[r_prow], writes=[r_subg])
        oh_s = arena[:, 0:NOH * 128 * 4].bitcast(F32).rearrange("p (n q) -> p n q", q=128)
        negm_s = arena[:, NOH * 128 * 4:NOH * 128 * 4 + 512].bitcast(F32)
        r_oh, r_negm = Res(), Res()
        S.dma("sp", lambda e: e.dma_start(out=oh_s, in_=oh_d.rearrange("p (n q) -> p n q", q=128)), writes=[r_oh])
        S.dma("sp", lambda e: e.dma_start(out=negm_s, in_=negm_d), writes=[r_negm])
        rbd = arena[:, 40960:40960 + 32 * H * 4].bitcast(F32).rearrange("p (b h) -> p b h", h=H)
        r_rbd = Res()
        rbv = prows[:, PS_RB:PS_RB + 128].rearrange("p (b h) -> p b h", h=H)
        for h in range(H):
            S.op("dve", lambda e, h=h: e.tensor_scalar(out=rbd[:, :, h], in0=rbv[:, :, h], scalar1=rbv[:, 15, h:h + 1], scalar2=1.0 / SCALE,
                                                        op0=ALU.subtract, op1=ALU.mult), reads=[r_prows], writes=[r_rbd])
        acc = wk[1]
        accp = arena[:, 32768:32768 + 640 * 4].bitcast(F32)
        r_accp = Res()
        for h in range(H):
            on_pool = h >= 2
            A, r_A, eng = (accp, r_accp, "pool") if on_pool else (acc, r_wk[1], "dve")
            for typ in range(2):
                ents = [(i, b) for i, (t, b) in enumerate(_OH_ENTRIES) if t == typ]
                av = A[:, typ * 128:(typ + 1) * 128]
                tv = A[:, 512:640]
                if typ == 0:
                    S.op(eng, lambda e, av=av: e.tensor_copy(out=av, in_=negm_s), reads=[r_negm], writes=[r_A])
                else:
                    S.op(eng, lambda e, av=av: e.memset(av, 0.0), writes=[r_A])
                for (i, b) in ents:
                    if on_pool:
                        S.op("pool", lambda e, tv=tv, i=i, b=b, h=h: e.tensor_scalar(out=tv, in0=oh_s[:, i, :], scalar1=rbd[:, b, h:h + 1], scalar2=0.0, op0=ALU.mult, op1=ALU.add),
                             reads=[r_oh, r_rbd, r_A], writes=[r_A])
                        S.op("pool", lambda e, av=av, tv=tv: e.tensor_tensor(out=av, in0=av, in1=tv, op=ALU.add), reads=[r_A], writes=[r_A])
                    else:
                        S.op("dve", lambda e, av=av, i=i, b=b, h=h: e.scalar_tensor_tensor(out=av, in0=oh_s[:, i, :], scalar=rbd[:, b, h:h + 1], in1=av,
                                                                                            op0=ALU.mult, op1=ALU.add),
                             reads=[r_oh, r_rbd, r_A], writes=[r_A])
            S.op(eng, lambda e, h=h, A=A: e.tensor_copy(out=bcat[:, h, 0, :], in_=A[:, 0:256]), reads=[r_A], writes=[r_bcat], cowrite=True)
            S.op(eng, lambda e, h=h, A=A: e.tensor_tensor(out=A[:, 256:512], in0=A[:, 0:256], in1=bcat[:, h, 0, :], op=ALU.subtract),
                 reads=[r_A, r_bcat], writes=[r_A])
            S.op(eng, lambda e, h=h, A=A: e.tensor_copy(out=bcat[:, h, 1, :], in_=A[:, 256:512]), reads=[r_A], writes=[r_bcat], cowrite=True)
        if stage == 2:
            rt = sb("rt", [128, 256], F32)
            Mall = sb("Mall", [128, NT, 32], BF16)
            ustr_b = sb("ustr_b", [128, 128], BF16)
            ones_b = sb("ones_b", [128, 128], BF16)
            Lbuf = sb("Lbuf", [128, 8, 36], F32)
            r_L = [Res() for _ in range(8)]
            tball = sb("tball", [128, NT, 2, 4], F32)
            destall = sb("destall", [128, NT, 2], I32)
            srow = sb("srow_s", [128, 32], F32)
            wrt_s = sb("wrt_s", [128, 8, 36], F32)
            r_rt, r_Mall, r_ustr, r_tbk, r_desti, r_srow, r_wrt, r_tbl, r_x1b, r_y2, r_x1f = [Res() for _ in range(11)]
            S.dma("sp", lambda e: e.dma_start(out=srow[:], in_=srow_d), writes=[r_srow])
            S.dma("sp", lambda e: e.dma_start(out=wrt_s[:], in_=wrt_d.rearrange("p (k n) -> p k n", k=8)), writes=[r_wrt])
            S.dma("pool", lambda e: e.dma_start(out=ustr_b[:], in_=ustr_d), writes=[r_ustr])
            S.op("pool", lambda e: e.memset(ones_b[:], 1.0), writes=[r_ustr])
            S.dma("sp", lambda e: e.dma_start(out=tball[:], in_=tball_d.rearrange("p (t k f) -> p t k f", k=2, f=4)), writes=[r_tbk])
            def init_scratch():
                S.op("dve", lambda e: e.memset(wk[2][:], 0.0), writes=[r_wk[2]])
                zv = wk[2][:, :].bitcast(BF16)
                y2v = y2_d.rearrange("(p c) d -> p (c d)", p=128)
                for i in range(32):
                    S.dma("sp", lambda e, i=i: e.dma_start(out=y2v[:, i * 2048:(i + 1) * 2048], in_=zv), reads=[r_wk[2]], writes=[r_y2], cowrite=True)
                S.dma("sp", lambda e: e.dma_start(out=x1b_d[NTOK:NTOK + 128, :], in_=zv[:, 0:1024]), reads=[r_wk[2]], writes=[r_x1b], cowrite=True)
                tinit = wk[0][:, 0:(NSLOT // 128) * 4].rearrange("p (c f) -> p c f", f=4)
                S.op("dve", lambda e: e.memset(tinit[:, :, 0:1], float(NTOK)), writes=[r_wk[0]])
                S.op("dve", lambda e: e.memset(tinit[:, :, 1:2], 0.0), writes=[r_wk[0]])
                S.op("dve", lambda e: e.memset(tinit[:, :, 2:3], BIGIDX), writes=[r_wk[0]])
                S.op("dve", lambda e: e.memset(tinit[:, :, 3:4], 0.0), writes=[r_wk[0]])
                S.dma("sp", lambda e: e.dma_start(out=tbl_d.rearrange("(p c) f -> p c f", p=128), in_=tinit), reads=[r_wk[0]], writes=[r_tbl])
        S.dma("pool", lambda e: e.dma_start(out=wo_s[:], in_=wo_d.rearrange("p (k c) -> p k c", k=8), max_dma_last_dim=4096), writes=[r_wo])

        def carve(off, shape, dt):
            esz = 2 if dt == BF16 else 4
            n = int(np.prod(shape))
            v = arena[:, off:off + n * esz].bitcast(dt)
            if len(shape) == 2:
                v = v.rearrange("p (a b) -> p a b", a=shape[0])
            elif len(shape) == 3:
                v = v.rearrange("p (a b c) -> p a b c", a=shape[0], b=shape[1])
            return v, off + n * esz

        gin_row = prow[:, PR_G2:PR_G2 + 1024]
        bin_row = prow[:, PR_B2:PR_B2 + 1024]

        smcs = sb("smcs", [128, 16], F32)
        sml2 = sb("sml2", [128, 16], F32)
        stt2 = sb("stt2", [128, 12], F32)
        r_sml2, r_stt2 = Res(), Res()
        LNS = [(sml, r_sml, stt, r_stt), (sml2, r_sml2, stt2, r_stt2)]

        def ln_stats(src, r_src, si=0):
            sm, r_sm, sx, r_sx = LNS[si]
            S.ops("dve", [lambda e: e.bn_stats(out=sx[:, 0:6], in_=src[:, 0:512]),
                          lambda e: e.bn_stats(out=sx[:, 6:12], in_=src[:, 512:1024])], reads=(r_src if isinstance(r_src, list) else [r_src]), writes=[r_sx])
            S.op("dve", lambda e: e.bn_aggr(out=sm[:, 6:8], in_=sx[:, 0:12]), reads=[r_sx], writes=[r_sm])
            S.op("act", lambda e: e.activation(out=sm[:, 8:9], in_=sm[:, 7:8], func=AF.Ln, bias=epsc[:, 0:1], scale=1.0),
                 reads=[r_sm, r_eps], writes=[r_sm])
            S.op("act", lambda e: e.activation(out=sm[:, 8:9], in_=sm[:, 8:9], func=AF.Exp, scale=-0.5), reads=[r_sm], writes=[r_sm])
            S.op("dve", lambda e: e.tensor_scalar(out=sm[:, 9:10], in0=sm[:, 6:7], scalar1=sm[:, 8:9], scalar2=-1.0, op0=ALU.mult, op1=ALU.mult),
                 reads=[r_sm], writes=[r_sm])
            return sm, r_sm

        r_cw = [Res() for _ in range(NE)]
        conv_next = [0]

        def emit_conv(n=1):
            if stage != 2:
                return
            for _ in range(n):
                ex = conv_next[0]
                if ex >= NE:
                    return
                conv_next[0] += 1
                for (dst, src) in ((wgb_d, wg_d), (wub_d, wu_d), (wdb_d, wd_d)):
                    S.dma("pool", lambda e, dst=dst, src=src, ex=ex: e.dma_start(out=dst[ex], in_=src[ex], max_dma_last_dim=8192), writes=[r_cw[ex]], cowrite=True)

        pregs = {}

        def preg(e, val):
            if val not in pregs:
                pregs[val] = e.to_reg(val)
            return pregs[val]

        def carve_from(buf, off, shape, dt):
            esz = 2 if dt == BF16 else 4
            n = int(np.prod(shape))
            v = buf[:, off:off + n * esz].bitcast(dt)
            if len(shape) == 2:
                v = v.rearrange("p (a b) -> p a b", a=shape[0])
            return v, off + n * esz

        def moe_phase():
            emit_conv(NE)
            S.flush()
            S.barrier()
            cnt["wide"] = "moe"
            S.dma("sp", lambda e: e.dma_start(out=prow[:, PR_G2:PR_G2 + 2048], in_=prow2_d), writes=[r_prow])
            ubytes = uT[:, :, :].rearrange("p a b -> p (a b)").bitcast(mybir.dt.uint8)
            obytes = oT[:, :, :].rearrange("p a b -> p (a b)").bitcast(mybir.dt.uint8)
            off = 0
            wgb, wub, wdb = [], [], []
            for i in range(2):
                a, off = carve(off, [8, 512], BF16); wgb.append(a)
                a, off = carve(off, [8, 512], BF16); wub.append(a)
                a, off = carve(off, [4, 1024], BF16); wdb.append(a)
            XgT, hT, sg, yo = [], [], [], []
            for i in range(2):
                a, off = carve(off, [8, CAP], BF16); XgT.append(a)
                a, off = carve(off, [4, CAP], BF16); hT.append(a)
                a, off = carve(off, [CAP], F32); sg.append(a)
            assert off <= ARENA, off
            uo = 0
            xg, tbe, idxt, idxa = [], [], [], []
            for i in range(2):
                a, uo = carve_from(ubytes, uo, [NCT, 1024], BF16); xg.append(a)
            NIB = 3
            for i in range(NIB):
                a, uo = carve_from(ubytes, uo, [NCT, 4], F32); tbe.append(a)
                a, uo = carve_from(ubytes, uo, [NCT], I32); idxt.append(a)
                a, uo = carve_from(ubytes, uo, [NCT], I32); idxa.append(a)
            NYO = 4
            for i in range(NYO):
                a, uo = carve_from(ubytes, uo, [1024], BF16); yo.append(a)
            assert uo <= 32768
            r_wg, r_wu, r_wd = [Res(), Res()], [Res(), Res()], [Res(), Res()]
            r_xg = [[Res() for _ in range(NCT)] for _ in range(2)]
            r_XgT, r_hT, r_sg = [[Res(), Res()] for _ in range(3)]
            r_yo = [Res() for _ in range(NYO)]
            r_tbe, r_idt, r_ida = [[Res() for _ in range(NIB)] for _ in range(3)]
            nyo = [0]

            def load_expert(ex):
                bi = ex % 2
                ib = ex % NIB
                S.dma("sp", lambda e: e.dma_start(out=tbe[ib], in_=tbl_d[ex * CAP:(ex + 1) * CAP, :].rearrange("(c p) f -> p c f", p=128)), reads=[r_tbl], writes=[r_tbe[ib]])
                S.op("dve", lambda e: e.tensor_copy(out=idxt[ib], in_=tbe[ib][:, :, 0]), reads=[r_tbe[ib]], writes=[r_idt[ib]])
                S.op("dve", lambda e: e.tensor_copy(out=idxa[ib], in_=tbe[ib][:, :, 2]), reads=[r_tbe[ib]], writes=[r_ida[ib]])
                for c in range(NCT):
                    S.dma("pool", lambda e, c=c: e.indirect_dma_start(out=xg[bi][:, c, :], out_offset=None, in_=x1b_d[:, :],
                                                                       in_offset=bass.IndirectOffsetOnAxis(ap=idxt[ib][:, c:c + 1], axis=0)),
                          reads=[r_idt[ib], r_x1b], writes=[r_xg[bi][c]])
                S.dma("sp", lambda e: e.dma_start(out=wgb[bi], in_=wgb_d[ex].rearrange("p (k f) -> p k f", k=8)), reads=[r_cw[ex]], writes=[r_wg[bi]])
                S.dma("sp", lambda e: e.dma_start(out=wub[bi], in_=wub_d[ex].rearrange("p (k f) -> p k f", k=8)), reads=[r_cw[ex]], writes=[r_wu[bi]])
                S.dma("sp", lambda e: e.dma_start(out=wdb[bi], in_=wdb_d[ex].rearrange("p (k f) -> p k f", k=4)), reads=[r_cw[ex]], writes=[r_wd[bi]])

            def compute_expert(ex):
                bi = ex % 2
                ib = ex % NIB
                for c in range(NCT):
                    for half in range(2):
                        pt_ap, r_p = next_pt()
                        S.ops("pe", [(lambda e, k=k, c=c, pt_ap=pt_ap: e.transpose(out=pt_ap[:, (k % 4) * 128:(k % 4 + 1) * 128], in_=xg[bi][:, c, k * 128:(k + 1) * 128], identity=identb[:]))
                                     for k in range(half * 4, half * 4 + 4)], reads=[r_xg[bi][c], r_identb], writes=[r_p])
                        evac(XgT[bi][:, half * 4:half * 4 + 4, c * 128:(c + 1) * 128], pt_ap[:, 0:512].rearrange("p (a b) -> p a b", a=4), reads=[r_p], writes=[r_XgT[bi]], eng="dve")
                for fc in range(4):
                    psG, r_psG = next_pg()
                    mm(psG[:, 0:CAP], [(wgb[bi][:, k, fc * 128:(fc + 1) * 128], XgT[bi][:, k, :]) for k in range(8)], reads=[r_wg[bi], r_XgT[bi]], writes=[r_psG])
                    si = fc % 2
                    S.op("act", lambda e, psG=psG, si=si: e.activation(out=sg[si], in_=psG[:, 0:CAP], func=AF.Silu), reads=[r_psG], writes=[r_sg[si]])
                    psU, r_psU = next_pg()
                    mm(psU[:, 0:CAP], [(wub[bi][:, k, fc * 128:(fc + 1) * 128], XgT[bi][:, k, :]) for k in range(8)], reads=[r_wu[bi], r_XgT[bi]], writes=[r_psU])
                    S.op("dve", lambda e, psU=psU, fc=fc, si=si: e.tensor_tensor(out=hT[bi][:, fc, :], in0=psU[:, 0:CAP], in1=sg[si], op=ALU.mult),
                         reads=[r_psU, r_sg[si]], writes=[r_hT[bi]])
                for c in range(NCT):
                    yi = nyo[0] % NYO
                    nyo[0] += 1
                    for hh in range(2):
                        ps, r_ps = next_pg()
                        mm(ps[:, :], [(hT[bi][:, fc, c * 128:(c + 1) * 128], wdb[bi][:, fc, hh * 512:(hh + 1) * 512]) for fc in range(4)], reads=[r_hT[bi], r_wd[bi]], writes=[r_ps])
                        S.op("act", lambda e, ps=ps, hh=hh, c=c, yi=yi: e.activation(out=yo[yi][:, hh * 512:(hh + 1) * 512], in_=ps[:, :], func=AF.Identity, scale=tbe[ib][:, c, 1:2]),
                             reads=[r_ps, r_tbe[ib]], writes=[r_yo[yi]])
                    S.dma("pool", lambda e, c=c, yi=yi: e.indirect_dma_start(out=y2_d[:, :], out_offset=bass.IndirectOffsetOnAxis(ap=idxa[ib][:, c:c + 1], axis=0),
                                                                             in_=yo[yi][:, :], in_offset=None, bounds_check=preg(e, 2 * NTOK - 1), oob_is_err=False),
                          reads=[r_yo[yi], r_ida[ib]], writes=[r_y2], cowrite=True)

            if _DBG.get("nopf"):
                for ex in range(NE):
                    load_expert(ex)
                    compute_expert(ex)
            else:
                load_expert(0)
                for ex in range(NE):
                    if ex + 1 < NE:
                        load_expert(ex + 1)
                    compute_expert(ex)

            S.barrier()
            NFS = 6
            FX, FY, FT, FN = [], [], [], []
            ao, oo = 0, 0
            for i in range(NFS):
                if i < 4:
                    a, ao = carve(ao, [1024], F32); FX.append(a)
                    a, ao = carve(ao, [2, 1024], BF16); FY.append(a)
                    a, ao = carve(ao, [1024], F32); FT.append(a)
                    a, ao = carve(ao, [1024], F32); FN.append(a)
                else:
                    a, oo = carve_from(obytes, oo, [1024], F32); FX.append(a)
                    a, oo = carve_from(obytes, oo, [2, 1024], BF16); FY.append(a)
                    a, oo = carve_from(obytes, oo, [1024], F32); FT.append(a)
                    a, oo = carve_from(obytes, oo, [1024], F32); FN.append(a)
            assert ao <= ARENA and oo <= 32768
            r_FX, r_FY, r_FT, r_FN = [[Res() for _ in range(NFS)] for _ in range(4)]
            def fin_load(gT):
                row0 = gT * 128
                b = gT % NFS
                S.dma("sp", lambda e: e.dma_start(out=FX[b], in_=x1f_d[row0:row0 + 128, :]), reads=[r_x1f], writes=[r_FX[b]])
                S.dma("sp", lambda e: e.dma_start(out=FY[b], in_=y2_d[2 * row0:2 * row0 + 256, :].rearrange("(p two) d -> p two d", two=2)), reads=[r_y2], writes=[r_FY[b]])

            for gT in range(min(NFS - 1, NT)):
                fin_load(gT)

            def fin_A(gT):
                b = gT % NFS
                S.op("dve", lambda e: e.scalar_tensor_tensor(out=FT[b], in0=FX[b], scalar=ALPHA, in1=FY[b][:, 0, :], op0=ALU.mult, op1=ALU.add),
                     reads=[r_FX[b], r_FY[b]], writes=[r_FT[b]])
                S.op("dve", lambda e: e.tensor_tensor(out=FT[b], in0=FT[b], in1=FY[b][:, 1, :], op=ALU.add), reads=[r_FT[b], r_FY[b]], writes=[r_FT[b]])
                return ln_stats(FT[b], r_FT[b], gT % 2)

            def fin_B(gT, sm, r_sm):
                row0 = gT * 128
                b = gT % NFS
                S.op("act", lambda e: e.activation(out=FN[b], in_=FT[b], func=AF.Identity, bias=sm[:, 9:10], scale=sm[:, 8:9]),
                     reads=[r_FT[b], r_sm], writes=[r_FN[b]])
                S.op("pool", lambda e: e.tensor_tensor(out=FN[b], in0=FN[b], in1=prow[:, PR_G2:PR_G2 + 1024], op=ALU.mult), reads=[r_FN[b], r_prow], writes=[r_FN[b]])
                S.op("pool", lambda e: e.tensor_tensor(out=FN[b], in0=FN[b], in1=prow[:, PR_B2:PR_B2 + 1024], op=ALU.add), reads=[r_FN[b], r_prow], writes=[r_FN[b]])
                S.dma("sp", lambda e: e.dma_start(out=out_d[row0:row0 + 128, :], in_=FN[b]), reads=[r_FN[b]])
                if gT + NFS - 1 < NT:
                    fin_load(gT + NFS - 1)

            pend = fin_A(0)
            for gT in range(NT):
                nxt = fin_A(gT + 1) if gT + 1 < NT else None
                fin_B(gT, *pend)
                pend = nxt

        def route_tile(gT, row0, W):
            (Wu, Ru), (Wt, Rt), (Wxb, Rxb) = W["u"], W["t"], W["xb"]
            slot = gT % 8
            S.op("act", lambda e: e.activation(out=Wxb, in_=Wu, func=AF.Copy), reads=Ru, writes=Rxb)
            S.dma("sp", lambda e: e.dma_start(out=x1f_d[row0:row0 + 128, :], in_=Wu), reads=Ru, writes=[r_x1f], cowrite=True)
            S.dma("sp", lambda e: e.dma_start(out=x1b_d[row0:row0 + 128, :], in_=Wxb), reads=Rxb, writes=[r_x1b], cowrite=True)
            x1T = Wt.rearrange("p (k t) -> p k t", k=8)
            for half in range(2):
                ps, r_ps = next_pg()
                S.ops("pe", [(lambda e, c=c, ps=ps: e.transpose(out=ps[:, (c % 4) * 128:(c % 4 + 1) * 128], in_=Wu[:, c * 128:(c + 1) * 128], identity=identf[:]))
                             for c in range(half * 4, half * 4 + 4)], reads=Ru + [r_identf], writes=[r_ps])
                evac(x1T[:, half * 4:half * 4 + 4, :], ps[:, :].rearrange("p (a b) -> p a b", a=4), reads=[r_ps], writes=Rt)
            ps, r_ps = next_pg()
            mm(ps[:, 0:36], [(x1T[:, k, :], wrt_s[:, k, :]) for k in range(8)], reads=Rt + [r_wrt], writes=[r_ps])
            R = lambda a, b: rt[:, a:b]
            S.op("dve", lambda e, ps=ps: e.tensor_tensor(out=Lbuf[:, slot, :], in0=ps[:, 0:36], in1=prow[:, PR_BR:PR_BR + 36], op=ALU.add), reads=[r_ps, r_prow], writes=[r_L[slot]])
            th = []
            ctx = {}
            th.append(lambda: S.op("dve", lambda e: e.tensor_copy(out=R(0, 36), in_=Lbuf[:, slot, :]), reads=[r_L[slot], r_rt], writes=[r_rt]))

            def dv(fn, rd=(), wr=None, cw=False):
                th.append(lambda: S.op("dve", fn, reads=[r_rt] + list(rd), writes=[r_rt] if wr is None else wr, cowrite=cw))

            def ac(fn):
                th.append(lambda: S.op("act", fn, reads=[r_rt], writes=[r_rt]))

            dv(lambda e: e.tensor_reduce(out=R(37, 38), in_=R(0, 4), axis=AX.X, op=ALU.max, negate=True))
            ac(lambda e: e.activation(out=R(40, 44), in_=R(0, 4), func=AF.Exp, bias=R(37, 38), scale=1.0, accum_out=R(38, 39)))
            dv(lambda e: e.reciprocal(out=R(39, 40), in_=R(38, 39)))
            dv(lambda e: e.tensor_scalar(out=R(44, 48), in0=R(0, 4), scalar1=R(37, 38), scalar2=1.0e30, op0=ALU.add, op1=ALU.mult))
            for g in range(4):
                dv(lambda e, g=g: e.tensor_scalar(out=R(48 + g * 8, 56 + g * 8), in0=R(4 + g * 8, 12 + g * 8), scalar1=R(44 + g, 45 + g), scalar2=None, op0=ALU.add))
            dv(lambda e: e.max(out=R(80, 88), in_=R(48, 80)))
            dv(lambda e: e.tensor_tensor(out=R(88, 89), in0=R(80, 81), in1=R(81, 82), op=ALU.subtract))
            ac(lambda e: e.activation(out=R(89, 90), in_=R(88, 89), func=AF.Exp, scale=-1.0))
            dv(lambda e: e.tensor_scalar(out=R(89, 90), in0=R(89, 90), scalar1=1.0, scalar2=None, op0=ALU.add))
            dv(lambda e: e.reciprocal(out=R(89, 90), in_=R(89, 90)))
            dv(lambda e: e.tensor_tensor(out=tball[:, gT, 0, 1:2], in0=R(89, 90), in1=R(39, 40), op=ALU.mult), wr=[r_tbk], cw=True)
            dv(lambda e: e.tensor_tensor(out=tball[:, gT, 1, 1:2], in0=R(39, 40), in1=tball[:, gT, 0, 1:2], op=ALU.subtract), rd=[r_tbk], wr=[r_tbk], cw=True)
            for k in range(2):
                dv(lambda e, k=k: e.tensor_scalar(out=R(96 + 32 * k, 128 + 32 * k), in0=R(48, 80), scalar1=R(80 + k, 81 + k), scalar2=None, op0=ALU.is_equal))
            dv(lambda e: e.tensor_tensor(out=Mall[:, gT, :], in0=R(96, 128), in1=R(128, 160), op=ALU.add), wr=[r_Mall])

            def pm():
                ps2, r_ps2 = next_pg()
                ctx["ps"], ctx["r"] = ps2, r_ps2
                mm(ps2[:, 0:32], [(ustr_b[:], Mall[:, gT, :])] + [(ones_b[:], Mall[:, g2, :]) for g2 in range(gT)], reads=[r_Mall, r_ustr], writes=[r_ps2])
            th.append(pm)
            th.append(lambda: S.op("dve", lambda e: e.tensor_scalar(out=R(160, 192), in0=ctx["ps"][:, 0:32], scalar1=float(CAP), scalar2=None, op0=ALU.is_lt),
                                   reads=[ctx["r"], r_rt], writes=[r_rt]))
            th.append(lambda: S.op("dve", lambda e: e.tensor_tensor(out=R(192, 224), in0=ctx["ps"][:, 0:32], in1=srow[:, 0:32], op=ALU.add),
                                   reads=[ctx["r"], r_rt, r_srow], writes=[r_rt]))
            dv(lambda e: e.tensor_tensor(out=R(192, 224), in0=R(192, 224), in1=R(160, 192), op=ALU.mult))
            for k in range(2):
                dv(lambda e, k=k: e.tensor_tensor(out=R(224, 256), in0=R(96 + 32 * k, 128 + 32 * k), in1=R(192, 224), op=ALU.mult))
                dv(lambda e, k=k: e.tensor_reduce(out=R(92 + k, 93 + k), in_=R(224, 256), axis=AX.X, op=ALU.add))
                dv(lambda e, k=k: e.tensor_scalar(out=destall[:, gT, k:k + 1], in0=R(92 + k, 93 + k), scalar1=BIGIDX, scalar2=None, op0=ALU.add), wr=[r_desti], cw=True)
            for k in range(2):
                th.append(lambda k=k: S.dma("pool", lambda e: e.indirect_dma_start(out=tbl_d[:, :], out_offset=bass.IndirectOffsetOnAxis(ap=destall[:, gT, k:k + 1], axis=0),
                                                                                  in_=tball[:, gT, k, :], in_offset=None, bounds_check=preg(e, NSLOT - 1), oob_is_err=False),
                                            reads=[r_tbk, r_desti], writes=[r_tbl], cowrite=True))
            S.defer(th)

        for s in range(NSEQ if stop > 0 else 0):
            base = s * SEQ
            xh2 = [xh[0][:, :], wk[2][:, 0:512].bitcast(BF16)]
            r_xh2 = [r_xh[0], r_wk[2]]

            def p1_A(T):
                gT = s * 16 + T
                b = gT % 2
                r0 = base + T * 128
                S.dma("sp", lambda e: e.dma_start(out=xs[b][:], in_=x_d[r0:r0 + 128, :]), writes=[r_xs[b]])
                sm, r_sm = ln_stats(xs[b], r_xs[b], T % 2)
                S.op("dve", lambda e: e.tensor_copy(out=rstd_in[:, gT:gT + 1], in_=sm[:, 8:9]), reads=[r_sm], writes=[r_mvs[gT]])
                S.op("dve", lambda e: e.tensor_copy(out=nmr_in[:, gT:gT + 1], in_=sm[:, 9:10]), reads=[r_sm], writes=[r_mvs[gT]])
                S.op("act", lambda e: e.activation(out=xh2[b], in_=xs[b][:], func=AF.Identity, bias=nmr_in[:, gT:gT + 1], scale=rstd_in[:, gT:gT + 1]),
                     reads=[r_xs[b], r_mvs[gT]], writes=[r_xh2[b]])

            def p1_B(T):
                gT = s * 16 + T
                b = gT % 2
                for half in range(2):
                    pt_ap, r_p = next_pt()
                    S.ops("pe", [(lambda e, c=c, pt_ap=pt_ap: e.transpose(out=pt_ap[:, (c % 4) * 128:(c % 4 + 1) * 128], in_=xh2[b][:, c * 128:(c + 1) * 128], identity=identb[:]))
                                 for c in range(half * 4, half * 4 + 4)], reads=[r_xh2[b], r_identb], writes=[r_p])
                    for c in range(half * 4, half * 4 + 4):
                        src = pt_ap[:, (c % 4) * 128:(c % 4 + 1) * 128]
                        dst = uT[:, c, T * 128:(T + 1) * 128]
                        if half == 0:
                            S.op("act", lambda e, c=c, src=src, dst=dst: e.activation(out=dst, in_=src, func=AF.Identity, bias=pcols[:, PC_BIN + c:PC_BIN + c + 1],
                                                                                       scale=pcols[:, PC_GIN + c:PC_GIN + c + 1]),
                                 reads=[r_p, r_pcols], writes=[r_uT[2 * T + half]])
                        else:
                            S.op("dve", lambda e, c=c, src=src, dst=dst: e.tensor_scalar(out=dst, in0=src, scalar1=pcols[:, PC_GIN + c:PC_GIN + c + 1],
                                                                                          scalar2=pcols[:, PC_BIN + c:PC_BIN + c + 1], op0=ALU.mult, op1=ALU.add),
                                 reads=[r_p, r_pcols], writes=[r_uT[2 * T + half]])

            p1_A(0)
            for T in range(16):
                if T + 1 < 16:
                    p1_A(T + 1)
                p1_B(T)

            if stop < 2:
                continue
            S.barrier()
            cnt["wide"] = False
            if stage == 2 and s == 0:
                init_scratch()
            off = 0
            wqkv, off = carve(off, [3, 8, 256], BF16)
            qT, off = carve(off, [2, SEQ], BF16)
            kT, off = carve(off, [2, SEQ], BF16)
            Vh, off = carve(off, [16, 260], BF16)
            PT0, off = carve(off, [16, 512], BF16)
            PT1, off = carve(off, [16, 512], BF16)
            PT = [PT0, PT1]
            o1s, o2s, onbs = [], [], []
            for _i in range(2):
                a, off = carve(off, [258], F32); o1s.append(a)
                a, off = carve(off, [258], F32); o2s.append(a)
            for _i in range(3):
                a, off = carve(off, [256], BF16); onbs.append(a)
            assert off <= ARENA, off
            r_o1s, r_o2s, r_smcs = [[Res(), Res()] for _ in range(3)]
            r_onbs = [Res(), Res(), Res()]
            cnt["cmb"] = 0
            pend_tr = []
            r_wseg, r_q, r_k = [Res(), Res(), Res()], Res(), Res()
            r_V = [Res() for _ in range(16)]
            r_PT = [[Res() for _ in range(16)] for _ in range(2)]
            S.op("pool", lambda e: e.memset(Vh[:, :, 256:257], 1.0), writes=r_V)

            def load_wseg(h, si):
                src = wqkv_d[h].rearrange("p (s k c) -> p s k c", s=3, k=8)
                S.dma("pool", lambda e: e.dma_start(out=wqkv[:, si, :, :], in_=src[:, si, :, :], max_dma_last_dim=8192), writes=[r_wseg[si]])

            for si in range(3):
                load_wseg(0, si)
            for h in range(H):
                for (dst, si, r_dst) in ((qT, 0, r_q), (kT, 1, r_k)):
                    for m in range(2):
                        for tg in range(4):
                            ps, r_ps = next_pg()
                            mm(ps[:, :], [(wqkv[:, si, k, m * 128:(m + 1) * 128], uT[:, k, tg * 512:(tg + 1) * 512]) for k in range(8)],
                               reads=[r_wseg[si]] + r_uT[tg * 8:tg * 8 + 8], writes=[r_ps])
                            evac(dst[:, m, tg * 512:(tg + 1) * 512], ps[:, :], reads=[r_ps], writes=[r_dst], )
                    if h + 1 < H:
                        load_wseg(h + 1, si)
                for T in range(16):
                    ps, r_ps = next_pg()
                    mm(ps[:, 0:256], [(uT[:, k, T * 128:(T + 1) * 128], wqkv[:, 2, k, :]) for k in range(8)], reads=[r_wseg[2], r_uT[2 * T], r_uT[2 * T + 1]], writes=[r_ps])
                    evac(Vh[:, T, 0:256], ps[:, 0:256], reads=[r_ps], writes=[r_V[T]])
                if h + 1 < H:
                    load_wseg(h + 1, 2)
                for g in range(4):
                    nk = 4 * g + 4
                    emit_conv(1)
                    for m in range(2):
                        for j in range(nk):
                            c0 = max(0, j - 4 * g) * 128
                            ps, r_ps = next_pg()
                            fns = [lambda e, ps=ps, j=j, c0=c0, m=m, g=g: e.matmul(ps[:, c0:512], lhsT=kT[:, m, j * 128:(j + 1) * 128],
                                                                                 rhs=qT[:, m, g * 512 + c0:(g + 1) * 512], start=True, stop=False)]
                            lo_q = j - 4 * g
                            if lo_q >= 0:
                                ncol = 256 if lo_q <= 2 else 128
                                cs, bs = lo_q * 128, 0
                            else:
                                ncol, cs, bs = (128, 0, 128) if lo_q == -1 else (0, 0, 0)
                            if ncol > 0:
                                for hl in range(2):
                                    fns.append(lambda e, ps=ps, cs=cs, ncol=ncol, bs=bs, hl=hl, h=h: e.matmul(ps[:, cs:cs + ncol], lhsT=identb[:], rhs=bcat[:, h, hl, bs:bs + ncol],
                                                                                                         start=False, stop=(hl == 1)))
                            else:
                                fns[0] = lambda e, ps=ps, j=j, c0=c0, m=m, g=g: e.matmul(ps[:, c0:512], lhsT=kT[:, m, j * 128:(j + 1) * 128],
                                                                                        rhs=qT[:, m, g * 512 + c0:(g + 1) * 512], start=True, stop=True)
                            S.ops("pe", fns, reads=[r_k, r_q, r_bcat, r_identb], writes=[r_ps])
                            S.op("act", lambda e, ps=ps, m=m, j=j, c0=c0: e.activation(out=PT[m][:, j, c0:512], in_=ps[:, c0:512], func=AF.Exp, scale=SCALE),
                                 reads=[r_ps], writes=[r_PT[m][j]])
                    for li in range(4):
                        i = 4 * g + li
                        ovs = []
                        for m in range(2):
                            pvb, r_pvb = next_pv()
                            mm(pvb[:, 0:257], [(PT[m][:, j, li * 128:(li + 1) * 128], Vh[:, j, 0:257]) for j in range(i + 1)],
                               reads=r_PT[m][0:i + 1] + r_V[0:i + 1], writes=[r_pvb])
                            ovs.append((pvb, r_pvb))
                        (o1, r_o1), (o2, r_o2) = ovs
                        while len(pend_tr) > 1:
                            pend_tr.pop(0)()
                        cs = cnt["cmb"] % 2
                        cnt["cmb"] += 1
                        c3 = (cnt["cmb"] - 1) % 3
                        a1, a2, onb, smc = o1s[cs], o2s[cs], onbs[c3], smcs[:, cs * 8:cs * 8 + 8]
                        r_a1, r_a2, r_onb, r_smc = r_o1s[cs], r_o2s[cs], r_onbs[c3], r_smcs[cs]
                        S.op("dve", lambda e, o1=o1, a1=a1: e.tensor_copy(out=a1[:, 0:257], in_=o1[:, 0:257]), reads=[r_o1], writes=[r_a1])
                        S.op("dve", lambda e, o2=o2, a2=a2: e.tensor_copy(out=a2[:, 0:257], in_=o2[:, 0:257]), reads=[r_o2], writes=[r_a2])
                        S.op("dve", lambda e, a1=a1, smc=smc: e.reciprocal(out=smc[:, 0:1], in_=a1[:, 256:257]), reads=[r_a1], writes=[r_smc])
                        S.op("dve", lambda e, a2=a2, smc=smc: e.reciprocal(out=smc[:, 1:2], in_=a2[:, 256:257]), reads=[r_a2, r_smc], writes=[r_smc])
                        S.op("dve", lambda e, smc=smc: e.tensor_tensor(out=smc[:, 1:2], in0=smc[:, 1:2], in1=lamc[:, 1:2], op=ALU.mult), reads=[r_smc, r_lamc], writes=[r_smc])
                        S.op("dve", lambda e, a1=a1, smc=smc: e.tensor_scalar(out=a1[:, 0:256], in0=a1[:, 0:256], scalar1=smc[:, 0:1], scalar2=None, op0=ALU.mult), reads=[r_a1, r_smc], writes=[r_a1])
                        S.op("dve", lambda e, a1=a1, a2=a2, smc=smc: e.scalar_tensor_tensor(out=a2[:, 0:256], in0=a2[:, 0:256], scalar=smc[:, 1:2], in1=a1[:, 0:256], op0=ALU.mult, op1=ALU.add),
                             reads=[r_a2, r_smc, r_a1], writes=[r_a2])
                        S.op("dve", lambda e, a1=a1, a2=a2, smc=smc: e.scalar_tensor_tensor(out=a1[:, 0:256], in0=a2[:, 0:256], scalar=1.0, in1=a2[:, 0:256], op0=ALU.mult, op1=ALU.mult,
                                                                                            accum_out=smc[:, 2:3]), reads=[r_a2], writes=[r_a1, r_smc])
                        S.op("act", lambda e, smc=smc: e.activation(out=smc[:, 3:4], in_=smc[:, 2:3], func=AF.Ln, bias=epsc[:, 1:2], scale=1.0 / 256.0),
                             reads=[r_smc, r_eps], writes=[r_smc])
                        S.op("act", lambda e, smc=smc: e.activation(out=smc[:, 3:4], in_=smc[:, 3:4], func=AF.Exp, scale=-0.5), reads=[r_smc], writes=[r_smc])
                        S.op("dve", lambda e, a2=a2, onb=onb, smc=smc: e.scalar_tensor_tensor(out=onb, in0=a2[:, 0:256], scalar=smc[:, 3:4], in1=subg[:], op0=ALU.mult, op1=ALU.mult),
                             reads=[r_a2, r_smc, r_subg], writes=[r_onb])

                        def tr_out(i=i, h=h, onb=onb, r_onb=r_onb):
                            pt_ap, r_p = next_pt()
                            S.ops("pe", [(lambda e, cc=cc, pt_ap=pt_ap, onb=onb: e.transpose(out=pt_ap[:, cc * 128:(cc + 1) * 128], in_=onb[:, cc * 128:(cc + 1) * 128], identity=identb[:]))
                                         for cc in range(2)], reads=[r_onb, r_identb], writes=[r_p])
                            evac(oT[:, 2 * h:2 * h + 2, i * 128:(i + 1) * 128], pt_ap[:, 0:256].rearrange("p (a b) -> p a b", a=2), reads=[r_p], writes=[r_oT[i]], )
                        pend_tr.append(tr_out)

            while pend_tr:
                pend_tr.pop(0)()
            if stage == 3 and s == 0:
                S.dma("pool", lambda e: e.dma_start(out=out_d[0:2048, :].rearrange("(c p two) d -> p c (two d)", c=8, two=2), in_=oT[:, :, :]), reads=r_oT)
                S.dma("pool", lambda e: e.dma_start(out=out_d[2048:4096, :].rearrange("(c p two) d -> p c (two d)", c=8, two=2), in_=uT[:, :, :]), reads=r_uT)
                break
            if stop < 3:
                continue
            S.barrier()
            cnt["wide"] = "p3"
            off = 0
            wr2, wa2, w22 = [], [], []
            for _i in range(2):
                a, off = carve(off, [4, 8, 128], BF16); wr2.append(a)
                a, off = carve(off, [8, 128], BF16); wa2.append(a)
                a, off = carve(off, [2, 8, 128], BF16); w22.append(a)
            ybuf, off = carve(off, [1026], F32)
            cbs, off = carve(off, [1024], BF16)
            sa, off = carve(off, [512], F32)
            tmp, off = carve(off, [512], F32)
            zb, off = carve(off, [1024], F32)
            off_cT = off
            cT, off = carve(off, [8, 1024], BF16)
            mT, off = carve(off, [8, 1024], BF16)
            assert off <= ARENA, off
            r_wr2, r_wa2, r_w22 = [Res(), Res()], [Res(), Res()], [[Res(), Res()], [Res(), Res()]]
            r_y, r_cbs, r_sa, r_tmp, r_z = [Res() for _ in range(5)]
            r_cT = [Res() for _ in range(8)]
            r_mT = [Res() for _ in range(8)]

            def load_w1(n):
                j, bi = n % 8, n % 2
                src = wr_d[j].rearrange("p (s k c) -> p s k c", s=5, k=8)
                S.dma("pool", lambda e: e.dma_start(out=wr2[bi], in_=src[:, 0:4, :, :], max_dma_last_dim=8192), writes=[r_wr2[bi]])
                S.dma("pool", lambda e: e.dma_start(out=wa2[bi], in_=wa_d[j].rearrange("p (k c) -> p k c", k=8)), writes=[r_wa2[bi]])

            def load_w2(n):
                j, bi = n % 8, n % 2
                src = wr_d[j].rearrange("p (s k c) -> p s k c", s=5, k=8)
                S.dma("pool", lambda e: e.dma_start(out=w22[bi][:, 0, :, :], in_=src[:, 4, :, :]), writes=[r_w22[bi][0]])
                S.dma("pool", lambda e: e.dma_start(out=w22[bi][:, 1, :, :], in_=wb_d[j].rearrange("p (k c) -> p k c", k=8)), writes=[r_w22[bi][1]])

            load_w1(0)
            for hs in range(2):
                h0 = hs * 1024
                for j in range(8):
                    n1 = hs * 8 + j
                    bi = n1 % 2
                    wr, wa, r_wr, r_wa = wr2[bi], wa2[bi], r_wr2[bi], r_wa2[bi]
                    if j + 1 < 8:
                        load_w1(n1 + 1)
                    else:
                        load_w2(hs * 8)
                    if hs == 0:
                        S.op("dve", lambda e: e.memset(ybuf[:, 0:2], 0.0), writes=[r_y])
                    else:
                        S.op("dve", lambda e, j=j: e.tensor_copy(out=ybuf[:, 0:2], in_=halo[:, j, :]), reads=[r_halo], writes=[r_y])
                    for tg2 in range(2):
                        t0 = h0 + tg2 * 512
                        rU = r_uT[t0 // 64:t0 // 64 + 8]
                        l0 = tg2 * 512

                        def proj(si):
                            ps, r_ps = next_pg()
                            mm(ps[:, :], [(wr[:, si, k, :], uT[:, k, t0:t0 + 512]) for k in range(8)], reads=[r_wr] + rU, writes=[r_ps])
                            return ps, r_ps
                        pc, r_pc = proj(1)
                        S.op("act", lambda e, pc=pc: e.activation(out=tmp, in_=pc[:, :], func=AF.Copy), reads=[r_pc], writes=[r_tmp])
                        ph, r_ph = proj(2)
                        S.op("dve", lambda e, ph=ph, l0=l0: e.tensor_tensor(out=ybuf[:, 2 + l0:2 + l0 + 512], in0=ph[:, :], in1=tmp, op=ALU.mult),
                             reads=[r_ph, r_tmp], writes=[r_y])
                        pb, r_pb = proj(0)
                        S.op("act", lambda e, pb=pb, l0=l0: e.activation(out=cbs[:, l0:l0 + 512], in_=pb[:, :], func=AF.Copy), reads=[r_pb], writes=[r_cbs])
                        pa, r_pa = proj(3)
                        S.op("act", lambda e, pa=pa, j=j: e.activation(out=sa, in_=pa[:, :], func=AF.Sigmoid, bias=pcols[:, PC_BGA + j:PC_BGA + j + 1], scale=1.0),
                             reads=[r_pa, r_pcols], writes=[r_sa])
                        pya, r_pya = next_pg()
                        mm(pya[:, :], [(wa[:, k, :], oT[:, k, t0:t0 + 512]) for k in range(8)], reads=[r_wa] + r_oT[t0 // 128:t0 // 128 + 4], writes=[r_pya])
                        S.op("dve", lambda e, pya=pya, j=j, l0=l0: e.tensor_tensor(out=mT[:, j, l0:l0 + 512], in0=pya[:, :], in1=sa, op=ALU.mult),
                             reads=[r_pya, r_sa], writes=[r_mT[j]])
                    cw = lambda w, j=j: pcols[:, PC_CW + w * 8 + j:PC_CW + w * 8 + j + 1]
                    S.op("dve", lambda e, cw=cw: e.tensor_scalar(out=zb, in0=ybuf[:, 2:1026], scalar1=cw(2), scalar2=None, op0=ALU.mult), reads=[r_y, r_pcols], writes=[r_z])
                    S.op("dve", lambda e, cw=cw: e.scalar_tensor_tensor(out=zb, in0=ybuf[:, 1:1025], scalar=cw(1), in1=zb, op0=ALU.mult, op1=ALU.add),
                         reads=[r_y, r_z, r_pcols], writes=[r_z])
                    S.op("dve", lambda e, cw=cw: e.scalar_tensor_tensor(out=zb, in0=ybuf[:, 0:1024], scalar=cw(0), in1=zb, op0=ALU.mult, op1=ALU.add),
                         reads=[r_y, r_z, r_pcols], writes=[r_z])
                    S.op("pool", lambda e, j=j: e.tensor_tensor(out=cT[:, j, :], in0=zb, in1=cbs, op=ALU.mult), reads=[r_z, r_cbs], writes=[r_cT[j]])
                    if hs == 0:
                        S.op("dve", lambda e, j=j: e.tensor_copy(out=halo[:, j, :], in_=ybuf[:, 1024:1026]), reads=[r_y], writes=[r_halo])
                for j in range(8):
                    n2 = hs * 8 + j
                    bi = n2 % 2
                    w2, r_w2 = w22[bi], r_w22[bi]
                    if j + 1 < 8:
                        load_w2(n2 + 1)
                    elif hs == 0:
                        load_w1(8)
                    for tg2 in range(2):
                        l0 = tg2 * 512
                        t0 = h0 + l0
                        pgb, r_pgb = next_pg()
                        mm(pgb[:, :], [(w2[:, 0, k, :], uT[:, k, t0:t0 + 512]) for k in range(8)], reads=[r_w2[0]] + r_uT[t0 // 64:t0 // 64 + 8], writes=[r_pgb])
                        S.op("act", lambda e, pgb=pgb, j=j: e.activation(out=sa, in_=pgb[:, :], func=AF.Sigmoid, bias=pcols[:, PC_BGB + j:PC_BGB + j + 1], scale=1.0),
                             reads=[r_pgb, r_pcols], writes=[r_sa])
                        pyb, r_pyb = next_pg()
                        mm(pyb[:, :], [(w2[:, 1, k, :], cT[:, k, l0:l0 + 512]) for k in range(8)], reads=[r_w2[1]] + r_cT, writes=[r_pyb])
                        S.op("dve", lambda e, pyb=pyb: e.tensor_tensor(out=tmp, in0=pyb[:, :], in1=sa, op=ALU.mult), reads=[r_pyb, r_sa], writes=[r_tmp])
                        S.op("pool", lambda e, j=j, l0=l0: e.tensor_tensor(out=mT[:, j, l0:l0 + 512], in0=mT[:, j, l0:l0 + 512], in1=tmp, op=ALU.add),
                             reads=[r_tmp, r_mT[j]], writes=[r_mT[j]])
                if stage == 4 and s == 0 and hs == 0:
                    for ii, (buf, rr) in enumerate(((mT, r_mT), (cT, r_cT))):
                        S.dma("pool", lambda e, ii=ii, buf=buf: e.dma_start(out=out_d[ii * 1024:(ii + 1) * 1024, :].rearrange("(c p) d -> p c d", c=8), in_=buf), reads=rr)
                    break
                if stage == 5 and hs == 1:
                    break
                WS = [{"u": (wk[0][:, :], [r_wk[0]]), "t": (wk[1][:, :], [r_wk[1]]), "n": (wk[2][:, :], [r_wk[2]]), "xb": (xh[0][:, :], [r_xh[0]])},
                      {"u": (arena[:, off_cT:off_cT + 4096].bitcast(F32), r_cT[0:2]), "t": (arena[:, off_cT + 4096:off_cT + 8192].bitcast(F32), r_cT[2:4]),
                       "n": (arena[:, off_cT + 8192:off_cT + 12288].bitcast(F32), r_cT[4:6]), "xb": (arena[:, off_cT + 12288:off_cT + 14336].bitcast(BF16), r_cT[6:7])}]
                def ln1_A(tl):
                    T = hs * 8 + tl
                    gT = s * 16 + T
                    row0 = base + T * 128
                    b = gT % 2
                    W = WS[tl % 2] if stage == 2 else WS[0]
                    (Wu, Ru), (Wt, Rt), (Wn, Rn) = W["u"], W["t"], W["n"]
                    if tl == 0:
                        S.dma("sp", lambda e: e.dma_start(out=xs[b][:], in_=x_d[row0:row0 + 128, :]), writes=[r_xs[b]])
                    if tl + 1 < 8:
                        S.dma("sp", lambda e: e.dma_start(out=xs[1 - b][:], in_=x_d[row0 + 128:row0 + 256, :]), writes=[r_xs[1 - b]])
                    S.op("act", lambda e: e.activation(out=Wu, in_=xs[b][:], func=AF.Identity, bias=nmr_in[:, gT:gT + 1], scale=rstd_in[:, gT:gT + 1]),
                         reads=[r_xs[b], r_mvs[gT]], writes=Ru)
                    S.op("pool", lambda e: e.tensor_tensor(out=Wu, in0=Wu, in1=gin_row, op=ALU.mult), reads=Ru + [r_prow], writes=Ru)
                    S.op("pool", lambda e: e.tensor_tensor(out=Wu, in0=Wu, in1=bin_row, op=ALU.add), reads=Ru + [r_prow], writes=Ru)

                def ln1_A2(tl):
                    W = WS[tl % 2] if stage == 2 else WS[0]
                    (Wu, Ru), (Wt, Rt), (Wn, Rn) = W["u"], W["t"], W["n"]
                    for hh in range(2):
                        ps, r_ps = next_pg()
                        mm(ps[:, :], [(mT[:, k, tl * 128:(tl + 1) * 128], wo_s[:, k, hh * 512:(hh + 1) * 512]) for k in range(8)], reads=r_mT + [r_wo], writes=[r_ps])
                        S.op("dve", lambda e, ps=ps, hh=hh: e.scalar_tensor_tensor(out=Wt[:, hh * 512:(hh + 1) * 512], in0=Wu[:, hh * 512:(hh + 1) * 512], scalar=ALPHA,
                                                                                   in1=ps[:, :], op0=ALU.mult, op1=ALU.add),
                             reads=[r_ps] + Ru, writes=Rt)
                    return ln_stats(Wt, Rt, tl % 2)

                def ln1_B(tl, sm, r_sm):
                    T = hs * 8 + tl
                    gT = s * 16 + T
                    row0 = base + T * 128
                    W = WS[tl % 2] if stage == 2 else WS[0]
                    (Wu, Ru), (Wt, Rt), (Wn, Rn) = W["u"], W["t"], W["n"]
                    S.op("act", lambda e: e.activation(out=Wn, in_=Wt, func=AF.Identity, bias=sm[:, 9:10], scale=sm[:, 8:9]),
                         reads=Rt + [r_sm], writes=Rn)
                    S.op("pool", lambda e: e.tensor_tensor(out=Wn, in0=Wn, in1=prow[:, PR_G1:PR_G1 + 1024], op=ALU.mult), reads=Rn + [r_prow], writes=Rn)
                    S.op("dve", lambda e: e.tensor_tensor(out=Wu, in0=Wn, in1=prow[:, PR_B1:PR_B1 + 1024], op=ALU.add), reads=Rn + [r_prow], writes=Ru)
                    if stage == 6:
                        S.dma("sp", lambda e: e.dma_start(out=out_d[row0:row0 + 128, :], in_=wk[1][:]), reads=[r_wk[1]])
                    elif stage == 1:
                        S.dma("sp", lambda e: e.dma_start(out=out_d[row0:row0 + 128, :], in_=wk[0][:]), reads=[r_wk[0]])
                    else:
                        route_tile(gT, row0, W)

                if stage == 2:
                    ln1_A(0)
                    for tl in range(8):
                        if tl + 1 < 8:
                            ln1_A(tl + 1)
                        pend = ln1_A2(tl)
                        ln1_B(tl, *pend)
                else:
                    for tl in range(8):
                        ln1_A(tl)
                        pend = ln1_A2(tl)
                        if stage == 5:
                            S.dma("sp", lambda e: e.dma_start(out=out_d[0:128, :], in_=wk[0][:]), reads=[r_wk[0]])
                            S.dma("sp", lambda e: e.dma_start(out=out_d[128:256, :], in_=wk[1][:]), reads=[r_wk[1]])
                            break
                        ln1_B(tl, *pend)
            if stage in (4, 5):
                break
            S.barrier()

        if stage == 2:
            moe_phase()
        S.finish()
        S.emit()
    return nc


_PROG = {}


def _tile_w(w, ncol_blocks, cb):
    return np.ascontiguousarray(w.reshape(8, 128, ncol_blocks, cb).transpose(2, 1, 0, 3).reshape(ncol_blocks, 128, 8 * cb))


def _cols(v):
    return np.ascontiguousarray(np.asarray(v, np.float32).reshape(8, 128).T)


def kernel(stage=2, **inp):
    f = lambda a: np.asarray(a, dtype=np.float32)
    x = f(inp["x"])
    w_in = f(inp["w_in"])[0]
    wq = _tile_w(w_in[:, 0:1024], 4, 256).reshape(4, 128, 8, 256)
    wk_ = _tile_w(w_in[:, 1024:2048], 4, 256).reshape(4, 128, 8, 256)
    wv = _tile_w(w_in[:, 2048:3072], 4, 256).reshape(4, 128, 8, 256)
    wqkv = np.ascontiguousarray(np.stack([wq, wk_, wv], axis=2).reshape(4, 128, 3 * 8 * 256))
    wr5 = [_tile_w(w_in[:, 3072 + si * 1024:3072 + (si + 1) * 1024], 8, 128).reshape(8, 128, 8, 128) for si in range(5)]
    wr = np.ascontiguousarray(np.stack(wr5, axis=2).reshape(8, 128, 5 * 8 * 128))
    wa = _tile_w(f(inp["w_a_proj"])[0], 8, 128)
    wb = _tile_w(f(inp["w_b_proj"])[0], 8, 128)
    wo = _tile_w(f(inp["w_o"])[0], 1, 1024)[0]
    pcols = np.zeros((128, PC_N), np.float32)
    pcols[:, PC_GIN:PC_GIN + 8] = _cols(inp["ln_in_g"])
    pcols[:, PC_BIN:PC_BIN + 8] = _cols(inp["ln_in_b"])
    pcols[:, PC_BGA:PC_BGA + 8] = _cols(f(inp["b_gate"])[0, 0])
    pcols[:, PC_BGB:PC_BGB + 8] = _cols(f(inp["b_gate"])[0, 1])
    cw = f(inp["conv_w"])[0, :, 0, :]
    for w in range(3):
        pcols[:, PC_CW + w * 8:PC_CW + (w + 1) * 8] = _cols(cw[w])
    prow1 = np.zeros((PR_N,), np.float32)
    prow1[PR_G1:PR_G1 + 1024] = f(inp["ln1_g"])[0]
    prow1[PR_B1:PR_B1 + 1024] = f(inp["ln1_b"])[0]
    prow1[PR_SUBG:PR_SUBG + 256] = f(inp["subln_g"])[0]
    prow1[PR_G2:PR_G2 + 1024] = f(inp["ln_in_g"])
    prow1[PR_B2:PR_B2 + 1024] = f(inp["ln_in_b"])
    prow1[PR_BR:PR_BR + 4] = f(inp["b_group"])[0]
    prow1[PR_BR + 4:PR_BR + 36] = f(inp["b_sub"])[0].reshape(-1)
    prow = np.ascontiguousarray(np.broadcast_to(prow1[None, :], (128, PR_N)))
    prs1 = np.zeros((PS_N,), np.float32)
    prs1[PS_LQ:PS_LQ + 256] = f(inp["lambda_q"])[0].reshape(-1)
    prs1[PS_LK:PS_LK + 256] = f(inp["lambda_k"])[0].reshape(-1)
    prs1[PS_RB:PS_RB + 128] = f(inp["rel_bias"]).reshape(-1)
    prows = np.ascontiguousarray(np.broadcast_to(prs1[None, :], (128, PS_N)))
    ident = np.eye(128, dtype=np.float32)
    shared = {"wqkv": wqkv, "wr": wr, "wa": wa, "wb": wb, "wo": wo, "pcols": pcols, "prow": prow, "prows": prows, "oh": _OH_NP, "negm": _NEGM_NP,
              "ident": ident}
    if stage == 2:
        prow2 = np.ascontiguousarray(np.broadcast_to(np.concatenate([f(inp["ln2_g"])[0], f(inp["ln2_b"])[0]])[None, :], (128, 2048)))
        srow = np.ascontiguousarray(np.broadcast_to((np.arange(32, dtype=np.float32) * CAP - BIGIDX)[None, :], (128, 32)))
        tb0 = np.zeros((128, NT, 2, 4), np.float32)
        tokid = np.arange(NT, dtype=np.float32)[None, :] * 128 + np.arange(128, dtype=np.float32)[:, None]
        for k in range(2):
            tb0[:, :, k, 0] = tokid
            tb0[:, :, k, 2] = 2 * tokid + k
        ustr = np.triu(np.ones((128, 128), np.float32), 1)
        wrf = np.concatenate([f(inp["w_group"])[0]] + [f(inp["w_sub"])[0, g] for g in range(4)], axis=1)
        wrt = np.ascontiguousarray(wrf.reshape(8, 128, 36).transpose(1, 0, 2).reshape(128, 8 * 36))
        wg = np.ascontiguousarray(f(inp["w_gate_e"])[0].reshape(NE, 8, 128, 512).transpose(0, 2, 1, 3).reshape(NE, 128, 8 * 512))
        wu = np.ascontiguousarray(f(inp["w_up_e"])[0].reshape(NE, 8, 128, 512).transpose(0, 2, 1, 3).reshape(NE, 128, 8 * 512))
        wd = np.ascontiguousarray(f(inp["w_down_e"])[0].reshape(NE, 4, 128, 1024).transpose(0, 2, 1, 3).reshape(NE, 128, 4 * 1024))
        shared.update({"prow2": prow2, "srow": srow, "tball0": np.ascontiguousarray(tb0.reshape(128, NT * 8)), "ustr": ustr, "wrt": wrt, "wg": wg, "wu": wu, "wd": wd})
    if stage not in _PROG:
        _PROG[stage] = build_program(stage)
    nc = _PROG[stage]
    xr = x.reshape(NCORES, NTOK, D)
    in_maps = [dict(shared, x=np.ascontiguousarray(xr[c])) for c in range(NCORES)]
    res = run_bass_kernel_spmd(nc, in_maps, core_ids=list(range(NCORES)))
    out = np.stack([np.asarray(r["out"], np.float32) for r in res.results], axis=0)
    return out.reshape(16, SEQ, D)
```
